# Optimizing a Trainium2 kernel written in Bass

```python
import math
import jax
import jax.numpy as jnp
from jax import lax
import numpy as np

D_MODEL = 1024
BATCH = 8
SEQ = 4096
DEPTH = 1

GRID_W = 64
CTX_LEN = 256
EPS = 1e-6

M_HEADS = 8
M_DQK = 64
M_DV = 128
M_QK_W = M_HEADS * M_DQK
M_V_W = M_HEADS * M_DV
M_CHUNK = 128
CONV_K = 3

S5_WIDTH = 512
S5_GROUP = 16
S5_GROUPS = S5_WIDTH // S5_GROUP
S5_STATE = 64

N_EXPERTS = 32
TOP_K = 4
D_FF = D_MODEL
SWIGLU_LIMIT = 7.0
SWIGLU_ALPHA = 1.702

OFF_QK = 0
OFF_V = OFF_QK + 2 * M_QK_W
OFF_IF = OFF_V + M_V_W
N_IF = 2 * 2 * M_HEADS
OFF_U = OFF_IF + N_IF
STATE_COLS = OFF_U + S5_WIDTH
OFF_O = STATE_COLS
OFF_G = OFF_O + M_V_W
IN_COLS = OFF_G + 2 * D_MODEL
N_MOD = 6

kernel_name = 'hybrid_mlstm_s5_moe_diffusion_block'


def rmsnorm(x, g):
    xf = x.astype(jnp.float32)
    y = xf * lax.rsqrt(jnp.mean(xf * xf, axis=-1, keepdims=True) + EPS)
    return (y * g.astype(jnp.float32)).astype(x.dtype)


def modulate(h, shift, scale):
    return h * (1.0 + scale) + shift


def conv_ctx(x, w):
    length = x.shape[1]
    w = w.astype(x.dtype)
    xp = jnp.pad(x, ((0, 0), (1, 1), (0, 0)))
    return xp[:, 0:length] * w[1, 0] + xp[:, 1:length + 1] * w[1, 1] + xp[:, 2:length + 2] * w[1, 2]


def conv_lat(x, w):
    bsz, length, ch = x.shape
    rows = length // GRID_W
    xg = x.reshape(bsz, rows, GRID_W, ch)
    y = lax.conv_general_dilated(xg, w.astype(x.dtype).reshape(CONV_K, CONV_K, 1, ch), (1, 1), 'SAME',
                                 dimension_numbers=('NHWC', 'HWIO', 'NHWC'), feature_group_count=ch)
    return y.reshape(bsz, length, ch)


def split_heads(a, dh):
    bsz, length, _ = a.shape
    return a.reshape(bsz, length, -1, dh).transpose(0, 2, 1, 3)


def mlstm_chunkwise(q, k, v, li, lf, state, return_h):
    bsz, nh, length, _ = q.shape
    nc = length // M_CHUNK

    def chunks(a):
        return jnp.moveaxis(a.reshape(bsz, nh, nc, M_CHUNK, *a.shape[3:]), 2, 0)

    lower = jnp.tril(jnp.ones((M_CHUNK, M_CHUNK), dtype=bool))

    def step(carry, xs):
        cm, nv, m = carry
        qc, kc, vc, lic, lfc = xs
        b = jnp.cumsum(lfc, axis=-1)
        b_end = b[..., -1]
        w_end = b_end[..., None] - b + lic
        m_new = jnp.maximum(b_end + m, jnp.max(w_end, axis=-1))
        dec = jnp.exp(b_end + m - m_new)
        we = jnp.exp(w_end - m_new[..., None])
        c_new = dec[..., None, None] * cm + jnp.einsum('bhs,bhsv,bhsk->bhvk', we, vc, kc)
        n_new = dec[..., None] * nv + jnp.einsum('bhs,bhsk->bhk', we, kc)
        if not return_h:
            return (c_new, n_new, m_new), None
        logd = jnp.where(lower, b[..., :, None] - b[..., None, :] + lic[..., None, :], -jnp.inf)
        inter = b + m[..., None]
        mt = jnp.maximum(inter, jnp.max(logd, axis=-1))
        s = jnp.einsum('bhtk,bhsk->bhts', qc, kc) * jnp.exp(logd - mt[..., None])
        ie = jnp.exp(inter - mt)
        num = ie[..., None] * jnp.einsum('bhvk,bhtk->bhtv', cm, qc) + jnp.einsum('bhts,bhsv->bhtv', s, vc)
        den = ie * jnp.einsum('bhk,bhtk->bht', nv, qc) + jnp.sum(s, axis=-1)
        h = num / jnp.maximum(jnp.abs(den), jnp.exp(-mt))[..., None]
        return (c_new, n_new, m_new), h

    state, hs = lax.scan(step, state, (chunks(q), chunks(k), chunks(v), chunks(li), chunks(lf)))
    if not return_h:
        return state, None
    return state, jnp.moveaxis(hs, 0, 2).reshape(bsz, nh, length, -1)


def mlstm_dir(q, k, v, li, lf, state, reverse, return_h):
    if reverse:
        q, k, v, li, lf = (jnp.flip(a, axis=2) for a in (q, k, v, li, lf))
    state, h = mlstm_chunkwise(q, k, v, li, lf, state, return_h)
    if reverse and return_h:
        h = jnp.flip(h, axis=2)
    return state, h


def s5_discretise(a_re, a_im, log_dt, b_re, b_im):
    f32 = jnp.float32
    a_re, a_im, b_re, b_im = (t.astype(f32) for t in (a_re, a_im, b_re, b_im))
    dt = jnp.exp(log_dt.astype(f32))[:, None]
    mag = jnp.exp(dt * a_re)
    ang = dt * a_im
    ab_re = mag * jnp.cos(ang)
    ab_im = mag * jnp.sin(ang)
    den = a_re * a_re + a_im * a_im
    xr = ab_re - 1.0
    cf_re = (xr * a_re + ab_im * a_im) / den
    cf_im = (ab_im * a_re - xr * a_im) / den
    bb_re = cf_re[..., None] * b_re - cf_im[..., None] * b_im
    bb_im = cf_re[..., None] * b_im + cf_im[..., None] * b_re
    return ab_re, ab_im, bb_re, bb_im


def _complex_affine_combine(e1, e2):
    a1r, a1i, b1r, b1i = e1
    a2r, a2i, b2r, b2i = e2
    return (a2r * a1r - a2i * a1i,
            a2r * a1i + a2i * a1r,
            a2r * b1r - a2i * b1i + b2r,
            a2r * b1i + a2i * b1r + b2i)


def s5_scan(bu_re, bu_im, ab_re, ab_im, h0, reverse):
    h0_re, h0_im = h0
    first = -1 if reverse else 0
    bu_re = bu_re.at[:, first].add(ab_re * h0_re - ab_im * h0_im)
    bu_im = bu_im.at[:, first].add(ab_re * h0_im + ab_im * h0_re)
    shape = bu_re.shape
    elems = (jnp.broadcast_to(ab_re, shape), jnp.broadcast_to(ab_im, shape), bu_re, bu_im)
    _, _, hr, hi = lax.associative_scan(_complex_affine_combine, elems, reverse=reverse, axis=1)
    last = 0 if reverse else -1
    return hr, hi, (hr[:, last], hi[:, last])


def mixer_core(proj, conv_fn, conv_w, b_if, disc, c_re, c_im, d_skip, m_state, s_state, return_out):
    f32 = jnp.float32
    bsz, length, _ = proj.shape
    qk = jax.nn.silu(conv_fn(proj[..., OFF_QK:OFF_V], conv_w)).astype(f32)
    q = split_heads(qk[..., :M_QK_W], M_DQK) * (M_DQK ** -0.5)
    k = split_heads(qk[..., M_QK_W:], M_DQK)
    v = split_heads(proj[..., OFF_V:OFF_IF].astype(f32), M_DV)
    gif = (proj[..., OFF_IF:OFF_U].reshape(bsz, length, 2, 2, M_HEADS) + b_if).astype(f32)
    gif = jnp.transpose(gif, (2, 3, 0, 4, 1))
    u = proj[..., OFF_U:STATE_COLS].astype(f32).reshape(bsz, length, S5_GROUPS, S5_GROUP)
    m_final, s_final, h_dirs, y_dirs = [], [], [], []
    for d in range(2):
        rev = d == 1
        st, h = mlstm_dir(q, k, v, gif[d, 0], jax.nn.log_sigmoid(gif[d, 1]), m_state[d], rev, return_out)
        ab_re, ab_im, bb_re, bb_im = disc[d]
        bu_re = jnp.einsum('blgc,gpc->blgp', u, bb_re)
        bu_im = jnp.einsum('blgc,gpc->blgp', u, bb_im)
        hr, hi, fin = s5_scan(bu_re, bu_im, ab_re, ab_im, s_state[d], rev)
        m_final.append(st)
        s_final.append(fin)
        if return_out:
            h_dirs.append(h)
            y_dirs.append(jnp.einsum('blgp,gcp->blgc', hr, c_re[d].astype(f32))
                          - jnp.einsum('blgp,gcp->blgc', hi, c_im[d].astype(f32)))
    if not return_out:
        return m_final, s_final, None, None
    h_m = h_dirs[0] + h_dirs[1]
    y_s = y_dirs[0] + y_dirs[1] + d_skip.astype(f32).reshape(S5_GROUPS, S5_GROUP) * u
    return m_final, s_final, h_m, y_s.reshape(bsz, length, S5_WIDTH)


def mixer_out(proj, h_m, y_s, g_mh, w_glu, b_glu, w_a, w_b, b_gate, w_o):
    dt = proj.dtype
    f32 = jnp.float32
    bsz, length, _ = proj.shape
    hn = h_m * lax.rsqrt(jnp.mean(h_m * h_m, axis=-1, keepdims=True) + EPS)
    hn = hn.transpose(0, 2, 1, 3).reshape(bsz, length, M_V_W) * g_mh.astype(f32)
    o_gate = jax.nn.sigmoid(proj[..., OFF_O:OFF_G].astype(f32))
    y_a = (hn * o_gate).astype(dt) @ w_a
    ys = jax.nn.gelu(y_s).astype(dt)
    ys = ys * jax.nn.sigmoid(ys @ w_glu + b_glu)
    y_b = ys @ w_b
    gates = jax.nn.sigmoid(proj[..., OFF_G:IN_COLS] + b_gate)
    merged = gates[..., :D_MODEL] * y_a + gates[..., D_MODEL:] * y_b
    return merged @ w_o


def moe(h, w_router, b_router, w_e_in, b_e_in, w_e_out, b_e_out):
    logits = (h @ w_router + b_router).astype(jnp.float32)
    top_v, top_i = lax.top_k(logits, TOP_K)
    probs = jax.nn.softmax(top_v, axis=-1)
    gates = jnp.einsum('nk,nke->ne', probs, jax.nn.one_hot(top_i, N_EXPERTS, dtype=jnp.float32)).astype(h.dtype)
    out = jnp.zeros_like(h)
    for e in range(N_EXPERTS):
        z = h @ w_e_in[e] + b_e_in[e]
        glu = jnp.minimum(z[:, :D_FF], SWIGLU_LIMIT)
        lin = jnp.clip(z[:, D_FF:], -SWIGLU_LIMIT, SWIGLU_LIMIT)
        act = glu * jax.nn.sigmoid(SWIGLU_ALPHA * glu) * (lin + 1.0)
        out = out + gates[:, e:e + 1] * (act @ w_e_out[e] + b_e_out[e])
    return out


def setup_inputs(seed: int = 0) -> dict:
    key = jax.random.key(seed)
    ks = jax.random.split(key, 40)
    f32 = jnp.float32
    D = D_MODEL

    def nrm(k, shape, scale):
        return jax.random.normal(k, shape, f32) * scale

    x = nrm(ks[0], (BATCH, SEQ, D), 1.0)
    c = nrm(ks[1], (BATCH, D), 1.0)
    ctx = nrm(ks[2], (BATCH, CTX_LEN, D), 1.0)
    c_ctx = nrm(ks[3], (D,), 1.0)
    w_ada = nrm(ks[4], (DEPTH, D, N_MOD * D), 0.5 * D ** -0.5)
    b_ada = nrm(ks[5], (DEPTH, N_MOD * D), 0.02)
    g_norm1 = 1.0 + nrm(ks[6], (DEPTH, D), 0.02)
    g_norm2 = 1.0 + nrm(ks[7], (DEPTH, D), 0.02)
    w_in = nrm(ks[8], (DEPTH, D, IN_COLS), D ** -0.5)
    w_conv_qk = nrm(ks[9], (DEPTH, CONV_K, CONV_K, 2 * M_QK_W), 1.0 / CONV_K)
    b_i = nrm(ks[10], (DEPTH, 2, 1, M_HEADS), 0.1)
    b_f = jnp.linspace(3.0, 6.0, M_HEADS, dtype=f32) + nrm(ks[11], (DEPTH, 2, 1, M_HEADS), 0.1)
    b_ifgate = jnp.concatenate([b_i, b_f], axis=2)
    g_mh = 1.0 + nrm(ks[12], (DEPTH, M_V_W), 0.02)
    w_branch_m = nrm(ks[13], (DEPTH, M_V_W, D), M_V_W ** -0.5)
    s5_a_re = -0.5 * jnp.exp(nrm(ks[14], (DEPTH, 2, S5_GROUPS, S5_STATE), 0.02))
    s5_a_im = math.pi * jnp.arange(S5_STATE, dtype=f32) + nrm(ks[15], (DEPTH, 2, S5_GROUPS, S5_STATE), 0.01)
    s5_log_dt = jax.random.uniform(ks[16], (DEPTH, 2, S5_GROUPS), f32, math.log(1e-3), math.log(1e-1))
    s5_b_re = nrm(ks[17], (DEPTH, 2, S5_GROUPS, S5_STATE, S5_GROUP), (2 * S5_GROUP) ** -0.5)
    s5_b_im = nrm(ks[18], (DEPTH, 2, S5_GROUPS, S5_STATE, S5_GROUP), (2 * S5_GROUP) ** -0.5)
    s5_c_re = nrm(ks[19], (DEPTH, 2, S5_GROUPS, S5_GROUP, S5_STATE), 0.5)
    s5_c_im = nrm(ks[20], (DEPTH, 2, S5_GROUPS, S5_GROUP, S5_STATE), 0.5)
    s5_d = nrm(ks[21], (DEPTH, S5_WIDTH), 1.0)
    w_glu = nrm(ks[22], (DEPTH, S5_WIDTH, S5_WIDTH), S5_WIDTH ** -0.5)
    b_glu = nrm(ks[23], (DEPTH, S5_WIDTH), 0.02)
    w_branch_s = nrm(ks[24], (DEPTH, S5_WIDTH, D), S5_WIDTH ** -0.5)
    b_merge_gate = nrm(ks[25], (DEPTH, 2 * D), 0.02)
    w_o = nrm(ks[26], (DEPTH, D, D), D ** -0.5)
    w_router = nrm(ks[27], (DEPTH, D, N_EXPERTS), D ** -0.5)
    b_router = nrm(ks[28], (DEPTH, N_EXPERTS), 0.01)
    w_e_in = nrm(ks[29], (DEPTH, N_EXPERTS, D, 2 * D_FF), D ** -0.5)
    b_e_in = nrm(ks[30], (DEPTH, N_EXPERTS, 2 * D_FF), 0.01)
    w_e_out = nrm(ks[31], (DEPTH, N_EXPERTS, D_FF, D), D_FF ** -0.5)
    b_e_out = nrm(ks[32], (DEPTH, N_EXPERTS, D), 0.01)
    g_final = 1.0 + nrm(ks[33], (D,), 0.02)
    return {'x': x, 'c': c, 'ctx': ctx, 'c_ctx': c_ctx, 'w_ada': w_ada, 'b_ada': b_ada,
            'g_norm1': g_norm1, 'g_norm2': g_norm2, 'w_in': w_in, 'w_conv_qk': w_conv_qk,
            'b_ifgate': b_ifgate, 'g_mh': g_mh, 'w_branch_m': w_branch_m,
            's5_a_re': s5_a_re, 's5_a_im': s5_a_im, 's5_log_dt': s5_log_dt,
            's5_b_re': s5_b_re, 's5_b_im': s5_b_im, 's5_c_re': s5_c_re, 's5_c_im': s5_c_im,
            's5_d': s5_d, 'w_glu': w_glu, 'b_glu': b_glu, 'w_branch_s': w_branch_s,
            'b_merge_gate': b_merge_gate, 'w_o': w_o, 'w_router': w_router, 'b_router': b_router,
            'w_e_in': w_e_in, 'b_e_in': b_e_in, 'w_e_out': w_e_out, 'b_e_out': b_e_out,
            'g_final': g_final}


def reference(x, c, ctx, c_ctx, w_ada, b_ada, g_norm1, g_norm2, w_in, w_conv_qk, b_ifgate, g_mh,
              w_branch_m, s5_a_re, s5_a_im, s5_log_dt, s5_b_re, s5_b_im, s5_c_re, s5_c_im, s5_d,
              w_glu, b_glu, w_branch_s, b_merge_gate, w_o, w_router, b_router, w_e_in, b_e_in,
              w_e_out, b_e_out, g_final):
    f32 = jnp.float32
    D = D_MODEL
    bsz = x.shape[0]
    m_zero = (jnp.zeros((bsz, M_HEADS, M_DV, M_DQK), f32), jnp.zeros((bsz, M_HEADS, M_DQK), f32),
              jnp.zeros((bsz, M_HEADS), f32))
    s_zero = (jnp.zeros((bsz, S5_GROUPS, S5_STATE), f32), jnp.zeros((bsz, S5_GROUPS, S5_STATE), f32))
    for l in range(DEPTH):
        last = l == DEPTH - 1
        mod = jax.nn.silu(c) @ w_ada[l] + b_ada[l]
        sh1, sc1, gt1, sh2, sc2, gt2 = jnp.split(mod[:, None, :], N_MOD, axis=-1)
        n_mod_c = 2 if last else N_MOD
        mod_c = jnp.split(jax.nn.silu(c_ctx) @ w_ada[l][:, :n_mod_c * D] + b_ada[l][:n_mod_c * D], n_mod_c)
        h_l = modulate(rmsnorm(x, g_norm1[l]), sh1, sc1)
        h_c = modulate(rmsnorm(ctx, g_norm1[l]), mod_c[0], mod_c[1])
        n_cols_c = STATE_COLS if last else IN_COLS
        proj_c = h_c @ w_in[l][:, :n_cols_c]
        proj_l = h_l @ w_in[l]
        disc = [s5_discretise(s5_a_re[l, d], s5_a_im[l, d], s5_log_dt[l, d], s5_b_re[l, d], s5_b_im[l, d])
                for d in range(2)]
        m_ctx, s_ctx, hm_c, ys_c = mixer_core(proj_c, conv_ctx, w_conv_qk[l], b_ifgate[l], disc,
                                              s5_c_re[l], s5_c_im[l], s5_d[l],
                                              [m_zero, m_zero], [s_zero, s_zero], not last)
        _, _, hm_l, ys_l = mixer_core(proj_l, conv_lat, w_conv_qk[l], b_ifgate[l], disc,
                                      s5_c_re[l], s5_c_im[l], s5_d[l], m_ctx, s_ctx, True)
        x = x + gt1 * mixer_out(proj_l, hm_l, ys_l, g_mh[l], w_glu[l], b_glu[l], w_branch_m[l],
                                w_branch_s[l], b_merge_gate[l], w_o[l])
        h2 = modulate(rmsnorm(x, g_norm2[l]), sh2, sc2)
        x = x + gt2 * moe(h2.reshape(-1, D), w_router[l], b_router[l], w_e_in[l], b_e_in[l],
                          w_e_out[l], b_e_out[l]).reshape(x.shape)
        if not last:
            ctx = ctx + mod_c[2] * mixer_out(proj_c, hm_c, ys_c, g_mh[l], w_glu[l], b_glu[l], w_branch_m[l],
                                             w_branch_s[l], b_merge_gate[l], w_o[l])
            h2c = modulate(rmsnorm(ctx, g_norm2[l]), mod_c[3], mod_c[4])
            ctx = ctx + mod_c[5] * moe(h2c.reshape(-1, D), w_router[l], b_router[l], w_e_in[l], b_e_in[l],
                                       w_e_out[l], b_e_out[l]).reshape(ctx.shape)
    return rmsnorm(x, g_final)
```

```python
import numpy as np
import concourse.bass as bass
import concourse.mybir as mybir
from concourse.bass_utils import run_bass_kernel_spmd

F32 = mybir.dt.float32
BF16 = mybir.dt.bfloat16
ALU = mybir.AluOpType
AF = mybir.ActivationFunctionType
AX = mybir.AxisListType

ENGS = ["tensor", "vector", "scalar", "gpsimd", "sync"]
EPOCH = 16000


class V:
    __slots__ = ("ap", "name", "key")

    def __init__(self, ap, name, key=None):
        self.ap = ap
        self.name = name
        self.key = key

    def m(self, fn):
        return V(fn(self.ap), self.name, self.key)


class _TK:
    def __init__(self, t, key):
        self.t = t
        self.key = key

    def __getitem__(self, idx):
        return V(self.t.h[idx], self.t.name, self.key)


class T:
    def __init__(self, h, name):
        self.h = h
        self.name = name

    def __getitem__(self, idx):
        return V(self.h[idx], self.name, None)

    def k(self, key):
        return _TK(self, key)


class Prog:
    def __init__(self, nc, stack):
        self.nc = nc
        self.stack = stack
        self.semstack = stack
        self.items = {e: [] for e in ENGS}
        self.cnt = {e: 0 for e in ENGS}
        self.esems = {e: [] for e in ENGS}
        self.state = {}
        self.waited = {e: {} for e in ENGS}
        self.dsem = {}
        self.ntiles = 0
        self.nwaits = 0
        self.persist = set()

    def sb(self, name, shape, dt):
        h = self.stack.enter_context(self.nc.sbuf_tensor(name, list(shape), dt))
        return T(h, name)

    def ps(self, name, shape, dt=F32):
        h = self.stack.enter_context(self.nc.psum_tensor(name, list(shape), dt))
        return T(h, name)

    def dram(self, name, shape, dt, kind="Internal"):
        h = self.nc.dram_tensor(name, list(shape), dt, kind=kind)
        return T(h.ap() if hasattr(h, "ap") else h, name)

    def _esem(self, e, ep):
        while len(self.esems[e]) <= ep:
            s = self.semstack.enter_context(self.nc.semaphore("s_%s_%d" % (e, len(self.esems[e]))))
            self.esems[e].append(s)
        return self.esems[e][ep]

    def _dsem(self, key):
        if key not in self.dsem:
            if not hasattr(self, "free_d"):
                self.free_d = {"sw": [], "hw": []}
            fd = self.free_d[key[1]]
            if fd:
                fd.sort(key=lambda x: x[1])
                self.dsem[key] = fd.pop(0)
            else:
                self.ndsem = getattr(self, "ndsem", 0) + 1
                s = self.semstack.enter_context(self.nc.semaphore("d_%d" % self.ndsem))
                self.dsem[key] = [s, 0]
        return self.dsem[key]

    def _states(self, name, key):
        d = self.state.setdefault(name, {})
        if key is None:
            if None not in d:
                d[None] = [None, {}]
            return list(d.values())
        if key not in d:
            d[key] = [None, {}]
        out = [d[key]]
        if None in d:
            out.append(d[None])
        return out

    def _wait(self, eng, dep):
        if dep is None:
            return
        if dep[0] == "E":
            _, e2, idx = dep
            if e2 == eng and eng == "tensor":
                return
            ep = (idx - 1) // EPOCH
            val = idx - ep * EPOCH
            sem = self._esem(e2, ep)
            sk = ("E", e2, ep)
        else:
            _, dk = dep
            sem, val = self.dsem[dk]
            sk = ("D", id(sem))
        w = self.waited[eng]
        if w.get(sk, 0) >= val:
            return
        w[sk] = val
        self.nwaits += 1
        self.items[eng].append(lambda E, sem=sem, val=val: E.wait_ge(sem, val))

    def _deps(self, eng, reads, writes):
        for v in reads:
            for st in self._states(v.name, v.key):
                self._wait(eng, st[0])
        for v in writes:
            for st in self._states(v.name, v.key):
                self._wait(eng, st[0])
                for r in st[1].values():
                    self._wait(eng, r)

    def _record(self, dep, reads, writes):
        for v in reads:
            d = self.state.setdefault(v.name, {})
            if v.key not in d:
                d[v.key] = [None, {}]
            d[v.key][1][dep[:2]] = dep
        for v in writes:
            d = self.state.setdefault(v.name, {})
            if v.key is None:
                for k in list(d.keys()):
                    d[k] = [dep, {}]
                d[None] = [dep, {}]
            else:
                d[v.key] = [dep, {}]

    def op(self, eng, fn, reads, writes):
        reads = [r for r in reads if isinstance(r, V)]
        writes = [w for w in writes if isinstance(w, V)]
        self._deps(eng, reads, writes)
        self.cnt[eng] += 1
        idx = self.cnt[eng]
        ep = (idx - 1) // EPOCH
        sem = self._esem(eng, ep)
        self.items[eng].append(lambda E, fn=fn, sem=sem: fn(E).then_inc(sem, 1))
        self._record(("E", eng, idx), reads, writes)

    def dma(self, eng, out, in_, semkey=None, **kw):
        if semkey is None:
            semkey = out.name if not out.name.startswith("dr_") else in_.name
        semkey = (semkey, "sw" if eng == "gpsimd" else "hw")
        self._deps(eng, [in_], [out])
        ds = self._dsem(semkey)
        ds[1] += 16
        assert ds[1] < 60000, semkey
        sem = ds[0]
        o, i = out.ap, in_.ap
        self.items[eng].append(lambda E, o=o, i=i, sem=sem, kw=kw: E.dma_start(out=o, in_=i, **kw).then_inc(sem, 16))
        self._record(("D", semkey), [in_], [out])

    def mm(self, out, lhsT, rhs, start=True, stop=True):
        self.op("tensor", lambda E: E.matmul(out.ap, lhsT.ap, rhs.ap, start=start, stop=stop),
                [lhsT, rhs] + ([] if start else [out]), [out])

    def tr(self, out, in_, ident):
        self.op("tensor", lambda E: E.transpose(out.ap, in_.ap, ident.ap), [in_, ident], [out])

    def act(self, out, in_, func, bias=0.0, scale=1.0, accum=None, eng="scalar"):
        b = bias.ap if isinstance(bias, V) else bias
        s = scale.ap if isinstance(scale, V) else scale
        kw = {}
        if accum is not None:
            kw["accum_out"] = accum.ap
        self.op("scalar", lambda E: E.activation(out.ap, in_.ap, func, bias=b, scale=s, **kw),
                [in_, bias, scale], [out] + ([accum] if accum is not None else []))

    def tt(self, eng, out, a, b, op):
        self.op(eng, lambda E: E.tensor_tensor(out.ap, a.ap, b.ap, op), [a, b], [out])

    def ts(self, eng, out, a, s1, op0, s2=None, op1=None, accum=None):
        x1 = s1.ap if isinstance(s1, V) else s1
        x2 = s2.ap if isinstance(s2, V) else s2
        kw = {}
        if op1 is not None:
            kw["op1"] = op1
        if accum is not None:
            kw["accum_out"] = accum.ap
        self.op(eng, lambda E: E.tensor_scalar(out.ap, a.ap, x1, x2, op0, **kw), [a, s1, s2],
                [out] + ([accum] if accum is not None else []))

    def stt(self, eng, out, a, s, b, op0, op1):
        x = s.ap if isinstance(s, V) else s
        self.op(eng, lambda E: E.scalar_tensor_tensor(out.ap, a.ap, x, b.ap, op0, op1), [a, s, b], [out])

    def cp(self, eng, out, in_):
        if eng == "scalar":
            self.op(eng, lambda E: E.copy(out.ap, in_.ap), [in_], [out])
        else:
            self.op(eng, lambda E: E.tensor_copy(out.ap, in_.ap), [in_], [out])

    def memset(self, eng, out, val):
        self.op(eng, lambda E: E.memset(out.ap, val), [], [out])

    def finish(self, outs):
        for v in outs:
            for st in self._states(v.name, v.key):
                self._wait("sync", st[0])
        for e in ENGS:
            if self.cnt[e] > 0:
                self._wait("sync", ("E", e, self.cnt[e]))
        for dk in list(self.dsem.keys()):
            self._wait("sync", ("D", dk))

    def barrier(self):
        for e in ENGS:
            for e2 in ENGS:
                if self.cnt[e2] > 0:
                    self._wait(e, ("E", e2, self.cnt[e2]))
            for dk in list(self.dsem.keys()):
                if dk in self.persist:
                    continue
                self._wait(e, ("D", dk))
        self.state = {}
        if not hasattr(self, "free_d"):
            self.free_d = {"sw": [], "hw": []}
        for k, v in self.dsem.items():
            if k in self.persist:
                continue
            self.free_d[k[1]].append(v)
        self.dsem = {k: v for k, v in self.dsem.items() if k in self.persist}

    def flush(self):
        self.build()
        self.items = {e: [] for e in ENGS}

    def build(self):
        nc = self.nc
        with nc.Block() as block:
            for e in ENGS:
                items = self.items[e]
                if not items:
                    continue

                def body(E, items=items):
                    for it in items:
                        it(E)
                getattr(block, e)(body)
from contextlib import ExitStack
import ml_dtypes

D = 1024
L = 4096
LC = 256
LT = L + LC
NE = 32
OFF_QK, OFF_V, OFF_IF, OFF_U, OFF_O, OFF_G, IN_COLS = 0, 1024, 2048, 2080, 2592, 3616, 5664
EPS = 1e-6


def host_prep(inp, b):
    f = np.float32
    A = lambda a: np.ascontiguousarray(a, dtype=f)
    m = {}
    m["dr_x"] = A(inp["x"][b])
    m["dr_ctx"] = A(inp["ctx"][b])
    cc = np.stack([inp["c"][b].reshape(8, 128).T, inp["c_ctx"].reshape(8, 128).T], axis=-1)
    m["dr_cc"] = A(cc)
    m["dr_w_ada"] = A(inp["w_ada"][0])
    m["dr_b_ada"] = A(inp["b_ada"][0].reshape(48, 128).T)
    m["dr_g1"] = A(inp["g_norm1"][0].reshape(8, 128).T)
    m["dr_g2"] = A(inp["g_norm2"][0].reshape(8, 128).T)
    m["dr_w_in"] = A(inp["w_in"][0])
    m["dr_wconv"] = A(inp["w_conv_qk"][0].reshape(9, 8, 128).transpose(2, 1, 0))
    m["dr_bif"] = A(np.broadcast_to(inp["b_ifgate"][0].reshape(1, 32), (128, 32)))
    m["dr_gmh"] = A(np.broadcast_to(inp["g_mh"][0].reshape(1, 1024), (128, 1024)))
    m["dr_w_a"] = A(inp["w_branch_m"][0])
    row = np.zeros((2, 3, 128, 2048), f)
    col = np.zeros((2, 3, 128, 16), f)
    for d in range(2):
        ldt = np.repeat(inp["s5_log_dt"][0, d], 64)
        for i, arr in enumerate([inp["s5_a_re"][0, d].reshape(-1), inp["s5_a_im"][0, d].reshape(-1), ldt]):
            row[d, i] = np.broadcast_to(arr.reshape(1, 2048), (128, 2048))
            col[d, i] = arr.reshape(16, 128).T
    m["dr_s5row"] = row
    m["dr_s5col"] = col
    bbd = np.zeros((2, 2, 128, 4, 512), f)
    cbd = np.zeros((2, 2, 128, 16, 128), f)
    for d in range(2):
        for ri, (bsrc, csrc) in enumerate([(inp["s5_b_re"], inp["s5_c_re"]), (inp["s5_b_im"], inp["s5_c_im"])]):
            for g in range(32):
                j, gl = divmod(g, 8)
                bbd[d, ri, gl * 16:(gl + 1) * 16, j, gl * 64:(gl + 1) * 64] = bsrc[0, d, g].T
                q, g2 = divmod(g, 2)
                cbd[d, ri, g2 * 64:(g2 + 1) * 64, q, (q % 4) * 32 + g2 * 16:(q % 4) * 32 + g2 * 16 + 16] = csrc[0, d, g].T
    m["dr_bbd"] = bbd.reshape(2, 2, 128, 2048)
    m["dr_cbd"] = cbd.reshape(2, 2, 128, 2048)
    m["dr_s5d"] = A(inp["s5_d"][0].reshape(4, 128).T)
    m["dr_w_glu"] = A(inp["w_glu"][0])
    m["dr_b_glu"] = A(inp["b_glu"][0].reshape(4, 128).T)
    m["dr_w_b"] = A(inp["w_branch_s"][0])
    m["dr_b_gate"] = A(inp["b_merge_gate"][0].reshape(16, 128).T)
    m["dr_w_o"] = A(inp["w_o"][0])
    m["dr_w_r"] = A(inp["w_router"][0])
    m["dr_b_r"] = A(np.broadcast_to(inp["b_router"][0].reshape(1, 32), (128, 32)))
    m["dr_w_ein"] = A(inp["w_e_in"][0])
    m["dr_b_ein"] = A(inp["b_e_in"][0].reshape(32, 16, 128).transpose(2, 0, 1).reshape(128, 512))
    m["dr_w_eout"] = A(inp["w_e_out"][0])
    m["dr_b_eout"] = A(inp["b_e_out"][0])
    m["dr_gfin"] = A(np.broadcast_to(inp["g_final"].reshape(1, 1024), (128, 1024)))
    cst = np.zeros((128, 7, 128), f)
    ii = np.arange(128)
    cst[:, 0] = np.eye(128)
    cst[:, 1] = (ii[:, None] <= ii[None, :])
    cst[:, 2] = (ii[:, None] >= ii[None, :])
    cst[:, 3] = 1.0
    cst[:, 4] = ii[None, :]
    cst[:, 5] = 127 - ii[None, :]
    cst[:, 6, 0] = ii
    cst[:, 6, 1] = 127 - ii
    m["dr_cst"] = cst
    return m

class Ctx:
    pass


def declare_io(P, dbg):
    C = Ctx()
    def din(name, shape, dt=F32):
        return P.dram("dr_" + name, shape, dt, kind="ExternalInput")
    C.x = din("x", [L, D]); C.ctx = din("ctx", [LC, D]); C.cc = din("cc", [128, 8, 2])
    C.w_ada = din("w_ada", [D, 6144]); C.b_ada = din("b_ada", [128, 48])
    C.g1 = din("g1", [128, 8]); C.g2 = din("g2", [128, 8]); C.w_in = din("w_in", [D, IN_COLS])
    C.wconv = din("wconv", [128, 8, 9]); C.bif = din("bif", [128, 32]); C.gmh = din("gmh", [128, 1024])
    C.w_a = din("w_a", [D, D]); C.s5row = din("s5row", [2, 3, 128, 2048]); C.s5col = din("s5col", [2, 3, 128, 16])
    C.bbd = din("bbd", [2, 2, 128, 2048]); C.cbd = din("cbd", [2, 2, 128, 2048]); C.s5d = din("s5d", [128, 4])
    C.w_glu = din("w_glu", [512, 512]); C.b_glu = din("b_glu", [128, 4]); C.w_b = din("w_b", [512, D])
    C.b_gate = din("b_gate", [128, 16]); C.w_o = din("w_o", [D, D]); C.w_r = din("w_r", [D, 32])
    C.b_r = din("b_r", [128, 32]); C.w_ein = din("w_ein", [NE, D, 2048]); C.b_ein = din("b_ein", [128, 512])
    C.w_eout = din("w_eout", [NE, D, D]); C.b_eout = din("b_eout", [NE, D]); C.gfin = din("gfin", [128, 1024])
    C.cst = din("cst", [128, 7, 128])
    C.out = P.dram("dr_out", [L, D], F32, kind="ExternalOutput")
    def scr(name, shape, dt):
        kind = "ExternalOutput" if name in dbg else "Internal"
        return P.dram("dr_s_" + name, shape, dt, kind=kind)
    C.qkpre = scr("qkpre", [1024, LT], BF16)
    C.v = scr("v", [LT, 1024], BF16)
    C.gl = scr("gl", [LT, 32], F32)
    C.uT = scr("uT", [512, LT], BF16)
    C.GT = scr("GT", [2048, L], BF16)
    C.og = scr("og", [L, 1024], BF16)
    C.qT = scr("qT", [512, LT], BF16)
    C.kT = scr("kT", [512, LT], BF16)
    C.ktok = scr("ktok", [LT, 512], BF16)
    C.hd = [scr("hf", [L, 1024], F32), scr("hb", [L, 1024], F32)]
    C.hmT = scr("hmT", [1024, L], BF16)
    C.yT = [scr("yTf", [512, L], F32), scr("yTb", [512, L], F32)]
    C.x1 = scr("x1", [L, D], F32)
    C.h2T = scr("h2T", [1024, L], BF16)
    C.gates = scr("gates", [L, 32], F32)
    C.gatesT = scr("gatesT", [32, L], F32)
    C.modT = scr("modT", [128, 96], F32)
    C.weinb = scr("weinb", [NE, D, 2048], BF16)
    C.weoutb = scr("weoutb", [NE, D, D], BF16)
    return C


def bc(v, shape, axis_pat):
    return v.m(lambda a: a.rearrange(axis_pat, o=1).to_broadcast(shape))


def phase0(P, C):
    K = Ctx()
    K.cst = P.sb("cst", [128, 7, 128], F32)
    P.dma("sync", K.cst[:], C.cst[:])
    K.ident = K.cst[:, 0, :]; K.triF = K.cst[:, 1, :]; K.triB = K.cst[:, 2, :]; K.ones = K.cst[:, 3, :]
    K.S1 = P.sb("S1", [128, 8], F32); K.SH1 = P.sb("SH1", [128, 8], F32)
    K.S1c = P.sb("S1c", [128, 8], F32); K.SH1c = P.sb("SH1c", [128, 8], F32)
    K.S2 = P.sb("S2", [128, 8], F32); K.SH2 = P.sb("SH2", [128, 8], F32)
    K.gt1bc = P.sb("gt1bc", [128, 1024], F32); K.gt2bc = P.sb("gt2bc", [128, 1024], F32)
    with ExitStack() as ph:
        P.stack = ph
        cc = P.sb("cc", [128, 8, 2], F32); sc = P.sb("sc", [128, 8, 2], F32)
        bada = P.sb("bada", [128, 48], F32); g1 = P.sb("g1", [128, 8], F32); g2 = P.sb("g2", [128, 8], F32)
        modT = P.sb("modT", [128, 48, 2], F32); tmp8 = P.sb("tmp8", [128, 8], F32)
        gt = P.sb("gt", [128, 16], F32)
        wa = [P.sb("wa%d" % i, [128, 8, 512], F32) for i in range(2)]
        diag = [P.sb("diag%d" % i, [128, 128], F32) for i in range(2)]
        pm = P.ps("pm", [128, 512]); pg = P.ps("pg", [128, 1024])
        P.dma("sync", cc[:], C.cc[:]); P.dma("sync", bada[:], C.b_ada[:])
        P.dma("sync", g1[:], C.g1[:]); P.dma("sync", g2[:], C.g2[:])
        P.act(sc[:], cc[:], AF.Silu)
        wv = C.w_ada[:].m(lambda a: a.rearrange("(k p) n -> p k n", p=128))
        for i in range(12):
            w = wa[i % 2]
            P.dma("sync" if i % 2 == 0 else "gpsimd", w[:], V(wv.ap[:, :, i * 512:(i + 1) * 512], wv.name))
            for mI in range(4):
                nn = 4 * i + mI
                for k in range(8):
                    P.mm(pm[:, nn * 2:nn * 2 + 2], w[:, k, mI * 128:(mI + 1) * 128], sc[:, k, :], start=(k == 0), stop=(k == 7))
        P.tt("vector", modT[:], pm[:, 0:96].m(lambda a: a.rearrange("p (n t) -> p n t", t=2)),
             bada[:].m(lambda a: a.rearrange("p (n o) -> p n o", o=1).to_broadcast([128, 48, 2])), ALU.add)
        P.cp("vector", K.SH1[:], modT[:, 0:8, 0]); P.cp("vector", K.SH1c[:], modT[:, 0:8, 1])
        P.cp("vector", K.SH2[:], modT[:, 24:32, 0])
        P.ts("vector", tmp8[:], modT[:, 8:16, 0], 1.0, ALU.add); P.tt("vector", K.S1[:], tmp8[:], g1[:], ALU.mult)
        P.ts("vector", tmp8[:], modT[:, 8:16, 1], 1.0, ALU.add); P.tt("vector", K.S1c[:], tmp8[:], g1[:], ALU.mult)
        P.ts("vector", tmp8[:], modT[:, 32:40, 0], 1.0, ALU.add); P.tt("vector", K.S2[:], tmp8[:], g2[:], ALU.mult)
        P.cp("vector", gt[:, 0:8], modT[:, 16:24, 0]); P.cp("vector", gt[:, 8:16], modT[:, 40:48, 0])
        for j in range(16):
            dg = diag[j % 2]
            P.ts("vector", dg[:], K.ident, gt[:, j:j + 1], ALU.mult)
            P.mm(pg[:, (j % 8) * 128:(j % 8 + 1) * 128], K.ones, dg[:])
            if j == 7:
                P.cp("vector", K.gt1bc[:], pg[:])
            if j == 15:
                P.cp("vector", K.gt2bc[:], pg[:])
        P.dma("sync", C.modT[:], modT[:].m(lambda a: a.rearrange("p n t -> p (n t)")))
        P.barrier(); P.flush()
    return K


def norm_T(P, K, xt, S, SH, hT_dst, W, need_f32=None):
    P.act(W["junk"][:], xt, AF.Square, accum=W["ss"][:])
    P.ts("vector", W["ms"][:], W["ss"][:], 1.0 / D, ALU.mult, EPS, ALU.add)
    P.act(W["sq"][:], W["ms"][:], AF.Sqrt)
    P.op("vector", lambda E: E.reciprocal(W["rstd"].h[:], W["sq"].h[:]), [W["sq"][:]], [W["rstd"][:]])
    P.act(W["xn"][:], xt, AF.Identity, scale=W["rstd"][:])
    for j in range(8):
        P.tr(W["psT"][:, j * 128:(j + 1) * 128], W["xn"][:, j * 128:(j + 1) * 128], K.ident)
    for j in range(8):
        pj = W["psT"][:, j * 128:(j + 1) * 128]
        sj = V(S.ap[:, j:j + 1], S.name, S.key); bj = V(SH.ap[:, j:j + 1], SH.name, SH.key)
        if need_f32 is not None:
            nf = V(need_f32.ap[:, j, :], need_f32.name, need_f32.key)
            P.act(nf, pj, AF.Identity, bias=bj, scale=sj)
        else:
            P.act(V(hT_dst.ap[:, j, :], hT_dst.name, hT_dst.key), pj, AF.Identity, bias=bj, scale=sj)
    if need_f32 is not None:
        P.cp("vector", hT_dst, need_f32)


def norm_work(P, sfx=""):
    W = {}
    W["junk"] = P.sb("nw_junk" + sfx, [128, 1024], BF16)
    W["xn"] = P.sb("nw_xn" + sfx, [128, 1024], F32)
    W["tm"] = P.sb("nw_tm" + sfx, [128, 8, 128], F32)
    for n in ["ss", "ms", "sq", "rstd"]:
        W[n] = P.sb("nw_" + n + sfx, [128, 1], F32)
    W["psT"] = P.ps("nw_psT" + sfx, [128, 1024])
    return W


def phase1(P, C, K):
    with ExitStack() as ph:
        P.stack = ph
        win = P.sb("win", [128, 8, IN_COLS], BF16)
        for k in range(8):
            P.dma("gpsimd", win[:, k, :], C.w_in[k * 128:(k + 1) * 128, :])
        rr = lambda a: a.rearrange("(k p) n -> p k n", p=128)
        for e in range(NE):
            for kk in range(4):
                P.dma("gpsimd", V(rr(C.weinb.h[e, kk * 256:(kk + 1) * 256, :]), C.weinb.name),
                      V(rr(C.w_ein.h[e, kk * 256:(kk + 1) * 256, :]), C.w_ein.name), semkey="cvt")
            for kk in range(2):
                P.dma("gpsimd", V(rr(C.weoutb.h[e, kk * 512:(kk + 1) * 512, :]), C.weoutb.name),
                      V(rr(C.w_eout.h[e, kk * 512:(kk + 1) * 512, :]), C.w_eout.name), semkey="cvt")
            P.cvt_counts = getattr(P, "cvt_counts", []) + [P.dsem[("cvt", "sw")][1]]
        P.persist.add(("cvt", "sw"))
        bif = P.sb("bif", [128, 32], F32); P.dma("sync", bif[:], C.bif[:])
        bgate = P.sb("bgate", [128, 16], F32); P.dma("sync", bgate[:], C.b_gate[:])
        W = norm_work(P)
        xt = [P.sb("xt%d" % i, [128, 1024], F32) for i in range(2)]
        hT = [P.sb("hT%d" % i, [128, 8, 512], BF16) for i in range(2)]
        pA = [P.ps("pA%d" % i, [128, 512]) for i in range(4)]
        st_qk = [P.sb("st_qk%d" % i, [128, 8, 512], BF16) for i in range(2)]
        st_u = [P.sb("st_u%d" % i, [128, 4, 512], BF16) for i in range(2)]
        st_G = [P.sb("st_G%d" % i, [128, 8, 512], BF16) for i in range(2)]
        st_v = [P.sb("st_v%d" % i, [128, 1024], BF16) for i in range(2)]
        st_o = [P.sb("st_o%d" % i, [128, 1024], BF16) for i in range(2)]
        st_g = [P.sb("st_g%d" % i, [128, 4, 32], F32) for i in range(2)]
        gw = {n: P.sb("gw_" + n, [128, 32], F32) for n in ["g", "e", "sp"]}
        tiles = [(True, 0, 256, 0)] + [(False, i * 512, 512, LC + i * 512) for i in range(8)]
        pai = 0
        for ti, (isc, off, nt, lto) in enumerate(tiles):
            par = ti % 2
            nsub = nt // 128
            src = C.ctx if isc else C.x
            h = hT[par]
            for s in range(nsub):
                x_ = xt[(ti * 4 + s) % 2]
                P.dma("sync", x_[:], src[off + s * 128: off + (s + 1) * 128, :])
                norm_T(P, K, x_[:], K.S1c[:] if isc else K.S1[:], K.SH1c[:] if isc else K.SH1[:],
                       h[:, :, s * 128:(s + 1) * 128], W)
            def fm(col0, ncol, stage, func, bias=None, evac="scalar"):
                nonlocal pai
                for m in range(ncol):
                    pb = pA[pai % 4]; pai += 1
                    for k in range(8):
                        P.mm(pb[:, 0:nt], win[:, k, col0 + m * 128: col0 + (m + 1) * 128], h[:, k, 0:nt], start=(k == 0), stop=(k == 7))
                    if func is None:
                        if m % 2 == 0:
                            P.cp("scalar", stage[:, m, 0:nt], pb[:, 0:nt])
                        else:
                            P.cp("vector", stage[:, m, 0:nt], pb[:, 0:nt])
                    else:
                        P.act(stage[:, m, 0:nt], pb[:, 0:nt], func, bias=V(bias.ap[:, m:m + 1], bias.name) if isinstance(bias, V) else bias[:, m:m + 1])
            fm(OFF_QK, 8, st_qk[par], None)
            P.dma("sync", V(C.qkpre.h[:, lto:lto + nt].rearrange("(j p) t -> p j t", p=128), C.qkpre.name), st_qk[par][:, :, 0:nt])
            fm(OFF_U, 4, st_u[par], None)
            P.dma("sync", V(C.uT.h[:, lto:lto + nt].rearrange("(j p) t -> p j t", p=128), C.uT.name), st_u[par][:, :, 0:nt])
            if not isc:
                for gh in range(2):
                    fm(OFF_G + gh * 1024, 8, st_G[gh], AF.Sigmoid, bias=V(bgate.h[:, gh * 8:(gh + 1) * 8], bgate.name))
                    P.dma("sync", V(C.GT.h[gh * 1024:(gh + 1) * 1024, off:off + nt].rearrange("(j p) t -> p j t", p=128), C.GT.name), st_G[gh][:, :, 0:nt])
            for s in range(nsub):
                for hh in range(2):
                    pb = pA[pai % 4]; pai += 1
                    for k in range(8):
                        P.mm(pb[:], h[:, k, s * 128:(s + 1) * 128], win[:, k, OFF_V + hh * 512: OFF_V + (hh + 1) * 512], start=(k == 0), stop=(k == 7))
                    P.cp("vector", st_v[s % 2][:, hh * 512:(hh + 1) * 512], pb[:])
                P.dma("sync", C.v[lto + s * 128: lto + (s + 1) * 128, :], st_v[s % 2][:])
                pb = pA[pai % 4]; pai += 1
                for k in range(8):
                    P.mm(pb[:, 0:32], h[:, k, s * 128:(s + 1) * 128], win[:, k, OFF_IF:OFF_IF + 32], start=(k == 0), stop=(k == 7))
                g = gw["g"]
                P.tt("vector", g[:], pb[:, 0:32], bif[:], ALU.add)
                gv = g[:].m(lambda a: a.rearrange("p (d i h) -> p d i h", d=2, i=2))
                sg = st_g[par][:, s, :].m(lambda a: a.rearrange("p (i d h) -> p i d h", i=2, d=2))
                P.cp("vector", V(sg.ap[:, 0], sg.name), V(gv.ap[:, :, 0, :], gv.name))
                P.act(gw["e"][:], g[:], AF.Exp, scale=-1.0)
                P.act(gw["sp"][:], gw["e"][:], AF.Ln, bias=1.0)
                spv = gw["sp"][:].m(lambda a: a.rearrange("p (d i h) -> p d i h", d=2, i=2))
                P.ts("vector", V(sg.ap[:, 1], sg.name), V(spv.ap[:, :, 1, :], spv.name), -1.0, ALU.mult)
                if not isc:
                    for hh in range(2):
                        pb = pA[pai % 4]; pai += 1
                        for k in range(8):
                            P.mm(pb[:], h[:, k, s * 128:(s + 1) * 128], win[:, k, OFF_O + hh * 512: OFF_O + (hh + 1) * 512], start=(k == 0), stop=(k == 7))
                        P.act(st_o[s % 2][:, hh * 512:(hh + 1) * 512], pb[:], AF.Sigmoid)
                    P.dma("sync", C.og[off + s * 128: off + (s + 1) * 128, :], st_o[s % 2][:])
            P.dma("sync", V(C.gl.h[lto:lto + nt, :].rearrange("(s p) c -> p s c", p=128), C.gl.name), st_g[par][:, 0:nsub, :])
        P.barrier(); P.flush()

def phase2(P, C, K):
    with ExitStack() as ph:
        P.stack = ph
        wc = P.sb("wc", [128, 8, 9], F32); P.dma("sync", wc[:], C.wconv[:])
        pre = [P.sb("pre%d" % i, [128, LT], BF16) for i in range(2)]
        acc = [P.sb("cacc%d" % i, [128, LT], F32) for i in range(2)]
        sl = [P.sb("csl%d" % i, [128, LT], F32) for i in range(2)]
        ob = [P.sb("cob%d" % i, [128, LT], BF16) for i in range(2)]
        kst = [P.sb("kst%d" % i, [128, 4, 128], BF16) for i in range(2)]
        pT = [P.ps("pT%d" % i, [128, 512]) for i in range(2)]
        for j in range(8):
            e = "vector"
            p_, a_, s_, o_ = pre[j % 2], acc[j % 2], sl[j % 2], ob[j % 2]
            P.dma("sync", p_[:], C.qkpre[j * 128:(j + 1) * 128, :])
            def w(t):
                return wc[:, j, t:t + 1]
            P.ts(e, a_[:, 0:LC], p_[:, 0:LC], w(4), ALU.mult)
            P.stt(e, a_[:, 1:LC], p_[:, 0:LC - 1], w(3), a_[:, 1:LC], ALU.mult, ALU.add)
            P.stt(e, a_[:, 0:LC - 1], p_[:, 1:LC], w(5), a_[:, 0:LC - 1], ALU.mult, ALU.add)
            pv = p_[:, LC:LT].m(lambda a: a.rearrange("p (r w) -> p r w", w=64))
            av = a_[:, LC:LT].m(lambda a: a.rearrange("p (r w) -> p r w", w=64))
            P.ts(e, a_[:, LC:LT], p_[:, LC:LT], w(4), ALU.mult)
            for dr in range(3):
                for dw in range(3):
                    if dr == 1 and dw == 1:
                        continue
                    ro = slice(max(0, 1 - dr), 64 - max(0, dr - 1)); ri = slice(max(0, dr - 1), 64 - max(0, 1 - dr))
                    co = slice(max(0, 1 - dw), 64 - max(0, dw - 1)); ci = slice(max(0, dw - 1), 64 - max(0, 1 - dw))
                    o_v = V(av.ap[:, ro, co], av.name); i_v = V(pv.ap[:, ri, ci], pv.name)
                    P.stt(e, o_v, i_v, w(dr * 3 + dw), o_v, ALU.mult, ALU.add)
            P.act(s_[:], a_[:], AF.Silu)
            if j < 4:
                if j % 2 == 0:
                    P.act(o_[:], s_[:], AF.Identity, scale=0.125)
                else:
                    P.ts("vector", o_[:], s_[:], 0.125, ALU.mult)
                P.dma("sync", C.qT[j * 128:(j + 1) * 128, :], o_[:])
            else:
                jj = j - 4
                P.cp("scalar" if j % 2 == 0 else "vector", o_[:], s_[:])
                P.dma("sync", C.kT[jj * 128:(jj + 1) * 128, :], o_[:])
                for g4 in range(9):
                    nb = min(4, 34 - g4 * 4)
                    pt = pT[g4 % 2]; ks = kst[g4 % 2]
                    for bI in range(nb):
                        blk = g4 * 4 + bI
                        P.tr(pt[:, bI * 128:(bI + 1) * 128], s_[:, blk * 128:(blk + 1) * 128], K.ident)
                    P.cp("scalar", ks[:, 0:nb, :], pt[:, 0:nb * 128].m(lambda a: a.rearrange("p (b c) -> p b c", c=128)))
                    P.dma("sync", V(C.ktok.h[g4 * 512: g4 * 512 + nb * 128, jj * 128:(jj + 1) * 128].rearrange("(b p) c -> p b c", p=128), C.ktok.name), ks[:, 0:nb, :])
        P.barrier(); P.flush()


HGRP = [(0, 3), (3, 3), (6, 2)]


def hoff(h):
    return (h // 3) * 512 + (h % 3) * 170


def chunk_orders():
    fwd = list(range(34))
    bwd = [1, 0] + list(range(33, 1, -1))
    return fwd, bwd


def phase3(P, C, K):
    with ExitStack() as ph:
        P.stack = ph
        Cn = P.sb("Cn", [128, 2, 8, 129], F32); Cnb = P.sb("Cnb", [128, 2, 8, 129], BF16)
        P.memset("vector", Cn[:], 0.0); P.memset("vector", Cnb[:], 0.0)
        NB = 2
        v1 = [[P.sb("v1_%d%d" % (d, i), [128, 8, 129], BF16) for i in range(NB)] for d in range(2)]
        glt = [[P.sb("glt_%d%d" % (d, i), [128, 32], F32) for i in range(NB)] for d in range(2)]
        kt = [[P.sb("kt_%d%d" % (d, i), [128, 9, 64], BF16) for i in range(NB)] for d in range(2)]
        qTt = [[P.sb("qTt_%d%d" % (d, i), [128, 8, 128], BF16) for i in range(NB)] for d in range(2)]
        kTt = [[P.sb("kTt_%d%d" % (d, i), [128, 8, 128], BF16) for i in range(NB)] for d in range(2)]
        for d in range(2):
            for i in range(NB):
                P.memset("vector", v1[d][i][:], 1.0)
                P.memset("vector", kt[d][i][:], 0.0)
                P.memset("vector", qTt[d][i][:], 0.0)
                P.memset("vector", kTt[d][i][:], 0.0)
        vw = [P.sb("vw%d" % d, [128, 8, 129], BF16) for d in range(2)]
        PT = [P.sb("PTs%d" % i, [128, 128], BF16) for i in range(4)]
        hd = [P.sb("hd%d" % d, [128, 8, 128], F32) for d in range(2)]
        sm = {n: [P.sb("sm_%s%d" % (n, d), [128, 8], F32) for d in range(2)] for n in ["nb", "t1", "ek", "t2", "w", "dec", "ad", "mx", "r"]}
        pG = P.ps("pG", [128, 512]); pPs = [P.ps("pP%d" % i, [128, 512]) for i in range(2)]
        pAccs = [P.ps("pAcc%d" % i, [128, 512]) for i in range(2)]; pUs = [P.ps("pU%d" % i, [128, 512]) for i in range(2)]
        gi_ctr = [0]; gu_ctr = [0]
        fwd, bwd = chunk_orders()
        pti = 0
        its3 = [(step, d) for step in range(34) for d in range(2)]

        def front(n):
            step, d = its3[n]
            g = (fwd, bwd)[d][step]
            lat = g >= 2
            bi = step % NB
            r0 = g * 128
            tri = K.triF if d == 0 else K.triB
            V1, GL, KT, QT, KTT = v1[d][bi], glt[d][bi], kt[d][bi], qTt[d][bi], kTt[d][bi]
            S = {nm_: sm[nm_][d] for nm_ in sm}
            g = (fwd, bwd)[d][step]
            lat = g >= 2
            bi = step % NB
            r0 = g * 128
            tri = K.triF if d == 0 else K.triB
            V1, GL, KT, QT, KTT = v1[d][bi], glt[d][bi], kt[d][bi], qTt[d][bi], kTt[d][bi]
            P.dma("sync", V1[:, :, 0:128], V(C.v.h[r0:r0 + 128, :].rearrange("p (h c) -> p h c", h=8), C.v.name))
            P.dma("sync", GL[:], C.gl[r0:r0 + 128, :])
            P.dma("sync", KT[:, 0:8, :], V(C.ktok.h[r0:r0 + 128, :].rearrange("p (h c) -> p h c", h=8), C.ktok.name))
            if lat:
                P.dma("sync", QT[0:64, :, :], V(C.qT.h[:, r0:r0 + 128].rearrange("(h k) t -> k h t", k=64), C.qT.name))
                P.dma("sync", KTT[0:64, :, :], V(C.kT.h[:, r0:r0 + 128].rearrange("(h k) t -> k h t", k=64), C.kT.name))
            li = GL[:, 8 * d: 8 * d + 8]; lf = GL[:, 16 + 8 * d: 16 + 8 * d + 8]
            pg = pG[:, 0:8]; pe = pG[:, 8:16]
            P.mm(pg, tri, lf); P.mm(pe, K.ones, lf)
            P.tt("vector", S["t1"][:], li, pg, ALU.subtract)
            P.tt("vector", S["t2"][:], S["t1"][:], pe, ALU.add)
            P.act(S["w"][:], S["t2"][:], AF.Exp)
            P.act(S["dec"][:], pe, AF.Exp)
            if lat:
                P.act(S["nb"][:], pg, AF.Exp, scale=-1.0)
                P.act(S["ek"][:], S["t1"][:], AF.Exp)
            P.tt("vector", vw[d][:], V1[:], S["w"][:].m(lambda a: a.rearrange("p (h o) -> p h o", o=1).to_broadcast([128, 8, 129])), ALU.mult)

        def back(n):
            nonlocal pti
            step, d = its3[n]
            g = (fwd, bwd)[d][step]
            lat = g >= 2
            bi = step % NB
            r0 = g * 128
            tri = K.triF if d == 0 else K.triB
            V1, GL, KT, QT, KTT = v1[d][bi], glt[d][bi], kt[d][bi], qTt[d][bi], kTt[d][bi]
            S = {nm_: sm[nm_][d] for nm_ in sm}
            if lat:
                for gi, (h0, nh) in enumerate(HGRP):
                    pa_bank = pAccs[gi_ctr[0] % 2]; gi_ctr[0] += 1
                    for h in range(h0, h0 + nh):
                        pp = pPs[pti % 2][:, 0:128]
                        P.mm(pp, KTT[:, h, :], QT[:, h, :])
                        pt = PT[pti % 4]; pti += 1
                        P.stt("vector", pt[:], pp, S["ek"][:, h:h + 1], tri, ALU.mult, ALU.mult)
                        pa = pa_bank[:, (h - h0) * 170:(h - h0) * 170 + 129]
                        P.mm(pa, QT[:, h, :], Cnb[:, d, h, :], start=True, stop=False)
                        P.mm(pa, pt[:], V1[:, h, :], start=False, stop=True)
                    accv = pa_bank[:, 0:nh * 170].m(lambda a: a.rearrange("p (h c) -> p h c", c=170))
                    P.act(S["ad"][:, h0:h0 + nh], V(accv.ap[:, :, 128], accv.name), AF.Abs)
                    P.tt("vector", S["mx"][:, h0:h0 + nh], S["ad"][:, h0:h0 + nh], S["nb"][:, h0:h0 + nh], ALU.max)
                    P.op("vector", lambda E, a=S["r"], b=S["mx"], h0=h0, nh=nh: E.reciprocal(a.h[:, h0:h0 + nh], b.h[:, h0:h0 + nh]), [S["mx"][:]], [S["r"][:]])
                    P.tt("vector", hd[d][:, h0:h0 + nh, :], V(accv.ap[:, :, 0:128], accv.name),
                         S["r"][:, h0:h0 + nh].m(lambda a, nh=nh: a.rearrange("p (h o) -> p h o", o=1).to_broadcast([128, nh, 128])), ALU.mult)
                P.dma("sync", V(C.hd[d].h[r0 - LC: r0 - LC + 128, :].rearrange("p (h c) -> p h c", h=8), C.hd[d].name), hd[d][:])
            for gi, (h0, nh) in enumerate(HGRP):
                pu_bank = pUs[gu_ctr[0] % 2]; gu_ctr[0] += 1
                for h in range(h0, h0 + nh):
                    pu = pu_bank[:, (h - h0) * 170:(h - h0) * 170 + 129]
                    P.mm(pu, KT[:, h:h + 2, :].m(lambda a: a.rearrange("p a b -> p (a b)")), vw[d][:, h, :])
                cg = Cn[0:64, d, h0:h0 + nh]
                P.tt("vector", cg, cg, S["dec"][0:64, h0:h0 + nh].m(lambda a, nh=nh: a.rearrange("p (h o) -> p h o", o=1).to_broadcast([64, nh, 129])), ALU.mult)
                uv = V(pu_bank.h[0:64, 0:nh * 170].rearrange("p (h c) -> p h c", c=170)[:, :, 0:129], pu_bank.name)
                P.tt("vector", cg, cg, uv, ALU.add)
                P.cp("scalar", Cnb[0:64, d, h0:h0 + nh], cg)

        front(0)
        for n in range(68):
            if n + 1 < 68:
                front(n + 1)
            back(n)
        P.barrier(); P.flush()

TWO_PI = 6.283185307179586
I32 = mybir.dt.int32


def sincos(P, ang, s_out, c_out, tmp, tmpi, shape):
    for phase_off, dst in ((0.0, s_out), (0.25, c_out)):
        P.ts("vector", tmp, ang, 1.0 / TWO_PI, ALU.mult, phase_off, ALU.add)
        P.cp("vector", tmpi, tmp)
        P.cp("vector", tmp, tmpi)
        P.stt("vector", tmp, tmp, -TWO_PI, ang, ALU.mult, ALU.add)
        if phase_off != 0.0:
            P.ts("vector", tmp, tmp, TWO_PI * phase_off, ALU.add)
        P.ts("vector", tmp, tmp, 3.14159, ALU.min, -3.14159, ALU.max)
        P.act(dst, tmp, AF.Sin)


def phase4(P, C, K):
    with ExitStack() as ph:
        P.stack = ph
        Er = [P.sb("Er%d" % d, [128, 2048], F32) for d in range(2)]; Ei = [P.sb("Ei%d" % d, [128, 2048], F32) for d in range(2)]
        Fr = [P.sb("Fr%d" % d, [128, 16, 128], F32) for d in range(2)]; Fi = [P.sb("Fi%d" % d, [128, 16, 128], F32) for d in range(2)]
        Bbr = [P.sb("Bbr%d" % d, [128, 2048], BF16) for d in range(2)]; Bbi = [P.sb("Bbi%d" % d, [128, 2048], BF16) for d in range(2)]
        Cr = [P.sb("Cr%d" % d, [128, 2048], BF16) for d in range(2)]; Cin = [P.sb("Cin%d" % d, [128, 2048], BF16) for d in range(2)]
        Crn = [P.sb("Crn%d" % d, [128, 2048], BF16) for d in range(2)]
        ntribf = [P.sb("ntribf%d" % d, [128, 128], BF16) for d in range(2)]
        A8r = [P.sb("A8r%d" % d, [128, 16], F32) for d in range(2)]; A8i = [P.sb("A8i%d" % d, [128, 16], F32) for d in range(2)]
        tribf = [P.sb("tribf%d" % d, [128, 128], BF16) for d in range(2)]
        P.cp("vector", tribf[0][:], K.triF); P.cp("vector", tribf[1][:], K.triB)
        P.ts("vector", ntribf[0][:], K.triF, -1.0, ALU.mult); P.ts("vector", ntribf[1][:], K.triB, -1.0, ALU.mult)
        cc = [[(P.sb("cR%d%d" % (d, i), [128, 16], F32), P.sb("cI%d%d" % (d, i), [128, 16], F32)) for i in range(2)] for d in range(2)]
        for d in range(2):
            P.memset("vector", cc[d][0][0][:], 0.0); P.memset("vector", cc[d][0][1][:], 0.0)
        with ExitStack() as su:
            P.stack = su
            T = [P.sb("su%d" % i, [128, 2048], F32) for i in range(10)]
            TI = P.sb("sui", [128, 2048], I32)
            colp = P.sb("colp", [128, 3, 16], F32); lam = P.sb("lamc", [128, 2, 16], F32)
            for d in range(2):
                aRe, aIm, ldt, lRe, lIm, t5, t6, t7, t8, t9 = [t[:] for t in T]
                ti = TI[:]
                P.dma("sync", aRe, C.s5row[d, 0]); P.dma("sync", aIm, C.s5row[d, 1]); P.dma("sync", ldt, C.s5row[d, 2])
                P.act(ldt, ldt, AF.Exp)
                P.tt("vector", lRe, ldt, aRe, ALU.mult); P.tt("vector", lIm, ldt, aIm, ALU.mult)
                P.act(t5, lRe, AF.Exp)
                sincos(P, lIm, t6, t7, t8, ti, None)
                P.tt("vector", t7, t7, t5, ALU.mult)
                P.tt("vector", t6, t6, t5, ALU.mult)
                P.ts("vector", t7, t7, -1.0, ALU.add)
                P.tt("vector", t5, aRe, aRe, ALU.mult); P.tt("vector", t8, aIm, aIm, ALU.mult)
                P.tt("vector", t5, t5, t8, ALU.add)
                P.op("vector", lambda E, a=T[5]: E.reciprocal(a.h[:], a.h[:]), [t5], [t5])
                P.tt("vector", t8, t7, aRe, ALU.mult); P.tt("vector", t9, t6, aIm, ALU.mult)
                P.tt("vector", t8, t8, t9, ALU.add); P.tt("vector", t8, t8, t5, ALU.mult)
                P.tt("vector", t9, t6, aRe, ALU.mult); P.tt("vector", t6, t7, aIm, ALU.mult)
                P.tt("vector", t9, t9, t6, ALU.subtract); P.tt("vector", t9, t9, t5, ALU.mult)
                P.dma("sync", t5, C.bbd[d, 0]); P.dma("sync", t6, C.bbd[d, 1])
                P.tt("vector", t7, t8, t5, ALU.mult); P.tt("vector", aRe, t9, t6, ALU.mult)
                P.tt("vector", Bbr[d][:], t7, aRe, ALU.subtract)
                P.tt("vector", t7, t8, t6, ALU.mult); P.tt("vector", aRe, t9, t5, ALU.mult)
                P.tt("vector", Bbi[d][:], t7, aRe, ALU.add)
                P.dma("sync", t5, C.cbd[d, 0]); P.dma("sync", t6, C.cbd[d, 1])
                P.cp("vector", Cr[d][:], t5); P.ts("vector", Cin[d][:], t6, -1.0, ALU.mult)
                P.ts("vector", Crn[d][:], t5, -1.0, ALU.mult)
                idx = K.cst[:, 6, d:d + 1]
                P.ts("vector", t5, lRe, idx, ALU.mult); P.act(t5, t5, AF.Exp, scale=-1.0)
                P.ts("vector", t6, lIm, idx, ALU.mult)
                sincos(P, t6, t7, t8, t9, ti, None)
                P.tt("vector", Er[d][:], t5, t8, ALU.mult)
                P.tt("vector", t7, t5, t7, ALU.mult); P.ts("vector", Ei[d][:], t7, -1.0, ALU.mult)
                for i in range(3):
                    P.dma("sync", colp[:, i, :], C.s5col[d, i])
                P.act(colp[:, 2, :], colp[:, 2, :], AF.Exp)
                P.tt("vector", lam[:, 0, :], colp[:, 2, :], colp[:, 0, :], ALU.mult)
                P.tt("vector", lam[:, 1, :], colp[:, 2, :], colp[:, 1, :], ALU.mult)
                irow = K.cst[:, 4 + d, :]
                fR = t5.m(lambda a: a.rearrange("p (q t) -> p q t", t=128)); fI = t6.m(lambda a: a.rearrange("p (q t) -> p q t", t=128))
                for q in range(16):
                    P.ts("vector", V(fR.ap[:, q, :], fR.name), irow, lam[:, 0, q:q + 1], ALU.mult)
                    P.ts("vector", V(fI.ap[:, q, :], fI.name), irow, lam[:, 1, q:q + 1], ALU.mult)
                P.act(t5, t5, AF.Exp)
                sincos(P, t6, t7, t8, t9, ti, None)
                P.tt("vector", Fr[d][:].m(lambda a: a.rearrange("p q t -> p (q t)")), t5, t8, ALU.mult)
                P.tt("vector", Fi[d][:].m(lambda a: a.rearrange("p q t -> p (q t)")), t5, t7, ALU.mult)
                s16 = [V(t.h[:, 0:16], t.name) for t in T[5:10]]; s16i = V(TI.h[:, 0:16], TI.name)
                P.ts("vector", s16[0], lam[:, 0, :], 128.0, ALU.mult); P.act(s16[0], s16[0], AF.Exp)
                P.ts("vector", s16[1], lam[:, 1, :], 128.0, ALU.mult)
                sincos(P, s16[1], s16[2], s16[3], s16[4], s16i, None)
                P.tt("vector", A8r[d][:], s16[0], s16[3], ALU.mult); P.tt("vector", A8i[d][:], s16[0], s16[2], ALU.mult)
            P.barrier(); P.flush()
        P.stack = ph
        ut = [[P.sb("ut%d%d" % (d, i), [128, 4, 128], BF16) for i in range(2)] for d in range(2)]
        Z = [[P.sb("Z%d_%d" % (k, d), [128, 2048], BF16) for d in range(2)] for k in range(4)]
        Sp = [(P.sb("Spr%d" % i, [128, 4, 128], F32), P.sb("Spi%d" % i, [128, 4, 128], F32)) for i in range(2)]
        Hq = [[P.sb("Hq%d_%d" % (k, i), [128, 4, 128], BF16) for i in range(2)] for k in range(4)]
        sl = [P.sb("s5l%d" % i, [128, 4], F32) for i in range(4)]
        ysb = [P.sb("ysb%d" % d, [128, 4, 128], F32) for d in range(2)]
        pBr = P.ps("pBr", [128, 512]); pBi = P.ps("pBi", [128, 512])
        pSrs = [P.ps("pSr%d" % i, [128, 512]) for i in range(2)]; pSis = [P.ps("pSi%d" % i, [128, 512]) for i in range(2)]
        pYs = [P.ps("pY%d" % i, [128, 512]) for i in range(2)]
        fwd, bwd = chunk_orders()
        its = [(step, d) for step in range(34) for d in range(2)]

        def BZ(n):
            step, d = its[n]
            g = (fwd, bwd)[d][step]; r0 = g * 128
            u_ = ut[d][step % 2]
            P.dma("sync", u_[:], V(C.uT.h[:, r0:r0 + 128].rearrange("(j p) t -> p j t", p=128), C.uT.name))
            for j in range(4):
                P.mm(pBr[:], u_[:, j, :], Bbr[d][:, j * 512:(j + 1) * 512])
                P.mm(pBi[:], u_[:, j, :], Bbi[d][:, j * 512:(j + 1) * 512])
                hs = slice(j * 512, (j + 1) * 512)
                P.tt("vector", Z[0][d][:, hs], Er[d][:, hs], pBr[:], ALU.mult)
                P.tt("vector", Z[3][d][:, hs], Ei[d][:, hs], pBr[:], ALU.mult)
                P.tt("vector", Z[1][d][:, hs], Ei[d][:, hs], pBi[:], ALU.mult)
                P.tt("vector", Z[2][d][:, hs], Er[d][:, hs], pBi[:], ALU.mult)

        def cumsum(n, qg):
            step, d = its[n]
            b = (4 * n + qg) % 2
            pSr = pSrs[b]; pSi = pSis[b]
            for qq in range(4):
                q = 4 * qg + qq
                ps_r = pSr[:, qq * 128:(qq + 1) * 128]
                P.mm(ps_r, Z[0][d][:, q * 128:(q + 1) * 128], tribf[d][:], start=True, stop=False)
                P.mm(ps_r, Z[1][d][:, q * 128:(q + 1) * 128], ntribf[d][:], start=False, stop=True)
            for qq in range(4):
                q = 4 * qg + qq
                ps_i = pSi[:, qq * 128:(qq + 1) * 128]
                P.mm(ps_i, Z[2][d][:, q * 128:(q + 1) * 128], tribf[d][:], start=True, stop=False)
                P.mm(ps_i, Z[3][d][:, q * 128:(q + 1) * 128], tribf[d][:], start=False, stop=True)

        def evacH(n, qg):
            step, d = its[n]
            lat = (fwd, bwd)[d][step] >= 2
            b = (4 * n + qg) % 2
            pSr = pSrs[b]; pSi = pSis[b]
            cR, cI = cc[d][step % 2]; nR, nI = cc[d][(step + 1) % 2]
            last = 127 if d == 0 else 0
            spr, spi = Sp[b]
            qs = slice(4 * qg, 4 * qg + 4)
            for qq in range(4):
                q = 4 * qg + qq
                P.act(spr[:, qq, :], pSr[:, qq * 128:(qq + 1) * 128], AF.Identity, bias=cR[:, q:q + 1])
            for qq in range(4):
                q = 4 * qg + qq
                P.act(spi[:, qq, :], pSi[:, qq * 128:(qq + 1) * 128], AF.Identity, bias=cI[:, q:q + 1])
            P.tt("vector", sl[0][:], A8r[d][:, qs], spr[:, :, last], ALU.mult)
            P.tt("vector", sl[1][:], A8i[d][:, qs], spi[:, :, last], ALU.mult)
            P.tt("vector", nR[:, qs], sl[0][:], sl[1][:], ALU.subtract)
            P.tt("vector", sl[2][:], A8r[d][:, qs], spi[:, :, last], ALU.mult)
            P.tt("vector", sl[3][:], A8i[d][:, qs], spr[:, :, last], ALU.mult)
            P.tt("vector", nI[:, qs], sl[2][:], sl[3][:], ALU.add)
            if lat:
                H = [Hq[k][b] for k in range(4)]
                P.tt("vector", H[0][:], Fr[d][:, qs, :], spr[:], ALU.mult)
                P.tt("vector", H[1][:], Fi[d][:, qs, :], spi[:], ALU.mult)
                P.tt("vector", H[2][:], Fr[d][:, qs, :], spi[:], ALU.mult)
                P.tt("vector", H[3][:], Fi[d][:, qs, :], spr[:], ALU.mult)

        def Yq(n, qg):
            step, d = its[n]
            if (fwd, bwd)[d][step] < 2:
                return
            b = (4 * n + qg) % 2
            H = [Hq[k][b] for k in range(4)]
            py = pYs[n % 2][:, qg * 128:(qg + 1) * 128]
            for qq in range(4):
                q = 4 * qg + qq
                cs = slice(q * 128, (q + 1) * 128)
                P.mm(py, Cr[d][:, cs], H[0][:, qq, :], start=(qq == 0), stop=False)
                P.mm(py, Crn[d][:, cs], H[1][:, qq, :], start=False, stop=False)
                P.mm(py, Cin[d][:, cs], H[2][:, qq, :], start=False, stop=False)
                P.mm(py, Cin[d][:, cs], H[3][:, qq, :], start=False, stop=(qq == 3))

        BZ(0)
        for n in range(68):
            step, d = its[n]
            g = (fwd, bwd)[d][step]; lat = g >= 2; r0 = g * 128
            if n + 1 < 68:
                BZ(n + 1)
            cumsum(n, 0); evacH(n, 0)
            cumsum(n, 1); evacH(n, 1)
            Yq(n, 0)
            cumsum(n, 2); evacH(n, 2)
            Yq(n, 1)
            cumsum(n, 3); evacH(n, 3)
            Yq(n, 2); Yq(n, 3)
            if lat:
                P.cp("scalar", ysb[d][:], pYs[n % 2][:].m(lambda a: a.rearrange("p (j t) -> p j t", t=128)))
                P.dma("sync", V(C.yT[d].h[:, r0 - LC: r0 - LC + 128].rearrange("(j p) t -> p j t", p=128), C.yT[d].name), ysb[d][:])
        P.barrier(); P.flush()

def bcl(v, n, m):
    return v.m(lambda a: a.rearrange("p (h o) -> p h o", o=1).to_broadcast([128, n, m]))


def phase5(P, C, K):
    with ExitStack() as ph:
        P.stack = ph
        gmh = P.sb("gmh", [128, 1024], F32); P.dma("sync", gmh[:], C.gmh[:])
        hf = [P.sb("p5hf%d" % i, [128, 8, 128], F32) for i in range(2)]
        hb = [P.sb("p5hb%d" % i, [128, 8, 128], F32) for i in range(2)]
        og = [P.sb("p5og%d" % i, [128, 1024], BF16) for i in range(2)]
        sqs = [P.sb("p5sq%d" % i, [128, 8, 128], F32) for i in range(2)]; hns = [P.sb("p5hn%d" % i, [128, 1024], F32) for i in range(2)]
        st = [P.sb("p5st%d" % i, [128, 8, 128], BF16) for i in range(2)]
        s8s = [{n: P.sb("p5_%s%d" % (n, i), [128, 8], F32) for n in ["ss", "ms", "sq", "r"]} for i in range(2)]
        pT = P.ps("p5pT", [128, 1024])

        def s1(t):
            a, b_, o_ = hf[t % 2], hb[t % 2], og[t % 2]
            sq = sqs[t % 2]; s8 = s8s[t % 2]
            r0 = t * 128
            P.dma("sync", a[:], V(C.hd[0].h[r0:r0 + 128, :].rearrange("p (h c) -> p h c", h=8), C.hd[0].name))
            P.dma("sync", b_[:], V(C.hd[1].h[r0:r0 + 128, :].rearrange("p (h c) -> p h c", h=8), C.hd[1].name))
            P.dma("sync", o_[:], C.og[r0:r0 + 128, :])
            P.tt("vector", a[:], a[:], b_[:], ALU.add)
            for h in range(8):
                P.act(sq[:, h, :], a[:, h, :], AF.Square, accum=s8["ss"][:, h:h + 1])
            P.ts("vector", s8["ms"][:], s8["ss"][:], 1.0 / 128, ALU.mult, EPS, ALU.add)
            P.act(s8["sq"][:], s8["ms"][:], AF.Sqrt)
            P.op("vector", lambda E, s8=s8: E.reciprocal(s8["r"].h[:], s8["sq"].h[:]), [s8["sq"][:]], [s8["r"][:]])

        def s2(t):
            a, o_ = hf[t % 2], og[t % 2]
            hn = hns[t % 2]; s8 = s8s[t % 2]
            r0 = t * 128
            hv = hn[:].m(lambda x: x.rearrange("p (h c) -> p h c", h=8))
            P.tt("vector", hv, a[:], bcl(s8["r"][:], 8, 128), ALU.mult)
            P.tt("vector", hn[:], hn[:], gmh[:], ALU.mult)
            P.tt("vector", hn[:], hn[:], o_[:], ALU.mult)
            for j in range(8):
                P.tr(pT[:, j * 128:(j + 1) * 128], hn[:, j * 128:(j + 1) * 128], K.ident)
            s_ = st[t % 2]
            P.cp("scalar", s_[:], pT[:].m(lambda x: x.rearrange("p (j t) -> p j t", j=8)))
            P.dma("sync", V(C.hmT.h[:, r0:r0 + 128].rearrange("(j p) t -> p j t", p=128), C.hmT.name), s_[:])

        s1(0)
        for t in range(32):
            if t + 1 < 32:
                s1(t + 1)
            s2(t)
        P.barrier(); P.flush()


def phase6(P, C, K):
    with ExitStack() as ph:
        P.stack = ph
        wA = P.sb("wA", [128, 8, 1024], BF16); wG = P.sb("wG", [128, 4, 512], BF16)
        wB = P.sb("wB", [128, 4, 1024], BF16); wO = P.sb("wO", [128, 8, 1024], BF16)
        wR = P.sb("wR", [128, 8, 32], F32); bR = P.sb("bR", [128, 32], F32)
        bglu = P.sb("bglu", [128, 4], F32); s5d = P.sb("s5dt", [128, 4], F32)
        P.dma("gpsimd", wA[:], V(C.w_a.h.rearrange("(k p) n -> p k n", p=128), C.w_a.name))
        P.dma("gpsimd", wG[:], V(C.w_glu.h.rearrange("(k p) n -> p k n", p=128), C.w_glu.name))
        P.dma("gpsimd", wB[:], V(C.w_b.h.rearrange("(k p) n -> p k n", p=128), C.w_b.name))
        P.dma("sync", wR[:], V(C.w_r.h.rearrange("(k p) n -> p k n", p=128), C.w_r.name))
        P.dma("sync", bR[:], C.b_r[:]); P.dma("sync", bglu[:], C.b_glu[:]); P.dma("sync", s5d[:], C.s5d[:])
        with ExitStack() as su:
            P.stack = su
            wo32 = P.sb("wo32", [128, 8, 1024], F32)
            P.dma("sync", wo32[:], V(C.w_o.h.rearrange("(k p) n -> p k n", p=128), C.w_o.name))
            P.tt("vector", wO[:], wo32[:], K.gt1bc[:].m(lambda a: a.rearrange("p (o n) -> p o n", o=1).to_broadcast([128, 8, 1024])), ALU.mult)
            P.barrier(); P.flush()
        P.stack = ph
        W = norm_work(P, "6")
        hmT = [P.sb("p6hm%d" % i, [128, 8, 512], BF16) for i in range(2)]
        yf = P.sb("p6yf", [128, 4, 512], F32); yb = P.sb("p6yb", [128, 4, 512], F32)
        uT = P.sb("p6u", [128, 4, 512], BF16); GT = [P.sb("p6G%d" % i, [128, 16, 512], BF16) for i in range(2)]
        x2 = P.sb("p6x2", [128, 4, 512], F32); sg = P.sb("p6sg", [128, 4, 512], F32)
        ysg = P.sb("p6ysg", [128, 4, 512], BF16); ys2 = P.sb("p6ys2", [128, 4, 512], BF16)
        sgz = P.sb("p6sgz", [128, 512], F32); m1 = P.sb("p6m1", [128, 512], F32); m2 = P.sb("p6m2", [128, 512], F32)
        mg = P.sb("p6mg", [128, 8, 512], BF16)
        xt = [P.sb("p6xt%d" % i, [128, 1024], F32) for i in range(3)]
        h2s = P.sb("p6h2s", [128, 8, 512], BF16); h2f = P.sb("p6h2f", [128, 8, 128], F32)
        lg = P.sb("p6lg", [128, 32], F32); m8 = P.sb("p6m8", [128, 8], F32); nm = P.sb("p6nm", [128, 1], F32)
        msk = P.sb("p6msk", [128, 32], F32); ex = P.sb("p6ex", [128, 32], F32); ssum = P.sb("p6ssum", [128, 1], F32)
        gts = P.sb("p6gts", [128, 4, 32], F32); gT = P.sb("p6gT", [32, 512], F32)
        pa = [P.ps("p6pa%d" % i, [128, 512]) for i in range(4)]
        pr = P.ps("p6pr", [128, 512]); pgT = P.ps("p6pgT", [128, 512])
        pai = 0
        for t in range(8):
            o0 = t * 512
            hm = hmT[t % 2]; G = GT[t % 2]
            P.dma("sync", hm[:], V(C.hmT.h[:, o0:o0 + 512].rearrange("(j p) t -> p j t", p=128), C.hmT.name))
            P.dma("sync", yf[:], V(C.yT[0].h[:, o0:o0 + 512].rearrange("(j p) t -> p j t", p=128), C.yT[0].name))
            P.dma("sync", yb[:], V(C.yT[1].h[:, o0:o0 + 512].rearrange("(j p) t -> p j t", p=128), C.yT[1].name))
            P.dma("sync", uT[:], V(C.uT.h[:, LC + o0:LC + o0 + 512].rearrange("(j p) t -> p j t", p=128), C.uT.name))
            P.dma("sync", G[:], V(C.GT.h[:, o0:o0 + 512].rearrange("(j p) t -> p j t", p=128), C.GT.name))
            P.tt("vector", yf[:], yf[:], yb[:], ALU.add)
            for j in range(4):
                P.stt("vector", yf[:, j, :], uT[:, j, :], s5d[:, j:j + 1], yf[:, j, :], ALU.mult, ALU.add)
            P.act(x2[:], yf[:], AF.Square)
            P.ts("vector", x2[:], x2[:], 0.044715, ALU.mult, 1.0, ALU.add)
            P.tt("vector", x2[:], x2[:], yf[:], ALU.mult)
            P.act(sg[:], x2[:], AF.Sigmoid, scale=1.5957691216057308)
            P.tt("vector", ysg[:], yf[:], sg[:], ALU.mult)
            for n in range(4):
                pb = pa[pai % 4]; pai += 1
                for k in range(4):
                    P.mm(pb[:], wG[:, k, n * 128:(n + 1) * 128], ysg[:, k, :], start=(k == 0), stop=(k == 3))
                P.act(sgz[:], pb[:], AF.Sigmoid, bias=bglu[:, n:n + 1])
                P.tt("vector", ys2[:, n, :], ysg[:, n, :], sgz[:], ALU.mult)
            for n in range(8):
                pA_ = pa[pai % 4]; pai += 1
                for k in range(8):
                    P.mm(pA_[:], wA[:, k, n * 128:(n + 1) * 128], hm[:, k, :], start=(k == 0), stop=(k == 7))
                pB_ = pa[pai % 4]; pai += 1
                for k in range(4):
                    P.mm(pB_[:], wB[:, k, n * 128:(n + 1) * 128], ys2[:, k, :], start=(k == 0), stop=(k == 3))
                P.tt("vector", m1[:], pA_[:], G[:, n, :], ALU.mult)
                P.tt("vector", m2[:], pB_[:], G[:, 8 + n, :], ALU.mult)
                P.tt("vector", mg[:, n, :], m1[:], m2[:], ALU.add)
            def stA(s):
                nonlocal pai
                x_ = xt[s % 3]
                r0 = o0 + s * 128
                P.dma("sync", x_[:], C.x[r0:r0 + 128, :])
                for hh in range(2):
                    pb = pa[pai % 4]; pai += 1
                    for k in range(8):
                        P.mm(pb[:], mg[:, k, s * 128:(s + 1) * 128], wO[:, k, hh * 512:(hh + 1) * 512], start=(k == 0), stop=(k == 7))
                    P.tt("vector", x_[:, hh * 512:(hh + 1) * 512], x_[:, hh * 512:(hh + 1) * 512], pb[:], ALU.add)
                P.dma("sync", C.x1[r0:r0 + 128, :], x_[:])

            def stB(s):
                x_ = xt[s % 3]
                norm_T(P, K, x_[:], K.S2[:], K.SH2[:], h2s[:, :, s * 128:(s + 1) * 128], W, need_f32=h2f[:])
                for k in range(8):
                    P.mm(pr[:, 0:32], h2f[:, k, :], wR[:, k, :], start=(k == 0), stop=(k == 7))
                P.tt("vector", lg[:], pr[:, 0:32], bR[:], ALU.add)
                P.op("vector", lambda E: E.max(m8.h[:], lg.h[:]), [lg[:]], [m8[:]])
                P.ts("vector", msk[:], lg[:], m8[:, 3:4], ALU.is_ge)
                P.ts("vector", nm[:], m8[:, 0:1], -1.0, ALU.mult)
                P.act(ex[:], lg[:], AF.Exp, bias=nm[:])
                P.tt("vector", ex[:], ex[:], msk[:], ALU.mult)
                P.op("vector", lambda E: E.reduce_sum(ssum.h[:], ex.h[:], AX.X), [ex[:]], [ssum[:]])
                P.op("vector", lambda E: E.reciprocal(ssum.h[:], ssum.h[:]), [ssum[:]], [ssum[:]])
                P.ts("vector", gts[:, s, :], ex[:], ssum[:], ALU.mult)
                P.tr(pgT[0:32, s * 128:(s + 1) * 128], gts[:, s, :], K.ident)

            stA(0); stA(1); stB(0); stA(2); stB(1); stA(3); stB(2); stB(3)
            P.cp("vector", gT[:], pgT[0:32, :])
            P.dma("sync", V(C.h2T.h[:, o0:o0 + 512].rearrange("(j p) t -> p j t", p=128), C.h2T.name), h2s[:])
            P.dma("sync", V(C.gates.h[o0:o0 + 512, :].rearrange("(s p) e -> p s e", p=128), C.gates.name), gts[:])
            P.dma("sync", C.gatesT[:, o0:o0 + 512], gT[:])
        P.barrier(); P.flush()


def phase7(P, C, K):
    with ExitStack() as ph:
        P.stack = ph
        bein = P.sb("bein", [128, 32, 16], F32); P.dma("sync", bein[:], V(C.b_ein.h.rearrange("p (e j) -> p e j", j=16), C.b_ein.name))
        bein1 = P.sb("bein1", [128, 32, 8], F32)
        P.ts("vector", bein1[:], bein[:, :, 8:16], 1.0, ALU.add)
        beo = P.sb("beo", [32, 1024], F32); P.dma("sync", beo[:], C.b_eout[:])
        gfin = P.sb("gfin", [128, 1024], F32); P.dma("sync", gfin[:], C.gfin[:])
        Win = [P.sb("Win%d" % i, [128, 8, 2048], BF16) for i in range(2)]
        Wout = [P.sb("Wout%d" % i, [128, 8, 1024], BF16) for i in range(2)]
        h2 = P.sb("p7h2", [128, 8, 1024], BF16)
        acc = P.sb("p7acc", [128, 8, 1024], F32)
        gt_ = P.sb("p7g", [128, 8, 32], F32); gTt = P.sb("p7gT", [32, 1024], F32)
        actT = [P.sb("p7act%d" % i, [128, 8, 512], BF16) for i in range(2)]
        tg = [P.sb("p7tg%d" % i, [128, 512], F32) for i in range(2)]; ts_ = [P.sb("p7ts%d" % i, [128, 512], F32) for i in range(2)]
        tl = [P.sb("p7tl%d" % i, [128, 512], F32) for i in range(2)]
        x1t = [P.sb("p7x1", [128, 1024], F32)] * 2
        fs = {n: P.sb("p7_" + n, [128, 1], F32) for n in ["ss", "ms", "sq", "r"]}
        pz = [P.ps("p7pz%d" % i, [128, 512]) for i in range(4)]
        po = [P.ps("p7po%d" % i, [128, 512]) for i in range(4)]
        zi = 0; oi = 0; ti = 0; wi = 0
        ck = ("cvt", "sw")
        P.persist.discard(ck)
        cvt_sem = P.dsem[ck][0]
        rr = lambda a: a.rearrange("(k p) n -> p k n", p=128)
        for grp in range(4):
            g0 = grp * 1024
            P.dma("sync", h2[:], V(C.h2T.h[:, g0:g0 + 1024].rearrange("(j p) t -> p j t", p=128), C.h2T.name))
            P.dma("sync", gt_[:], V(C.gates.h[g0:g0 + 1024, :].rearrange("(s p) e -> p s e", p=128), C.gates.name))
            P.dma("sync", gTt[:], C.gatesT[:, g0:g0 + 1024])
            P.ts("vector", gt_[:], gt_[:], 1.0 / 1.702, ALU.mult)
            for s in range(8):
                for hh in range(2):
                    pb = po[oi % 4]; oi += 1
                    P.mm(pb[:], gTt[:, s * 128:(s + 1) * 128], beo[:, hh * 512:(hh + 1) * 512])
                    P.cp("scalar", acc[:, s, hh * 512:(hh + 1) * 512], pb[:])
            for e in range(NE):
                wi_, wo_ = Win[wi % 2], Wout[wi % 2]; wi += 1
                if grp == 0:
                    cval = P.cvt_counts[min(e + 1, NE - 1)]
                    for eng_ in ("sync", "gpsimd"):
                        P.items[eng_].append(lambda E, sem=cvt_sem, val=cval: E.wait_ge(sem, val))
                        P.waited[eng_][("D", id(cvt_sem))] = cval
                        P.nwaits += 1
                for kk in range(4):
                    P.dma("sync" if kk < 2 else "gpsimd", wi_.k(("w", kk))[:, 2 * kk:2 * kk + 2, :],
                          V(rr(C.weinb.h[e, kk * 256:(kk + 1) * 256, :]), C.weinb.name))
                for kk in range(2):
                    P.dma("sync" if kk == 0 else "gpsimd", wo_.k(("w", kk))[:, 4 * kk:4 * kk + 4, :],
                          V(rr(C.weoutb.h[e, kk * 512:(kk + 1) * 512, :]), C.weoutb.name))
                for tt in range(2):
                    aT = actT[ti % 2]; ti += 1
                    for jn in range(8):
                        pg_ = pz[zi % 4]; pl_ = pz[(zi + 1) % 4]; zi += 2
                        for k in range(8):
                            P.mm(pg_[:], wi_[:, k, jn * 128:(jn + 1) * 128], h2[:, k, tt * 512:(tt + 1) * 512], start=(k == 0), stop=(k == 7))
                        for k in range(8):
                            P.mm(pl_[:], wi_[:, k, 1024 + jn * 128:1024 + (jn + 1) * 128], h2[:, k, tt * 512:(tt + 1) * 512], start=(k == 0), stop=(k == 7))
                        b = jn % 2
                        P.ts("vector", tg[b][:], pg_[:], bein[:, e, jn:jn + 1], ALU.add, 7.0, ALU.min)
                        P.act(ts_[b][:], tg[b][:], AF.Silu, scale=1.702)
                        P.ts("vector", tl[b][:], pl_[:], bein1[:, e, jn:jn + 1], ALU.add, -6.0, ALU.max)
                        P.stt("vector", aT[:, jn, :], tl[b][:], 8.0, ts_[b][:], ALU.min, ALU.mult)
                    for s in range(4):
                        sub = tt * 4 + s
                        for hh in range(2):
                            pb = po[oi % 4]; oi += 1
                            for k in range(8):
                                P.mm(pb[:], aT[:, k, s * 128:(s + 1) * 128], wo_[:, k, hh * 512:(hh + 1) * 512], start=(k == 0), stop=(k == 7))
                            av = V(acc.h[:, sub, hh * 512:(hh + 1) * 512], acc.name, (sub, hh))
                            P.stt("vector", av, pb[:], gt_[:, sub, e:e + 1], av, ALU.mult, ALU.add)
            for s in range(8):
                r0 = g0 + s * 128
                x_ = x1t[s % 2]
                P.dma("sync", x_[:], C.x1[r0:r0 + 128, :])
                P.tt("vector", acc[:, s, :], acc[:, s, :], K.gt2bc[:], ALU.mult)
                P.tt("vector", x_[:], x_[:], acc[:, s, :], ALU.add)
                P.act(actT[0][:, 0:2, :].m(lambda a: a.rearrange("p a b -> p (a b)")), x_[:], AF.Square, accum=fs["ss"][:])
                P.ts("vector", fs["ms"][:], fs["ss"][:], 1.0 / D, ALU.mult, EPS, ALU.add)
                P.act(fs["sq"][:], fs["ms"][:], AF.Sqrt)
                P.op("vector", lambda E: E.reciprocal(fs["r"].h[:], fs["sq"].h[:]), [fs["sq"][:]], [fs["r"][:]])
                P.act(x_[:], x_[:], AF.Identity, scale=fs["r"][:])
                P.tt("vector", x_[:], x_[:], gfin[:], ALU.mult)
                P.dma("sync", C.out[r0:r0 + 128, :], x_[:])
        P.barrier(); P.flush()

PHASES = 99
DBG = ()


def build_program(phases=99, dbg=(), only=None):
    nc = bass.Bass("TRN2", target_bir_lowering=False)
    with ExitStack() as outer:
        P = Prog(nc, outer)
        C = declare_io(P, dbg)
        K = phase0(P, C)
        if phases >= 1 and (only is None or 1 in only):
            phase1(P, C, K)
        if phases >= 2 and (only is None or 2 in only):
            phase2(P, C, K)
        if phases >= 3 and (only is None or 3 in only):
            phase3(P, C, K)
        if phases >= 4 and (only is None or 4 in only):
            phase4(P, C, K)
        if phases >= 5 and (only is None or 5 in only):
            phase5(P, C, K)
        if phases >= 6 and (only is None or 6 in only):
            phase6(P, C, K)
        if phases >= 7 and (only is None or 7 in only):
            phase7(P, C, K)
        P.stack = outer
        P.finish([C.out[:]])
        P.flush()
        print("instr counts", P.cnt, "waits", P.nwaits, "dma sems", {k: (len(v), max([x[1] for x in v] + [0])) for k, v in P.free_d.items()})
    return nc


_NC_CACHE = {}


def kernel(**inputs):
    inp = {k: np.asarray(v) for k, v in inputs.items()}
    key = (PHASES, DBG)
    if key not in _NC_CACHE:
        _NC_CACHE[key] = build_program(PHASES, DBG)
    nc = _NC_CACHE[key]
    in_maps = [host_prep(inp, b) for b in range(8)]
    res = run_bass_kernel_spmd(nc, in_maps, core_ids=list(range(8)))
    kernel.last = res
    out = np.stack([np.asarray(res.results[b]["dr_out"]) for b in range(8)], axis=0)
    return out.astype(np.float32)
```

```python
import numpy as np
import concourse.bass as bass
import concourse.mybir as mybir
from concourse.bass_utils import run_bass_kernel_spmd

F32 = mybir.dt.float32
BF16 = mybir.dt.bfloat16
ALU = mybir.AluOpType
AF = mybir.ActivationFunctionType
AX = mybir.AxisListType

ENGS = ["tensor", "vector", "scalar", "gpsimd", "sync"]
EPOCH = 16000


class V:
    __slots__ = ("ap", "name", "key")

    def __init__(self, ap, name, key=None):
        self.ap = ap
        self.name = name
        self.key = key

    def m(self, fn):
        return V(fn(self.ap), self.name, self.key)


class _TK:
    def __init__(self, t, key):
        self.t = t
        self.key = key

    def __getitem__(self, idx):
        return V(self.t.h[idx], self.t.name, self.key)


class T:
    def __init__(self, h, name):
        self.h = h
        self.name = name

    def __getitem__(self, idx):
        return V(self.h[idx], self.name, None)

    def k(self, key):
        return _TK(self, key)


class Prog:
    def __init__(self, nc, stack):
        self.nc = nc
        self.stack = stack
        self.semstack = stack
        self.items = {e: [] for e in ENGS}
        self.cnt = {e: 0 for e in ENGS}
        self.esems = {e: [] for e in ENGS}
        self.state = {}
        self.waited = {e: {} for e in ENGS}
        self.dsem = {}
        self.ntiles = 0
        self.nwaits = 0
        self.persist = set()

    def sb(self, name, shape, dt):
        h = self.stack.enter_context(self.nc.sbuf_tensor(name, list(shape), dt))
        return T(h, name)

    def ps(self, name, shape, dt=F32):
        h = self.stack.enter_context(self.nc.psum_tensor(name, list(shape), dt))
        return T(h, name)

    def dram(self, name, shape, dt, kind="Internal"):
        h = self.nc.dram_tensor(name, list(shape), dt, kind=kind)
        return T(h.ap() if hasattr(h, "ap") else h, name)

    def _esem(self, e, ep):
        while len(self.esems[e]) <= ep:
            s = self.semstack.enter_context(self.nc.semaphore("s_%s_%d" % (e, len(self.esems[e]))))
            self.esems[e].append(s)
        return self.esems[e][ep]

    def _dsem(self, key):
        if key not in self.dsem:
            if not hasattr(self, "free_d"):
                self.free_d = {"sw": [], "hw": []}
            fd = self.free_d[key[1]]
            if fd:
                fd.sort(key=lambda x: x[1])
                self.dsem[key] = fd.pop(0)
            else:
                self.ndsem = getattr(self, "ndsem", 0) + 1
                s = self.semstack.enter_context(self.nc.semaphore("d_%d" % self.ndsem))
                self.dsem[key] = [s, 0]
        return self.dsem[key]

    def _states(self, name, key):
        d = self.state.setdefault(name, {})
        if key is None:
            if None not in d:
                d[None] = [None, {}]
            return list(d.values())
        if key not in d:
            d[key] = [None, {}]
        out = [d[key]]
        if None in d:
            out.append(d[None])
        return out

    def _wait(self, eng, dep):
        if dep is None:
            return
        if dep[0] == "E":
            _, e2, idx = dep
            if e2 == eng and eng == "tensor":
                return
            ep = (idx - 1) // EPOCH
            val = idx - ep * EPOCH
            sem = self._esem(e2, ep)
            sk = ("E", e2, ep)
        else:
            _, dk = dep
            sem, val = self.dsem[dk]
            sk = ("D", id(sem))
        w = self.waited[eng]
        if w.get(sk, 0) >= val:
            return
        w[sk] = val
        self.nwaits += 1
        self.items[eng].append(lambda E, sem=sem, val=val: E.wait_ge(sem, val))

    def _deps(self, eng, reads, writes):
        for v in reads:
            for st in self._states(v.name, v.key):
                self._wait(eng, st[0])
        for v in writes:
            for st in self._states(v.name, v.key):
                self._wait(eng, st[0])
                for r in st[1].values():
                    self._wait(eng, r)

    def _record(self, dep, reads, writes):
        for v in reads:
            d = self.state.setdefault(v.name, {})
            if v.key not in d:
                d[v.key] = [None, {}]
            d[v.key][1][dep[:2]] = dep
        for v in writes:
            d = self.state.setdefault(v.name, {})
            if v.key is None:
                for k in list(d.keys()):
                    d[k] = [dep, {}]
                d[None] = [dep, {}]
            else:
                d[v.key] = [dep, {}]

    def op(self, eng, fn, reads, writes):
        reads = [r for r in reads if isinstance(r, V)]
        writes = [w for w in writes if isinstance(w, V)]
        self._deps(eng, reads, writes)
        self.cnt[eng] += 1
        idx = self.cnt[eng]
        ep = (idx - 1) // EPOCH
        sem = self._esem(eng, ep)
        self.items[eng].append(lambda E, fn=fn, sem=sem: fn(E).then_inc(sem, 1))
        self._record(("E", eng, idx), reads, writes)

    def dma(self, eng, out, in_, semkey=None, **kw):
        if semkey is None:
            semkey = out.name if not out.name.startswith("dr_") else in_.name
        semkey = (semkey, "sw" if eng == "gpsimd" else "hw")
        self._deps(eng, [in_], [out])
        ds = self._dsem(semkey)
        ds[1] += 16
        assert ds[1] < 60000, semkey
        sem = ds[0]
        o, i = out.ap, in_.ap
        self.items[eng].append(lambda E, o=o, i=i, sem=sem, kw=kw: E.dma_start(out=o, in_=i, **kw).then_inc(sem, 16))
        self._record(("D", semkey), [in_], [out])

    def mm(self, out, lhsT, rhs, start=True, stop=True):
        self.op("tensor", lambda E: E.matmul(out.ap, lhsT.ap, rhs.ap, start=start, stop=stop),
                [lhsT, rhs] + ([] if start else [out]), [out])

    def tr(self, out, in_, ident):
        self.op("tensor", lambda E: E.transpose(out.ap, in_.ap, ident.ap), [in_, ident], [out])

    def act(self, out, in_, func, bias=0.0, scale=1.0, accum=None, eng="scalar"):
        b = bias.ap if isinstance(bias, V) else bias
        s = scale.ap if isinstance(scale, V) else scale
        kw = {}
        if accum is not None:
            kw["accum_out"] = accum.ap
        self.op("scalar", lambda E: E.activation(out.ap, in_.ap, func, bias=b, scale=s, **kw),
                [in_, bias, scale], [out] + ([accum] if accum is not None else []))

    def tt(self, eng, out, a, b, op):
        self.op(eng, lambda E: E.tensor_tensor(out.ap, a.ap, b.ap, op), [a, b], [out])

    def ts(self, eng, out, a, s1, op0, s2=None, op1=None, accum=None):
        x1 = s1.ap if isinstance(s1, V) else s1
        x2 = s2.ap if isinstance(s2, V) else s2
        kw = {}
        if op1 is not None:
            kw["op1"] = op1
        if accum is not None:
            kw["accum_out"] = accum.ap
        self.op(eng, lambda E: E.tensor_scalar(out.ap, a.ap, x1, x2, op0, **kw), [a, s1, s2],
                [out] + ([accum] if accum is not None else []))

    def stt(self, eng, out, a, s, b, op0, op1):
        x = s.ap if isinstance(s, V) else s
        self.op(eng, lambda E: E.scalar_tensor_tensor(out.ap, a.ap, x, b.ap, op0, op1), [a, s, b], [out])

    def cp(self, eng, out, in_):
        if eng == "scalar":
            self.op(eng, lambda E: E.copy(out.ap, in_.ap), [in_], [out])
        else:
            self.op(eng, lambda E: E.tensor_copy(out.ap, in_.ap), [in_], [out])

    def memset(self, eng, out, val):
        self.op(eng, lambda E: E.memset(out.ap, val), [], [out])

    def finish(self, outs):
        for v in outs:
            for st in self._states(v.name, v.key):
                self._wait("sync", st[0])
        for e in ENGS:
            if self.cnt[e] > 0:
                self._wait("sync", ("E", e, self.cnt[e]))
        for dk in list(self.dsem.keys()):
            self._wait("sync", ("D", dk))

    def barrier(self):
        for e in ENGS:
            for e2 in ENGS:
                if self.cnt[e2] > 0:
                    self._wait(e, ("E", e2, self.cnt[e2]))
            for dk in list(self.dsem.keys()):
                if dk in self.persist:
                    continue
                self._wait(e, ("D", dk))
        self.state = {}
        if not hasattr(self, "free_d"):
            self.free_d = {"sw": [], "hw": []}
        for k, v in self.dsem.items():
            if k in self.persist:
                continue
            self.free_d[k[1]].append(v)
        self.dsem = {k: v for k, v in self.dsem.items() if k in self.persist}

    def flush(self):
        self.build()
        self.items = {e: [] for e in ENGS}

    def build(self):
        nc = self.nc
        with nc.Block() as block:
            for e in ENGS:
                items = self.items[e]
                if not items:
                    continue

                def body(E, items=items):
                    for it in items:
                        it(E)
                getattr(block, e)(body)
from contextlib import ExitStack
import ml_dtypes

D = 1024
L = 4096
LC = 256
LT = L + LC
NE = 32
OFF_QK, OFF_V, OFF_IF, OFF_U, OFF_O, OFF_G, IN_COLS = 0, 1024, 2048, 2080, 2592, 3616, 5664
EPS = 1e-6


def host_prep(inp, b):
    f = np.float32
    A = lambda a: np.ascontiguousarray(a, dtype=f)
    m = {}
    m["dr_x"] = A(inp["x"][b])
    m["dr_ctx"] = A(inp["ctx"][b])
    cc = np.stack([inp["c"][b].reshape(8, 128).T, inp["c_ctx"].reshape(8, 128).T], axis=-1)
    m["dr_cc"] = A(cc)
    m["dr_w_ada"] = A(inp["w_ada"][0])
    m["dr_b_ada"] = A(inp["b_ada"][0].reshape(48, 128).T)
    m["dr_g1"] = A(inp["g_norm1"][0].reshape(8, 128).T)
    m["dr_g2"] = A(inp["g_norm2"][0].reshape(8, 128).T)
    m["dr_w_in"] = A(inp["w_in"][0])
    m["dr_wconv"] = A(inp["w_conv_qk"][0].reshape(9, 8, 128).transpose(2, 1, 0))
    m["dr_bif"] = A(np.broadcast_to(inp["b_ifgate"][0].reshape(1, 32), (128, 32)))
    m["dr_gmh"] = A(np.broadcast_to(inp["g_mh"][0].reshape(1, 1024), (128, 1024)))
    m["dr_w_a"] = A(inp["w_branch_m"][0])
    row = np.zeros((2, 3, 128, 2048), f)
    col = np.zeros((2, 3, 128, 16), f)
    for d in range(2):
        ldt = np.repeat(inp["s5_log_dt"][0, d], 64)
        for i, arr in enumerate([inp["s5_a_re"][0, d].reshape(-1), inp["s5_a_im"][0, d].reshape(-1), ldt]):
            row[d, i] = np.broadcast_to(arr.reshape(1, 2048), (128, 2048))
            col[d, i] = arr.reshape(16, 128).T
    m["dr_s5row"] = row
    m["dr_s5col"] = col
    bbd = np.zeros((2, 2, 128, 4, 512), f)
    cbd = np.zeros((2, 2, 128, 16, 128), f)
    for d in range(2):
        for ri, (bsrc, csrc) in enumerate([(inp["s5_b_re"], inp["s5_c_re"]), (inp["s5_b_im"], inp["s5_c_im"])]):
            for g in range(32):
                j, gl = divmod(g, 8)
                bbd[d, ri, gl * 16:(gl + 1) * 16, j, gl * 64:(gl + 1) * 64] = bsrc[0, d, g].T
                q, g2 = divmod(g, 2)
                cbd[d, ri, g2 * 64:(g2 + 1) * 64, q, (q % 4) * 32 + g2 * 16:(q % 4) * 32 + g2 * 16 + 16] = csrc[0, d, g].T
    m["dr_bbd"] = bbd.reshape(2, 2, 128, 2048)
    m["dr_cbd"] = cbd.reshape(2, 2, 128, 2048)
    m["dr_s5d"] = A(inp["s5_d"][0].reshape(4, 128).T)
    m["dr_w_glu"] = A(inp["w_glu"][0])
    m["dr_b_glu"] = A(inp["b_glu"][0].reshape(4, 128).T)
    m["dr_w_b"] = A(inp["w_branch_s"][0])
    m["dr_b_gate"] = A(inp["b_merge_gate"][0].reshape(16, 128).T)
    m["dr_w_o"] = A(inp["w_o"][0])
    m["dr_w_r"] = A(inp["w_router"][0])
    m["dr_b_r"] = A(np.broadcast_to(inp["b_router"][0].reshape(1, 32), (128, 32)))
    m["dr_w_ein"] = A(inp["w_e_in"][0])
    m["dr_b_ein"] = A(inp["b_e_in"][0].reshape(32, 16, 128).transpose(2, 0, 1).reshape(128, 512))
    m["dr_w_eout"] = A(inp["w_e_out"][0])
    m["dr_b_eout"] = A(inp["b_e_out"][0])
    m["dr_gfin"] = A(np.broadcast_to(inp["g_final"].reshape(1, 1024), (128, 1024)))
    cst = np.zeros((128, 7, 128), f)
    ii = np.arange(128)
    cst[:, 0] = np.eye(128)
    cst[:, 1] = (ii[:, None] <= ii[None, :])
    cst[:, 2] = (ii[:, None] >= ii[None, :])
    cst[:, 3] = 1.0
    cst[:, 4] = ii[None, :]
    cst[:, 5] = 127 - ii[None, :]
    cst[:, 6, 0] = ii
    cst[:, 6, 1] = 127 - ii
    m["dr_cst"] = cst
    return m

class Ctx:
    pass


def declare_io(P, dbg):
    C = Ctx()
    def din(name, shape, dt=F32):
        return P.dram("dr_" + name, shape, dt, kind="ExternalInput")
    C.x = din("x", [L, D]); C.ctx = din("ctx", [LC, D]); C.cc = din("cc", [128, 8, 2])
    C.w_ada = din("w_ada", [D, 6144]); C.b_ada = din("b_ada", [128, 48])
    C.g1 = din("g1", [128, 8]); C.g2 = din("g2", [128, 8]); C.w_in = din("w_in", [D, IN_COLS])
    C.wconv = din("wconv", [128, 8, 9]); C.bif = din("bif", [128, 32]); C.gmh = din("gmh", [128, 1024])
    C.w_a = din("w_a", [D, D]); C.s5row = din("s5row", [2, 3, 128, 2048]); C.s5col = din("s5col", [2, 3, 128, 16])
    C.bbd = din("bbd", [2, 2, 128, 2048]); C.cbd = din("cbd", [2, 2, 128, 2048]); C.s5d = din("s5d", [128, 4])
    C.w_glu = din("w_glu", [512, 512]); C.b_glu = din("b_glu", [128, 4]); C.w_b = din("w_b", [512, D])
    C.b_gate = din("b_gate", [128, 16]); C.w_o = din("w_o", [D, D]); C.w_r = din("w_r", [D, 32])
    C.b_r = din("b_r", [128, 32]); C.w_ein = din("w_ein", [NE, D, 2048]); C.b_ein = din("b_ein", [128, 512])
    C.w_eout = din("w_eout", [NE, D, D]); C.b_eout = din("b_eout", [NE, D]); C.gfin = din("gfin", [128, 1024])
    C.cst = din("cst", [128, 7, 128])
    C.out = P.dram("dr_out", [L, D], F32, kind="ExternalOutput")
    def scr(name, shape, dt):
        kind = "ExternalOutput" if name in dbg else "Internal"
        return P.dram("dr_s_" + name, shape, dt, kind=kind)
    C.qkpre = scr("qkpre", [1024, LT], BF16)
    C.v = scr("v", [LT, 1024], BF16)
    C.gl = scr("gl", [LT, 32], F32)
    C.uT = scr("uT", [512, LT], BF16)
    C.GT = scr("GT", [2048, L], BF16)
    C.og = scr("og", [L, 1024], BF16)
    C.qT = scr("qT", [512, LT], BF16)
    C.kT = scr("kT", [512, LT], BF16)
    C.ktok = scr("ktok", [LT, 512], BF16)
    C.hd = [scr("hf", [L, 1024], F32), scr("hb", [L, 1024], F32)]
    C.hmT = scr("hmT", [1024, L], BF16)
    C.yT = [scr("yTf", [512, L], F32), scr("yTb", [512, L], F32)]
    C.x1 = scr("x1", [L, D], F32)
    C.h2T = scr("h2T", [1024, L], BF16)
    C.gates = scr("gates", [L, 32], F32)
    C.gatesT = scr("gatesT", [32, L], F32)
    C.modT = scr("modT", [128, 96], F32)
    C.wab = scr("wab", [D, D], BF16); C.wglub = scr("wglub", [512, 512], BF16); C.wbb = scr("wbb", [512, D], BF16)
    C.weinb = scr("weinb", [NE, D, 2048], BF16)
    C.weoutb = scr("weoutb", [NE, D, D], BF16)
    return C


def bc(v, shape, axis_pat):
    return v.m(lambda a: a.rearrange(axis_pat, o=1).to_broadcast(shape))


def phase0(P, C):
    K = Ctx()
    K.cst = P.sb("cst", [128, 7, 128], F32)
    P.dma("sync", K.cst[:], C.cst[:])
    K.ident = K.cst[:, 0, :]; K.triF = K.cst[:, 1, :]; K.triB = K.cst[:, 2, :]; K.ones = K.cst[:, 3, :]
    K.S1 = P.sb("S1", [128, 8], F32); K.SH1 = P.sb("SH1", [128, 8], F32)
    K.S1c = P.sb("S1c", [128, 8], F32); K.SH1c = P.sb("SH1c", [128, 8], F32)
    K.S2 = P.sb("S2", [128, 8], F32); K.SH2 = P.sb("SH2", [128, 8], F32)
    K.gt1bc = P.sb("gt1bc", [128, 1024], F32); K.gt2bc = P.sb("gt2bc", [128, 1024], F32)
    with ExitStack() as ph:
        P.stack = ph
        cc = P.sb("cc", [128, 8, 2], F32); sc = P.sb("sc", [128, 8, 2], F32)
        bada = P.sb("bada", [128, 48], F32); g1 = P.sb("g1", [128, 8], F32); g2 = P.sb("g2", [128, 8], F32)
        modT = P.sb("modT", [128, 48, 2], F32); tmp8 = P.sb("tmp8", [128, 8], F32)
        gt = P.sb("gt", [128, 16], F32)
        wa = [P.sb("wa%d" % i, [128, 8, 512], F32) for i in range(2)]
        diag = [P.sb("diag%d" % i, [128, 128], F32) for i in range(2)]
        pm = P.ps("pm", [128, 512]); pg = P.ps("pg", [128, 1024])
        P.dma("sync", cc[:], C.cc[:]); P.dma("sync", bada[:], C.b_ada[:])
        P.dma("sync", g1[:], C.g1[:]); P.dma("sync", g2[:], C.g2[:])
        P.act(sc[:], cc[:], AF.Silu)
        wv = C.w_ada[:].m(lambda a: a.rearrange("(k p) n -> p k n", p=128))
        for i in range(12):
            w = wa[i % 2]
            P.dma("sync" if i % 2 == 0 else "gpsimd", w[:], V(wv.ap[:, :, i * 512:(i + 1) * 512], wv.name))
            for mI in range(4):
                nn = 4 * i + mI
                for k in range(8):
                    P.mm(pm[:, nn * 2:nn * 2 + 2], w[:, k, mI * 128:(mI + 1) * 128], sc[:, k, :], start=(k == 0), stop=(k == 7))
        P.tt("vector", modT[:], pm[:, 0:96].m(lambda a: a.rearrange("p (n t) -> p n t", t=2)),
             bada[:].m(lambda a: a.rearrange("p (n o) -> p n o", o=1).to_broadcast([128, 48, 2])), ALU.add)
        P.cp("vector", K.SH1[:], modT[:, 0:8, 0]); P.cp("vector", K.SH1c[:], modT[:, 0:8, 1])
        P.cp("vector", K.SH2[:], modT[:, 24:32, 0])
        P.ts("vector", tmp8[:], modT[:, 8:16, 0], 1.0, ALU.add); P.tt("vector", K.S1[:], tmp8[:], g1[:], ALU.mult)
        P.ts("vector", tmp8[:], modT[:, 8:16, 1], 1.0, ALU.add); P.tt("vector", K.S1c[:], tmp8[:], g1[:], ALU.mult)
        P.ts("vector", tmp8[:], modT[:, 32:40, 0], 1.0, ALU.add); P.tt("vector", K.S2[:], tmp8[:], g2[:], ALU.mult)
        P.cp("vector", gt[:, 0:8], modT[:, 16:24, 0]); P.cp("vector", gt[:, 8:16], modT[:, 40:48, 0])
        for j in range(16):
            dg = diag[j % 2]
            P.ts("vector", dg[:], K.ident, gt[:, j:j + 1], ALU.mult)
            P.mm(pg[:, (j % 8) * 128:(j % 8 + 1) * 128], K.ones, dg[:])
            if j == 7:
                P.cp("vector", K.gt1bc[:], pg[:])
            if j == 15:
                P.cp("vector", K.gt2bc[:], pg[:])
        P.dma("sync", C.modT[:], modT[:].m(lambda a: a.rearrange("p n t -> p (n t)")))
        P.barrier(); P.flush()
    return K


def norm_T(P, K, xt, S, SH, hT_dst, W, need_f32=None):
    P.act(W["junk"][:], xt, AF.Square, accum=W["ss"][:])
    P.ts("vector", W["ms"][:], W["ss"][:], 1.0 / D, ALU.mult, EPS, ALU.add)
    P.act(W["sq"][:], W["ms"][:], AF.Sqrt)
    P.op("vector", lambda E: E.reciprocal(W["rstd"].h[:], W["sq"].h[:]), [W["sq"][:]], [W["rstd"][:]])
    P.act(W["xn"][:], xt, AF.Identity, scale=W["rstd"][:])
    for j in range(8):
        P.tr(W["psT"][:, j * 128:(j + 1) * 128], W["xn"][:, j * 128:(j + 1) * 128], K.ident)
    for j in range(8):
        pj = W["psT"][:, j * 128:(j + 1) * 128]
        sj = V(S.ap[:, j:j + 1], S.name, S.key); bj = V(SH.ap[:, j:j + 1], SH.name, SH.key)
        if need_f32 is not None:
            nf = V(need_f32.ap[:, j, :], need_f32.name, need_f32.key)
            P.act(nf, pj, AF.Identity, bias=bj, scale=sj)
        else:
            P.act(V(hT_dst.ap[:, j, :], hT_dst.name, hT_dst.key), pj, AF.Identity, bias=bj, scale=sj)
    if need_f32 is not None:
        P.cp("vector", hT_dst, need_f32)


def norm_work(P, sfx=""):
    W = {}
    W["junk"] = P.sb("nw_junk" + sfx, [128, 1024], BF16)
    W["xn"] = P.sb("nw_xn" + sfx, [128, 1024], F32)
    W["tm"] = P.sb("nw_tm" + sfx, [128, 8, 128], F32)
    for n in ["ss", "ms", "sq", "rstd"]:
        W[n] = P.sb("nw_" + n + sfx, [128, 1], F32)
    W["psT"] = P.ps("nw_psT" + sfx, [128, 1024])
    return W


def phase1(P, C, K):
    with ExitStack() as ph:
        P.stack = ph
        win = P.sb("win", [128, 8, IN_COLS], BF16)
        for k in range(8):
            P.dma("gpsimd", win[:, k, :], C.w_in[k * 128:(k + 1) * 128, :])
        rr = lambda a: a.rearrange("(k p) n -> p k n", p=128)
        for dst_, src_ in ((C.wab, C.w_a), (C.wglub, C.w_glu), (C.wbb, C.w_b)):
            P.dma("gpsimd", V(rr(dst_.h), dst_.name), V(rr(src_.h), src_.name), semkey="cvt6")
        for e in range(NE):
            for kk in range(4):
                P.dma("gpsimd", V(rr(C.weinb.h[e, kk * 256:(kk + 1) * 256, :]), C.weinb.name),
                      V(rr(C.w_ein.h[e, kk * 256:(kk + 1) * 256, :]), C.w_ein.name), semkey="cvt")
            for kk in range(2):
                P.dma("gpsimd", V(rr(C.weoutb.h[e, kk * 512:(kk + 1) * 512, :]), C.weoutb.name),
                      V(rr(C.w_eout.h[e, kk * 512:(kk + 1) * 512, :]), C.w_eout.name), semkey="cvt")
        P.persist.add(("cvt", "sw"))
        bif = P.sb("bif", [128, 32], F32); P.dma("sync", bif[:], C.bif[:])
        bgate = P.sb("bgate", [128, 16], F32); P.dma("sync", bgate[:], C.b_gate[:])
        W = norm_work(P)
        xt = [P.sb("xt%d" % i, [128, 1024], F32) for i in range(2)]
        hT = [P.sb("hT%d" % i, [128, 8, 512], BF16) for i in range(2)]
        pA = [P.ps("pA%d" % i, [128, 512]) for i in range(4)]
        st_qk = [P.sb("st_qk%d" % i, [128, 8, 512], BF16) for i in range(2)]
        st_u = [P.sb("st_u%d" % i, [128, 4, 512], BF16) for i in range(2)]
        st_G = [P.sb("st_G%d" % i, [128, 8, 512], BF16) for i in range(2)]
        st_v = [P.sb("st_v%d" % i, [128, 1024], BF16) for i in range(2)]
        st_o = [P.sb("st_o%d" % i, [128, 1024], BF16) for i in range(2)]
        st_g = [P.sb("st_g%d" % i, [128, 4, 32], F32) for i in range(2)]
        gw = {n: P.sb("gw_" + n, [128, 32], F32) for n in ["g", "e", "sp"]}
        tiles = [(True, 0, 256, 0)] + [(False, i * 512, 512, LC + i * 512) for i in range(8)]
        pai = 0
        for ti, (isc, off, nt, lto) in enumerate(tiles):
            par = ti % 2
            nsub = nt // 128
            src = C.ctx if isc else C.x
            h = hT[par]
            for s in range(nsub):
                x_ = xt[(ti * 4 + s) % 2]
                P.dma("sync", x_[:], src[off + s * 128: off + (s + 1) * 128, :])
                norm_T(P, K, x_[:], K.S1c[:] if isc else K.S1[:], K.SH1c[:] if isc else K.SH1[:],
                       h[:, :, s * 128:(s + 1) * 128], W)
            def fm(col0, ncol, stage, func, bias=None, evac="scalar"):
                nonlocal pai
                for m in range(ncol):
                    pb = pA[pai % 4]; pai += 1
                    for k in range(8):
                        P.mm(pb[:, 0:nt], win[:, k, col0 + m * 128: col0 + (m + 1) * 128], h[:, k, 0:nt], start=(k == 0), stop=(k == 7))
                    if func is None:
                        if m % 2 == 0:
                            P.cp("scalar", stage[:, m, 0:nt], pb[:, 0:nt])
                        else:
                            P.cp("vector", stage[:, m, 0:nt], pb[:, 0:nt])
                    else:
                        P.act(stage[:, m, 0:nt], pb[:, 0:nt], func, bias=V(bias.ap[:, m:m + 1], bias.name) if isinstance(bias, V) else bias[:, m:m + 1])
            fm(OFF_QK, 8, st_qk[par], None)
            P.dma("sync", V(C.qkpre.h[:, lto:lto + nt].rearrange("(j p) t -> p j t", p=128), C.qkpre.name), st_qk[par][:, :, 0:nt])
            fm(OFF_U, 4, st_u[par], None)
            P.dma("sync", V(C.uT.h[:, lto:lto + nt].rearrange("(j p) t -> p j t", p=128), C.uT.name), st_u[par][:, :, 0:nt])
            if not isc:
                for gh in range(2):
                    fm(OFF_G + gh * 1024, 8, st_G[gh], AF.Sigmoid, bias=V(bgate.h[:, gh * 8:(gh + 1) * 8], bgate.name))
                    P.dma("sync", V(C.GT.h[gh * 1024:(gh + 1) * 1024, off:off + nt].rearrange("(j p) t -> p j t", p=128), C.GT.name), st_G[gh][:, :, 0:nt])
            for s in range(nsub):
                for hh in range(2):
                    pb = pA[pai % 4]; pai += 1
                    for k in range(8):
                        P.mm(pb[:], h[:, k, s * 128:(s + 1) * 128], win[:, k, OFF_V + hh * 512: OFF_V + (hh + 1) * 512], start=(k == 0), stop=(k == 7))
                    P.cp("vector", st_v[s % 2][:, hh * 512:(hh + 1) * 512], pb[:])
                P.dma("sync", C.v[lto + s * 128: lto + (s + 1) * 128, :], st_v[s % 2][:])
                pb = pA[pai % 4]; pai += 1
                for k in range(8):
                    P.mm(pb[:, 0:32], h[:, k, s * 128:(s + 1) * 128], win[:, k, OFF_IF:OFF_IF + 32], start=(k == 0), stop=(k == 7))
                g = gw["g"]
                P.tt("vector", g[:], pb[:, 0:32], bif[:], ALU.add)
                gv = g[:].m(lambda a: a.rearrange("p (d i h) -> p d i h", d=2, i=2))
                sg = st_g[par][:, s, :].m(lambda a: a.rearrange("p (i d h) -> p i d h", i=2, d=2))
                P.cp("vector", V(sg.ap[:, 0], sg.name), V(gv.ap[:, :, 0, :], gv.name))
                P.act(gw["e"][:], g[:], AF.Exp, scale=-1.0)
                P.act(gw["sp"][:], gw["e"][:], AF.Ln, bias=1.0)
                spv = gw["sp"][:].m(lambda a: a.rearrange("p (d i h) -> p d i h", d=2, i=2))
                P.ts("vector", V(sg.ap[:, 1], sg.name), V(spv.ap[:, :, 1, :], spv.name), -1.0, ALU.mult)
                if not isc:
                    for hh in range(2):
                        pb = pA[pai % 4]; pai += 1
                        for k in range(8):
                            P.mm(pb[:], h[:, k, s * 128:(s + 1) * 128], win[:, k, OFF_O + hh * 512: OFF_O + (hh + 1) * 512], start=(k == 0), stop=(k == 7))
                        P.act(st_o[s % 2][:, hh * 512:(hh + 1) * 512], pb[:], AF.Sigmoid)
                    P.dma("sync", C.og[off + s * 128: off + (s + 1) * 128, :], st_o[s % 2][:])
            P.dma("sync", V(C.gl.h[lto:lto + nt, :].rearrange("(s p) c -> p s c", p=128), C.gl.name), st_g[par][:, 0:nsub, :])
        P.barrier(); P.flush()

def phase2(P, C, K):
    with ExitStack() as ph:
        P.stack = ph
        wc = P.sb("wc", [128, 8, 9], F32); P.dma("sync", wc[:], C.wconv[:])
        pre = [P.sb("pre%d" % i, [128, LT], BF16) for i in range(2)]
        acc = [P.sb("cacc%d" % i, [128, LT], F32) for i in range(2)]
        sl = [P.sb("csl%d" % i, [128, LT], F32) for i in range(2)]
        ob = [P.sb("cob%d" % i, [128, LT], BF16) for i in range(2)]
        kst = [P.sb("kst%d" % i, [128, 4, 128], BF16) for i in range(2)]
        pT = [P.ps("pT%d" % i, [128, 512]) for i in range(2)]
        for j in range(8):
            e = "vector"
            p_, a_, s_, o_ = pre[j % 2], acc[j % 2], sl[j % 2], ob[j % 2]
            P.dma("sync", p_[:], C.qkpre[j * 128:(j + 1) * 128, :])
            def w(t):
                return wc[:, j, t:t + 1]
            P.ts(e, a_[:, 0:LC], p_[:, 0:LC], w(4), ALU.mult)
            P.stt(e, a_[:, 1:LC], p_[:, 0:LC - 1], w(3), a_[:, 1:LC], ALU.mult, ALU.add)
            P.stt(e, a_[:, 0:LC - 1], p_[:, 1:LC], w(5), a_[:, 0:LC - 1], ALU.mult, ALU.add)
            pv = p_[:, LC:LT].m(lambda a: a.rearrange("p (r w) -> p r w", w=64))
            av = a_[:, LC:LT].m(lambda a: a.rearrange("p (r w) -> p r w", w=64))
            P.ts(e, a_[:, LC:LT], p_[:, LC:LT], w(4), ALU.mult)
            for dr in range(3):
                for dw in range(3):
                    if dr == 1 and dw == 1:
                        continue
                    ro = slice(max(0, 1 - dr), 64 - max(0, dr - 1)); ri = slice(max(0, dr - 1), 64 - max(0, 1 - dr))
                    co = slice(max(0, 1 - dw), 64 - max(0, dw - 1)); ci = slice(max(0, dw - 1), 64 - max(0, 1 - dw))
                    o_v = V(av.ap[:, ro, co], av.name); i_v = V(pv.ap[:, ri, ci], pv.name)
                    P.stt(e, o_v, i_v, w(dr * 3 + dw), o_v, ALU.mult, ALU.add)
            P.act(s_[:], a_[:], AF.Silu)
            if j < 4:
                if j % 2 == 0:
                    P.act(o_[:], s_[:], AF.Identity, scale=0.125)
                else:
                    P.ts("vector", o_[:], s_[:], 0.125, ALU.mult)
                P.dma("sync", C.qT[j * 128:(j + 1) * 128, :], o_[:])
            else:
                jj = j - 4
                P.cp("scalar" if j % 2 == 0 else "vector", o_[:], s_[:])
                P.dma("sync", C.kT[jj * 128:(jj + 1) * 128, :], o_[:])
                for g4 in range(9):
                    nb = min(4, 34 - g4 * 4)
                    pt = pT[g4 % 2]; ks = kst[g4 % 2]
                    for bI in range(nb):
                        blk = g4 * 4 + bI
                        P.tr(pt[:, bI * 128:(bI + 1) * 128], s_[:, blk * 128:(blk + 1) * 128], K.ident)
                    P.cp("scalar", ks[:, 0:nb, :], pt[:, 0:nb * 128].m(lambda a: a.rearrange("p (b c) -> p b c", c=128)))
                    P.dma("sync", V(C.ktok.h[g4 * 512: g4 * 512 + nb * 128, jj * 128:(jj + 1) * 128].rearrange("(b p) c -> p b c", p=128), C.ktok.name), ks[:, 0:nb, :])
        P.barrier(); P.flush()


HGRP = [(0, 3), (3, 3), (6, 2)]


def hoff(h):
    return (h // 3) * 512 + (h % 3) * 170


def chunk_orders():
    fwd = list(range(34))
    bwd = [1, 0] + list(range(33, 1, -1))
    return fwd, bwd


def phase3(P, C, K):
    with ExitStack() as ph:
        P.stack = ph
        Cn = P.sb("Cn", [128, 2, 8, 129], F32); Cnb = P.sb("Cnb", [128, 2, 8, 129], BF16)
        P.memset("vector", Cn[:], 0.0); P.memset("vector", Cnb[:], 0.0)
        NB = 2
        v1 = [[P.sb("v1_%d%d" % (d, i), [128, 8, 129], BF16) for i in range(NB)] for d in range(2)]
        glt = [[P.sb("glt_%d%d" % (d, i), [128, 32], F32) for i in range(NB)] for d in range(2)]
        kt = [[P.sb("kt_%d%d" % (d, i), [128, 9, 64], BF16) for i in range(NB)] for d in range(2)]
        qTt = [[P.sb("qTt_%d%d" % (d, i), [128, 8, 128], BF16) for i in range(NB)] for d in range(2)]
        kTt = [[P.sb("kTt_%d%d" % (d, i), [128, 8, 128], BF16) for i in range(NB)] for d in range(2)]
        for d in range(2):
            for i in range(NB):
                P.memset("vector", v1[d][i][:], 1.0)
                P.memset("vector", kt[d][i][:], 0.0)
                P.memset("vector", qTt[d][i][:], 0.0)
                P.memset("vector", kTt[d][i][:], 0.0)
        vw = [P.sb("vw%d" % d, [128, 8, 129], BF16) for d in range(2)]
        PT = [P.sb("PTs%d" % i, [128, 128], BF16) for i in range(4)]
        hd = [P.sb("hd%d" % d, [128, 8, 128], F32) for d in range(2)]
        sm = {n: [P.sb("sm_%s%d" % (n, d), [128, 8], F32) for d in range(2)] for n in ["nb", "t1", "ek", "t2", "w", "dec", "ad", "mx", "r"]}
        pG = P.ps("pG", [128, 512]); pPs = [P.ps("pP%d" % i, [128, 512]) for i in range(2)]
        pAccs = [P.ps("pAcc%d" % i, [128, 512]) for i in range(2)]; pUs = [P.ps("pU%d" % i, [128, 512]) for i in range(2)]
        gi_ctr = [0]; gu_ctr = [0]
        fwd, bwd = chunk_orders()
        pti = 0
        its3 = [(step, d) for step in range(34) for d in range(2)]

        def front(n):
            step, d = its3[n]
            g = (fwd, bwd)[d][step]
            lat = g >= 2
            bi = step % NB
            r0 = g * 128
            tri = K.triF if d == 0 else K.triB
            V1, GL, KT, QT, KTT = v1[d][bi], glt[d][bi], kt[d][bi], qTt[d][bi], kTt[d][bi]
            S = {nm_: sm[nm_][d] for nm_ in sm}
            g = (fwd, bwd)[d][step]
            lat = g >= 2
            bi = step % NB
            r0 = g * 128
            tri = K.triF if d == 0 else K.triB
            V1, GL, KT, QT, KTT = v1[d][bi], glt[d][bi], kt[d][bi], qTt[d][bi], kTt[d][bi]
            P.dma("sync", V1[:, :, 0:128], V(C.v.h[r0:r0 + 128, :].rearrange("p (h c) -> p h c", h=8), C.v.name))
            P.dma("sync", GL[:], C.gl[r0:r0 + 128, :])
            P.dma("sync", KT[:, 0:8, :], V(C.ktok.h[r0:r0 + 128, :].rearrange("p (h c) -> p h c", h=8), C.ktok.name))
            if lat:
                P.dma("sync", QT[0:64, :, :], V(C.qT.h[:, r0:r0 + 128].rearrange("(h k) t -> k h t", k=64), C.qT.name))
                P.dma("sync", KTT[0:64, :, :], V(C.kT.h[:, r0:r0 + 128].rearrange("(h k) t -> k h t", k=64), C.kT.name))
            li = GL[:, 8 * d: 8 * d + 8]; lf = GL[:, 16 + 8 * d: 16 + 8 * d + 8]
            pg = pG[:, 0:8]; pe = pG[:, 8:16]
            P.mm(pg, tri, lf); P.mm(pe, K.ones, lf)
            P.tt("vector", S["t1"][:], li, pg, ALU.subtract)
            P.tt("vector", S["t2"][:], S["t1"][:], pe, ALU.add)
            P.act(S["w"][:], S["t2"][:], AF.Exp)
            P.act(S["dec"][:], pe, AF.Exp)
            if lat:
                P.act(S["nb"][:], pg, AF.Exp, scale=-1.0)
                P.act(S["ek"][:], S["t1"][:], AF.Exp)
            P.tt("vector", vw[d][:], V1[:], S["w"][:].m(lambda a: a.rearrange("p (h o) -> p h o", o=1).to_broadcast([128, 8, 129])), ALU.mult)

        def back(n):
            nonlocal pti
            step, d = its3[n]
            g = (fwd, bwd)[d][step]
            lat = g >= 2
            bi = step % NB
            r0 = g * 128
            tri = K.triF if d == 0 else K.triB
            V1, GL, KT, QT, KTT = v1[d][bi], glt[d][bi], kt[d][bi], qTt[d][bi], kTt[d][bi]
            S = {nm_: sm[nm_][d] for nm_ in sm}
            if lat:
                for gi, (h0, nh) in enumerate(HGRP):
                    pa_bank = pAccs[gi_ctr[0] % 2]; gi_ctr[0] += 1
                    for h in range(h0, h0 + nh):
                        pp = pPs[pti % 2][:, 0:128]
                        P.mm(pp, KTT[:, h, :], QT[:, h, :])
                        pt = PT[pti % 4]; pti += 1
                        P.stt("vector", pt[:], pp, S["ek"][:, h:h + 1], tri, ALU.mult, ALU.mult)
                        pa = pa_bank[:, (h - h0) * 170:(h - h0) * 170 + 129]
                        P.mm(pa, QT[:, h, :], Cnb[:, d, h, :], start=True, stop=False)
                        P.mm(pa, pt[:], V1[:, h, :], start=False, stop=True)
                    accv = pa_bank[:, 0:nh * 170].m(lambda a: a.rearrange("p (h c) -> p h c", c=170))
                    P.act(S["ad"][:, h0:h0 + nh], V(accv.ap[:, :, 128], accv.name), AF.Abs)
                    P.tt("vector", S["mx"][:, h0:h0 + nh], S["ad"][:, h0:h0 + nh], S["nb"][:, h0:h0 + nh], ALU.max)
                    P.op("vector", lambda E, a=S["r"], b=S["mx"], h0=h0, nh=nh: E.reciprocal(a.h[:, h0:h0 + nh], b.h[:, h0:h0 + nh]), [S["mx"][:]], [S["r"][:]])
                    P.tt("vector", hd[d][:, h0:h0 + nh, :], V(accv.ap[:, :, 0:128], accv.name),
                         S["r"][:, h0:h0 + nh].m(lambda a, nh=nh: a.rearrange("p (h o) -> p h o", o=1).to_broadcast([128, nh, 128])), ALU.mult)
                P.dma("sync", V(C.hd[d].h[r0 - LC: r0 - LC + 128, :].rearrange("p (h c) -> p h c", h=8), C.hd[d].name), hd[d][:])
            for gi, (h0, nh) in enumerate(HGRP):
                pu_bank = pUs[gu_ctr[0] % 2]; gu_ctr[0] += 1
                for h in range(h0, h0 + nh):
                    pu = pu_bank[:, (h - h0) * 170:(h - h0) * 170 + 129]
                    P.mm(pu, KT[:, h:h + 2, :].m(lambda a: a.rearrange("p a b -> p (a b)")), vw[d][:, h, :])
                cg = Cn[0:64, d, h0:h0 + nh]
                P.tt("vector", cg, cg, S["dec"][0:64, h0:h0 + nh].m(lambda a, nh=nh: a.rearrange("p (h o) -> p h o", o=1).to_broadcast([64, nh, 129])), ALU.mult)
                uv = V(pu_bank.h[0:64, 0:nh * 170].rearrange("p (h c) -> p h c", c=170)[:, :, 0:129], pu_bank.name)
                P.tt("vector", cg, cg, uv, ALU.add)
                P.cp("scalar", Cnb[0:64, d, h0:h0 + nh], cg)

        front(0)
        for n in range(68):
            if n + 1 < 68:
                front(n + 1)
            back(n)
        P.barrier(); P.flush()

TWO_PI = 6.283185307179586
I32 = mybir.dt.int32


def sincos(P, ang, s_out, c_out, tmp, tmpi, shape):
    for phase_off, dst in ((0.0, s_out), (0.25, c_out)):
        P.ts("vector", tmp, ang, 1.0 / TWO_PI, ALU.mult, phase_off, ALU.add)
        P.cp("vector", tmpi, tmp)
        P.cp("vector", tmp, tmpi)
        P.stt("vector", tmp, tmp, -TWO_PI, ang, ALU.mult, ALU.add)
        if phase_off != 0.0:
            P.ts("vector", tmp, tmp, TWO_PI * phase_off, ALU.add)
        P.ts("vector", tmp, tmp, 3.14159, ALU.min, -3.14159, ALU.max)
        P.act(dst, tmp, AF.Sin)


def phase4(P, C, K):
    with ExitStack() as ph:
        P.stack = ph
        Er = [P.sb("Er%d" % d, [128, 2048], F32) for d in range(2)]; Ei = [P.sb("Ei%d" % d, [128, 2048], F32) for d in range(2)]
        Fr = [P.sb("Fr%d" % d, [128, 16, 128], F32) for d in range(2)]; Fi = [P.sb("Fi%d" % d, [128, 16, 128], F32) for d in range(2)]
        Bbr = [P.sb("Bbr%d" % d, [128, 2048], BF16) for d in range(2)]; Bbi = [P.sb("Bbi%d" % d, [128, 2048], BF16) for d in range(2)]
        Cr = [P.sb("Cr%d" % d, [128, 2048], BF16) for d in range(2)]; Cin = [P.sb("Cin%d" % d, [128, 2048], BF16) for d in range(2)]
        Crn = [P.sb("Crn%d" % d, [128, 2048], BF16) for d in range(2)]
        ntribf = [P.sb("ntribf%d" % d, [128, 128], BF16) for d in range(2)]
        A8r = [P.sb("A8r%d" % d, [128, 16], F32) for d in range(2)]; A8i = [P.sb("A8i%d" % d, [128, 16], F32) for d in range(2)]
        tribf = [P.sb("tribf%d" % d, [128, 128], BF16) for d in range(2)]
        P.cp("vector", tribf[0][:], K.triF); P.cp("vector", tribf[1][:], K.triB)
        P.ts("vector", ntribf[0][:], K.triF, -1.0, ALU.mult); P.ts("vector", ntribf[1][:], K.triB, -1.0, ALU.mult)
        cc = [[(P.sb("cR%d%d" % (d, i), [128, 16], F32), P.sb("cI%d%d" % (d, i), [128, 16], F32)) for i in range(2)] for d in range(2)]
        for d in range(2):
            P.memset("vector", cc[d][0][0][:], 0.0); P.memset("vector", cc[d][0][1][:], 0.0)
        with ExitStack() as su:
            P.stack = su
            T = [P.sb("su%d" % i, [128, 2048], F32) for i in range(10)]
            TI = P.sb("sui", [128, 2048], I32)
            colp = P.sb("colp", [128, 3, 16], F32); lam = P.sb("lamc", [128, 2, 16], F32)
            for d in range(2):
                aRe, aIm, ldt, lRe, lIm, t5, t6, t7, t8, t9 = [t[:] for t in T]
                ti = TI[:]
                P.dma("sync", aRe, C.s5row[d, 0]); P.dma("sync", aIm, C.s5row[d, 1]); P.dma("sync", ldt, C.s5row[d, 2])
                P.act(ldt, ldt, AF.Exp)
                P.tt("vector", lRe, ldt, aRe, ALU.mult); P.tt("vector", lIm, ldt, aIm, ALU.mult)
                P.act(t5, lRe, AF.Exp)
                sincos(P, lIm, t6, t7, t8, ti, None)
                P.tt("vector", t7, t7, t5, ALU.mult)
                P.tt("vector", t6, t6, t5, ALU.mult)
                P.ts("vector", t7, t7, -1.0, ALU.add)
                P.tt("vector", t5, aRe, aRe, ALU.mult); P.tt("vector", t8, aIm, aIm, ALU.mult)
                P.tt("vector", t5, t5, t8, ALU.add)
                P.op("vector", lambda E, a=T[5]: E.reciprocal(a.h[:], a.h[:]), [t5], [t5])
                P.tt("vector", t8, t7, aRe, ALU.mult); P.tt("vector", t9, t6, aIm, ALU.mult)
                P.tt("vector", t8, t8, t9, ALU.add); P.tt("vector", t8, t8, t5, ALU.mult)
                P.tt("vector", t9, t6, aRe, ALU.mult); P.tt("vector", t6, t7, aIm, ALU.mult)
                P.tt("vector", t9, t9, t6, ALU.subtract); P.tt("vector", t9, t9, t5, ALU.mult)
                P.dma("sync", t5, C.bbd[d, 0]); P.dma("sync", t6, C.bbd[d, 1])
                P.tt("vector", t7, t8, t5, ALU.mult); P.tt("vector", aRe, t9, t6, ALU.mult)
                P.tt("vector", Bbr[d][:], t7, aRe, ALU.subtract)
                P.tt("vector", t7, t8, t6, ALU.mult); P.tt("vector", aRe, t9, t5, ALU.mult)
                P.tt("vector", Bbi[d][:], t7, aRe, ALU.add)
                P.dma("sync", t5, C.cbd[d, 0]); P.dma("sync", t6, C.cbd[d, 1])
                P.cp("vector", Cr[d][:], t5); P.ts("vector", Cin[d][:], t6, -1.0, ALU.mult)
                P.ts("vector", Crn[d][:], t5, -1.0, ALU.mult)
                idx = K.cst[:, 6, d:d + 1]
                P.ts("vector", t5, lRe, idx, ALU.mult); P.act(t5, t5, AF.Exp, scale=-1.0)
                P.ts("vector", t6, lIm, idx, ALU.mult)
                sincos(P, t6, t7, t8, t9, ti, None)
                P.tt("vector", Er[d][:], t5, t8, ALU.mult)
                P.tt("vector", t7, t5, t7, ALU.mult); P.ts("vector", Ei[d][:], t7, -1.0, ALU.mult)
                for i in range(3):
                    P.dma("sync", colp[:, i, :], C.s5col[d, i])
                P.act(colp[:, 2, :], colp[:, 2, :], AF.Exp)
                P.tt("vector", lam[:, 0, :], colp[:, 2, :], colp[:, 0, :], ALU.mult)
                P.tt("vector", lam[:, 1, :], colp[:, 2, :], colp[:, 1, :], ALU.mult)
                irow = K.cst[:, 4 + d, :]
                fR = t5.m(lambda a: a.rearrange("p (q t) -> p q t", t=128)); fI = t6.m(lambda a: a.rearrange("p (q t) -> p q t", t=128))
                for q in range(16):
                    P.ts("vector", V(fR.ap[:, q, :], fR.name), irow, lam[:, 0, q:q + 1], ALU.mult)
                    P.ts("vector", V(fI.ap[:, q, :], fI.name), irow, lam[:, 1, q:q + 1], ALU.mult)
                P.act(t5, t5, AF.Exp)
                sincos(P, t6, t7, t8, t9, ti, None)
                P.tt("vector", Fr[d][:].m(lambda a: a.rearrange("p q t -> p (q t)")), t5, t8, ALU.mult)
                P.tt("vector", Fi[d][:].m(lambda a: a.rearrange("p q t -> p (q t)")), t5, t7, ALU.mult)
                s16 = [V(t.h[:, 0:16], t.name) for t in T[5:10]]; s16i = V(TI.h[:, 0:16], TI.name)
                P.ts("vector", s16[0], lam[:, 0, :], 128.0, ALU.mult); P.act(s16[0], s16[0], AF.Exp)
                P.ts("vector", s16[1], lam[:, 1, :], 128.0, ALU.mult)
                sincos(P, s16[1], s16[2], s16[3], s16[4], s16i, None)
                P.tt("vector", A8r[d][:], s16[0], s16[3], ALU.mult); P.tt("vector", A8i[d][:], s16[0], s16[2], ALU.mult)
            P.barrier(); P.flush()
        P.stack = ph
        ut = [[P.sb("ut%d%d" % (d, i), [128, 4, 128], BF16) for i in range(2)] for d in range(2)]
        Z = [[P.sb("Z%d_%d" % (k, d), [128, 2048], BF16) for d in range(2)] for k in range(4)]
        Sp = [(P.sb("Spr%d" % i, [128, 4, 128], F32), P.sb("Spi%d" % i, [128, 4, 128], F32)) for i in range(2)]
        Hq = [[P.sb("Hq%d_%d" % (k, i), [128, 4, 128], BF16) for i in range(2)] for k in range(4)]
        sl = [P.sb("s5l%d" % i, [128, 4], F32) for i in range(4)]
        ysb = [P.sb("ysb%d" % d, [128, 4, 128], F32) for d in range(2)]
        pBr = P.ps("pBr", [128, 512]); pBi = P.ps("pBi", [128, 512])
        pSrs = [P.ps("pSr%d" % i, [128, 512]) for i in range(2)]; pSis = [P.ps("pSi%d" % i, [128, 512]) for i in range(2)]
        pYs = [P.ps("pY%d" % i, [128, 512]) for i in range(2)]
        fwd, bwd = chunk_orders()
        its = [(step, d) for step in range(34) for d in range(2)]

        def BZ(n):
            step, d = its[n]
            g = (fwd, bwd)[d][step]; r0 = g * 128
            u_ = ut[d][step % 2]
            P.dma("sync", u_[:], V(C.uT.h[:, r0:r0 + 128].rearrange("(j p) t -> p j t", p=128), C.uT.name))
            for j in range(4):
                P.mm(pBr[:], u_[:, j, :], Bbr[d][:, j * 512:(j + 1) * 512])
                P.mm(pBi[:], u_[:, j, :], Bbi[d][:, j * 512:(j + 1) * 512])
                hs = slice(j * 512, (j + 1) * 512)
                P.tt("vector", Z[0][d][:, hs], Er[d][:, hs], pBr[:], ALU.mult)
                P.tt("vector", Z[3][d][:, hs], Ei[d][:, hs], pBr[:], ALU.mult)
                P.tt("vector", Z[1][d][:, hs], Ei[d][:, hs], pBi[:], ALU.mult)
                P.tt("vector", Z[2][d][:, hs], Er[d][:, hs], pBi[:], ALU.mult)

        def cumsum(n, qg):
            step, d = its[n]
            b = (4 * n + qg) % 2
            pSr = pSrs[b]; pSi = pSis[b]
            for qq in range(4):
                q = 4 * qg + qq
                ps_r = pSr[:, qq * 128:(qq + 1) * 128]
                P.mm(ps_r, Z[0][d][:, q * 128:(q + 1) * 128], tribf[d][:], start=True, stop=False)
                P.mm(ps_r, Z[1][d][:, q * 128:(q + 1) * 128], ntribf[d][:], start=False, stop=True)
            for qq in range(4):
                q = 4 * qg + qq
                ps_i = pSi[:, qq * 128:(qq + 1) * 128]
                P.mm(ps_i, Z[2][d][:, q * 128:(q + 1) * 128], tribf[d][:], start=True, stop=False)
                P.mm(ps_i, Z[3][d][:, q * 128:(q + 1) * 128], tribf[d][:], start=False, stop=True)

        def evacH(n, qg):
            step, d = its[n]
            lat = (fwd, bwd)[d][step] >= 2
            b = (4 * n + qg) % 2
            pSr = pSrs[b]; pSi = pSis[b]
            cR, cI = cc[d][step % 2]; nR, nI = cc[d][(step + 1) % 2]
            last = 127 if d == 0 else 0
            spr, spi = Sp[b]
            qs = slice(4 * qg, 4 * qg + 4)
            for qq in range(4):
                q = 4 * qg + qq
                P.act(spr[:, qq, :], pSr[:, qq * 128:(qq + 1) * 128], AF.Identity, bias=cR[:, q:q + 1])
            for qq in range(4):
                q = 4 * qg + qq
                P.act(spi[:, qq, :], pSi[:, qq * 128:(qq + 1) * 128], AF.Identity, bias=cI[:, q:q + 1])
            P.tt("vector", sl[0][:], A8r[d][:, qs], spr[:, :, last], ALU.mult)
            P.tt("vector", sl[1][:], A8i[d][:, qs], spi[:, :, last], ALU.mult)
            P.tt("vector", nR[:, qs], sl[0][:], sl[1][:], ALU.subtract)
            P.tt("vector", sl[2][:], A8r[d][:, qs], spi[:, :, last], ALU.mult)
            P.tt("vector", sl[3][:], A8i[d][:, qs], spr[:, :, last], ALU.mult)
            P.tt("vector", nI[:, qs], sl[2][:], sl[3][:], ALU.add)
            if lat:
                H = [Hq[k][b] for k in range(4)]
                P.tt("vector", H[0][:], Fr[d][:, qs, :], spr[:], ALU.mult)
                P.tt("vector", H[1][:], Fi[d][:, qs, :], spi[:], ALU.mult)
                P.tt("vector", H[2][:], Fr[d][:, qs, :], spi[:], ALU.mult)
                P.tt("vector", H[3][:], Fi[d][:, qs, :], spr[:], ALU.mult)

        def Yq(n, qg):
            step, d = its[n]
            if (fwd, bwd)[d][step] < 2:
                return
            b = (4 * n + qg) % 2
            H = [Hq[k][b] for k in range(4)]
            py = pYs[n % 2][:, qg * 128:(qg + 1) * 128]
            for qq in range(4):
                q = 4 * qg + qq
                cs = slice(q * 128, (q + 1) * 128)
                P.mm(py, Cr[d][:, cs], H[0][:, qq, :], start=(qq == 0), stop=False)
                P.mm(py, Crn[d][:, cs], H[1][:, qq, :], start=False, stop=False)
                P.mm(py, Cin[d][:, cs], H[2][:, qq, :], start=False, stop=False)
                P.mm(py, Cin[d][:, cs], H[3][:, qq, :], start=False, stop=(qq == 3))

        BZ(0)
        for n in range(68):
            step, d = its[n]
            g = (fwd, bwd)[d][step]; lat = g >= 2; r0 = g * 128
            if n + 1 < 68:
                BZ(n + 1)
            cumsum(n, 0); evacH(n, 0)
            cumsum(n, 1); evacH(n, 1)
            Yq(n, 0)
            cumsum(n, 2); evacH(n, 2)
            Yq(n, 1)
            cumsum(n, 3); evacH(n, 3)
            Yq(n, 2); Yq(n, 3)
            if lat:
                P.cp("scalar", ysb[d][:], pYs[n % 2][:].m(lambda a: a.rearrange("p (j t) -> p j t", t=128)))
                P.dma("sync", V(C.yT[d].h[:, r0 - LC: r0 - LC + 128].rearrange("(j p) t -> p j t", p=128), C.yT[d].name), ysb[d][:])
        P.barrier(); P.flush()

def bcl(v, n, m):
    return v.m(lambda a: a.rearrange("p (h o) -> p h o", o=1).to_broadcast([128, n, m]))


def phase5(P, C, K):
    with ExitStack() as ph:
        P.stack = ph
        gmh = P.sb("gmh", [128, 1024], F32); P.dma("sync", gmh[:], C.gmh[:])
        hf = [P.sb("p5hf%d" % i, [128, 8, 128], F32) for i in range(2)]
        hb = [P.sb("p5hb%d" % i, [128, 8, 128], F32) for i in range(2)]
        og = [P.sb("p5og%d" % i, [128, 1024], BF16) for i in range(2)]
        sqs = [P.sb("p5sq%d" % i, [128, 8, 128], F32) for i in range(2)]; hns = [P.sb("p5hn%d" % i, [128, 1024], F32) for i in range(2)]
        st = [P.sb("p5st%d" % i, [128, 8, 128], BF16) for i in range(2)]
        s8s = [{n: P.sb("p5_%s%d" % (n, i), [128, 8], F32) for n in ["ss", "ms", "sq", "r"]} for i in range(2)]
        pT = P.ps("p5pT", [128, 1024])

        def s1(t):
            a, b_, o_ = hf[t % 2], hb[t % 2], og[t % 2]
            sq = sqs[t % 2]; s8 = s8s[t % 2]
            r0 = t * 128
            P.dma("sync", a[:], V(C.hd[0].h[r0:r0 + 128, :].rearrange("p (h c) -> p h c", h=8), C.hd[0].name))
            P.dma("sync", b_[:], V(C.hd[1].h[r0:r0 + 128, :].rearrange("p (h c) -> p h c", h=8), C.hd[1].name))
            P.dma("sync", o_[:], C.og[r0:r0 + 128, :])
            P.tt("vector", a[:], a[:], b_[:], ALU.add)
            for h in range(8):
                P.act(sq[:, h, :], a[:, h, :], AF.Square, accum=s8["ss"][:, h:h + 1])
            P.ts("vector", s8["ms"][:], s8["ss"][:], 1.0 / 128, ALU.mult, EPS, ALU.add)
            P.act(s8["sq"][:], s8["ms"][:], AF.Sqrt)
            P.op("vector", lambda E, s8=s8: E.reciprocal(s8["r"].h[:], s8["sq"].h[:]), [s8["sq"][:]], [s8["r"][:]])

        def s2(t):
            a, o_ = hf[t % 2], og[t % 2]
            hn = hns[t % 2]; s8 = s8s[t % 2]
            r0 = t * 128
            hv = hn[:].m(lambda x: x.rearrange("p (h c) -> p h c", h=8))
            P.tt("vector", hv, a[:], bcl(s8["r"][:], 8, 128), ALU.mult)
            P.tt("vector", hn[:], hn[:], gmh[:], ALU.mult)
            P.tt("vector", hn[:], hn[:], o_[:], ALU.mult)
            for j in range(8):
                P.tr(pT[:, j * 128:(j + 1) * 128], hn[:, j * 128:(j + 1) * 128], K.ident)
            s_ = st[t % 2]
            P.cp("scalar", s_[:], pT[:].m(lambda x: x.rearrange("p (j t) -> p j t", j=8)))
            P.dma("sync", V(C.hmT.h[:, r0:r0 + 128].rearrange("(j p) t -> p j t", p=128), C.hmT.name), s_[:])

        s1(0)
        for t in range(32):
            if t + 1 < 32:
                s1(t + 1)
            s2(t)
        P.barrier(); P.flush()


def phase6(P, C, K):
    with ExitStack() as ph:
        P.stack = ph
        wA = P.sb("wA", [128, 8, 1024], BF16); wG = P.sb("wG", [128, 4, 512], BF16)
        wB = P.sb("wB", [128, 4, 1024], BF16); wO = P.sb("wO", [128, 8, 1024], BF16)
        wR = P.sb("wR", [128, 8, 32], F32); bR = P.sb("bR", [128, 32], F32)
        bglu = P.sb("bglu", [128, 4], F32); s5d = P.sb("s5dt", [128, 4], F32)
        P.dma("sync", wA[:], V(C.wab.h.rearrange("(k p) n -> p k n", p=128), C.wab.name))
        P.dma("sync", wG[:], V(C.wglub.h.rearrange("(k p) n -> p k n", p=128), C.wglub.name))
        P.dma("sync", wB[:], V(C.wbb.h.rearrange("(k p) n -> p k n", p=128), C.wbb.name))
        P.dma("sync", wR[:], V(C.w_r.h.rearrange("(k p) n -> p k n", p=128), C.w_r.name))
        P.dma("sync", bR[:], C.b_r[:]); P.dma("sync", bglu[:], C.b_glu[:]); P.dma("sync", s5d[:], C.s5d[:])
        with ExitStack() as su:
            P.stack = su
            wo32 = P.sb("wo32", [128, 8, 1024], F32)
            P.dma("sync", wo32[:], V(C.w_o.h.rearrange("(k p) n -> p k n", p=128), C.w_o.name))
            P.tt("vector", wO[:], wo32[:], K.gt1bc[:].m(lambda a: a.rearrange("p (o n) -> p o n", o=1).to_broadcast([128, 8, 1024])), ALU.mult)
            P.barrier(); P.flush()
        P.stack = ph
        W = norm_work(P, "6")
        hmT = [P.sb("p6hm%d" % i, [128, 8, 512], BF16) for i in range(2)]
        yf = P.sb("p6yf", [128, 4, 512], F32); yb = P.sb("p6yb", [128, 4, 512], F32)
        uT = P.sb("p6u", [128, 4, 512], BF16); GT = [P.sb("p6G%d" % i, [128, 16, 512], BF16) for i in range(2)]
        x2 = P.sb("p6x2", [128, 4, 512], F32); sg = P.sb("p6sg", [128, 4, 512], F32)
        ysg = P.sb("p6ysg", [128, 4, 512], BF16); ys2 = P.sb("p6ys2", [128, 4, 512], BF16)
        sgz = P.sb("p6sgz", [128, 512], F32); m1 = P.sb("p6m1", [128, 512], F32); m2 = P.sb("p6m2", [128, 512], F32)
        mg = P.sb("p6mg", [128, 8, 512], BF16)
        xt = [P.sb("p6xt%d" % i, [128, 1024], F32) for i in range(3)]
        h2s = P.sb("p6h2s", [128, 8, 512], BF16); h2f = P.sb("p6h2f", [128, 8, 128], F32)
        lg = P.sb("p6lg", [128, 32], F32); m8 = P.sb("p6m8", [128, 8], F32); nm = P.sb("p6nm", [128, 1], F32)
        msk = P.sb("p6msk", [128, 32], F32); ex = P.sb("p6ex", [128, 32], F32); ssum = P.sb("p6ssum", [128, 1], F32)
        gts = P.sb("p6gts", [128, 4, 32], F32); gT = P.sb("p6gT", [32, 512], F32)
        pa = [P.ps("p6pa%d" % i, [128, 512]) for i in range(4)]
        pr = P.ps("p6pr", [128, 512]); pgT = P.ps("p6pgT", [128, 512])
        pai = 0
        for t in range(8):
            o0 = t * 512
            hm = hmT[t % 2]; G = GT[t % 2]
            P.dma("sync", hm[:], V(C.hmT.h[:, o0:o0 + 512].rearrange("(j p) t -> p j t", p=128), C.hmT.name))
            P.dma("sync", yf[:], V(C.yT[0].h[:, o0:o0 + 512].rearrange("(j p) t -> p j t", p=128), C.yT[0].name))
            P.dma("sync", yb[:], V(C.yT[1].h[:, o0:o0 + 512].rearrange("(j p) t -> p j t", p=128), C.yT[1].name))
            P.dma("sync", uT[:], V(C.uT.h[:, LC + o0:LC + o0 + 512].rearrange("(j p) t -> p j t", p=128), C.uT.name))
            P.dma("sync", G[:], V(C.GT.h[:, o0:o0 + 512].rearrange("(j p) t -> p j t", p=128), C.GT.name))
            P.tt("vector", yf[:], yf[:], yb[:], ALU.add)
            for j in range(4):
                P.stt("vector", yf[:, j, :], uT[:, j, :], s5d[:, j:j + 1], yf[:, j, :], ALU.mult, ALU.add)
            P.act(x2[:], yf[:], AF.Square)
            P.ts("vector", x2[:], x2[:], 0.044715, ALU.mult, 1.0, ALU.add)
            P.tt("vector", x2[:], x2[:], yf[:], ALU.mult)
            P.act(sg[:], x2[:], AF.Sigmoid, scale=1.5957691216057308)
            P.tt("vector", ysg[:], yf[:], sg[:], ALU.mult)
            for n in range(4):
                pb = pa[pai % 4]; pai += 1
                for k in range(4):
                    P.mm(pb[:], wG[:, k, n * 128:(n + 1) * 128], ysg[:, k, :], start=(k == 0), stop=(k == 3))
                P.act(sgz[:], pb[:], AF.Sigmoid, bias=bglu[:, n:n + 1])
                P.tt("vector", ys2[:, n, :], ysg[:, n, :], sgz[:], ALU.mult)
            for n in range(8):
                pA_ = pa[pai % 4]; pai += 1
                for k in range(8):
                    P.mm(pA_[:], wA[:, k, n * 128:(n + 1) * 128], hm[:, k, :], start=(k == 0), stop=(k == 7))
                pB_ = pa[pai % 4]; pai += 1
                for k in range(4):
                    P.mm(pB_[:], wB[:, k, n * 128:(n + 1) * 128], ys2[:, k, :], start=(k == 0), stop=(k == 3))
                P.tt("vector", m1[:], pA_[:], G[:, n, :], ALU.mult)
                P.tt("vector", m2[:], pB_[:], G[:, 8 + n, :], ALU.mult)
                P.tt("vector", mg[:, n, :], m1[:], m2[:], ALU.add)
            def stA(s):
                nonlocal pai
                x_ = xt[s % 3]
                r0 = o0 + s * 128
                P.dma("sync", x_[:], C.x[r0:r0 + 128, :])
                for hh in range(2):
                    pb = pa[pai % 4]; pai += 1
                    for k in range(8):
                        P.mm(pb[:], mg[:, k, s * 128:(s + 1) * 128], wO[:, k, hh * 512:(hh + 1) * 512], start=(k == 0), stop=(k == 7))
                    P.tt("vector", x_[:, hh * 512:(hh + 1) * 512], x_[:, hh * 512:(hh + 1) * 512], pb[:], ALU.add)
                P.dma("sync", C.x1[r0:r0 + 128, :], x_[:])

            def stB(s):
                x_ = xt[s % 3]
                norm_T(P, K, x_[:], K.S2[:], K.SH2[:], h2s[:, :, s * 128:(s + 1) * 128], W, need_f32=h2f[:])
                for k in range(8):
                    P.mm(pr[:, 0:32], h2f[:, k, :], wR[:, k, :], start=(k == 0), stop=(k == 7))
                P.tt("vector", lg[:], pr[:, 0:32], bR[:], ALU.add)
                P.op("vector", lambda E: E.max(m8.h[:], lg.h[:]), [lg[:]], [m8[:]])
                P.ts("vector", msk[:], lg[:], m8[:, 3:4], ALU.is_ge)
                P.ts("vector", nm[:], m8[:, 0:1], -1.0, ALU.mult)
                P.act(ex[:], lg[:], AF.Exp, bias=nm[:])
                P.tt("vector", ex[:], ex[:], msk[:], ALU.mult)
                P.op("vector", lambda E: E.reduce_sum(ssum.h[:], ex.h[:], AX.X), [ex[:]], [ssum[:]])
                P.op("vector", lambda E: E.reciprocal(ssum.h[:], ssum.h[:]), [ssum[:]], [ssum[:]])
                P.ts("vector", gts[:, s, :], ex[:], ssum[:], ALU.mult)
                P.tr(pgT[0:32, s * 128:(s + 1) * 128], gts[:, s, :], K.ident)

            stA(0); stA(1); stB(0); stA(2); stB(1); stA(3); stB(2); stB(3)
            P.cp("vector", gT[:], pgT[0:32, :])
            P.dma("sync", V(C.h2T.h[:, o0:o0 + 512].rearrange("(j p) t -> p j t", p=128), C.h2T.name), h2s[:])
            P.dma("sync", V(C.gates.h[o0:o0 + 512, :].rearrange("(s p) e -> p s e", p=128), C.gates.name), gts[:])
            P.dma("sync", C.gatesT[:, o0:o0 + 512], gT[:])
        P.barrier(); P.flush()


def phase7(P, C, K):
    with ExitStack() as ph:
        P.stack = ph
        bein = P.sb("bein", [128, 32, 16], F32); P.dma("sync", bein[:], V(C.b_ein.h.rearrange("p (e j) -> p e j", j=16), C.b_ein.name))
        bein1 = P.sb("bein1", [128, 32, 8], F32)
        P.ts("vector", bein1[:], bein[:, :, 8:16], 1.0, ALU.add)
        beo = P.sb("beo", [32, 1024], F32); P.dma("sync", beo[:], C.b_eout[:])
        gfin = P.sb("gfin", [128, 1024], F32); P.dma("sync", gfin[:], C.gfin[:])
        Win = [P.sb("Win%d" % i, [128, 8, 2048], BF16) for i in range(2)]
        Wout = [P.sb("Wout%d" % i, [128, 8, 1024], BF16) for i in range(2)]
        h2 = P.sb("p7h2", [128, 8, 1024], BF16)
        acc = P.sb("p7acc", [128, 8, 1024], F32)
        gt_ = P.sb("p7g", [128, 8, 32], F32); gTt = P.sb("p7gT", [32, 1024], F32)
        actT = [P.sb("p7act%d" % i, [128, 8, 512], BF16) for i in range(2)]
        tg = [P.sb("p7tg%d" % i, [128, 512], F32) for i in range(2)]; ts_ = [P.sb("p7ts%d" % i, [128, 512], F32) for i in range(2)]
        tl = [P.sb("p7tl%d" % i, [128, 512], F32) for i in range(2)]
        x1t = [P.sb("p7x1", [128, 1024], F32)] * 2
        fs = {n: P.sb("p7_" + n, [128, 1], F32) for n in ["ss", "ms", "sq", "r"]}
        pz = [P.ps("p7pz%d" % i, [128, 512]) for i in range(4)]
        po = [P.ps("p7po%d" % i, [128, 512]) for i in range(4)]
        zi = 0; oi = 0; ti = 0; wi = 0
        ck = ("cvt", "sw")
        for eng_ in ("sync", "gpsimd"):
            P._wait(eng_, ("D", ck))
        P.persist.discard(ck)
        rr = lambda a: a.rearrange("(k p) n -> p k n", p=128)
        for grp in range(4):
            g0 = grp * 1024
            P.dma("sync", h2[:], V(C.h2T.h[:, g0:g0 + 1024].rearrange("(j p) t -> p j t", p=128), C.h2T.name))
            P.dma("sync", gt_[:], V(C.gates.h[g0:g0 + 1024, :].rearrange("(s p) e -> p s e", p=128), C.gates.name))
            P.dma("sync", gTt[:], C.gatesT[:, g0:g0 + 1024])
            P.ts("vector", gt_[:], gt_[:], 1.0 / 1.702, ALU.mult)
            for s in range(8):
                for hh in range(2):
                    pb = po[oi % 4]; oi += 1
                    P.mm(pb[:], gTt[:, s * 128:(s + 1) * 128], beo[:, hh * 512:(hh + 1) * 512])
                    P.cp("scalar", acc[:, s, hh * 512:(hh + 1) * 512], pb[:])
            for e in range(NE):
                wi_, wo_ = Win[wi % 2], Wout[wi % 2]; wi += 1
                for kk in range(4):
                    P.dma("sync" if kk < 2 else "gpsimd", wi_.k(("w", kk))[:, 2 * kk:2 * kk + 2, :],
                          V(rr(C.weinb.h[e, kk * 256:(kk + 1) * 256, :]), C.weinb.name))
                for kk in range(2):
                    P.dma("sync" if kk == 0 else "gpsimd", wo_.k(("w", kk))[:, 4 * kk:4 * kk + 4, :],
                          V(rr(C.weoutb.h[e, kk * 512:(kk + 1) * 512, :]), C.weoutb.name))
                for tt in range(2):
                    aT = actT[ti % 2]; ti += 1
                    for jn in range(8):
                        pg_ = pz[zi % 4]; pl_ = pz[(zi + 1) % 4]; zi += 2
                        for k in range(8):
                            P.mm(pg_[:], wi_[:, k, jn * 128:(jn + 1) * 128], h2[:, k, tt * 512:(tt + 1) * 512], start=(k == 0), stop=(k == 7))
                        for k in range(8):
                            P.mm(pl_[:], wi_[:, k, 1024 + jn * 128:1024 + (jn + 1) * 128], h2[:, k, tt * 512:(tt + 1) * 512], start=(k == 0), stop=(k == 7))
                        b = jn % 2
                        P.ts("vector", tg[b][:], pg_[:], bein[:, e, jn:jn + 1], ALU.add, 7.0, ALU.min)
                        P.act(ts_[b][:], tg[b][:], AF.Silu, scale=1.702)
                        P.ts("vector", tl[b][:], pl_[:], bein1[:, e, jn:jn + 1], ALU.add, -6.0, ALU.max)
                        P.stt("vector", aT[:, jn, :], tl[b][:], 8.0, ts_[b][:], ALU.min, ALU.mult)
                    for s in range(4):
                        sub = tt * 4 + s
                        for hh in range(2):
                            pb = po[oi % 4]; oi += 1
                            for k in range(8):
                                P.mm(pb[:], aT[:, k, s * 128:(s + 1) * 128], wo_[:, k, hh * 512:(hh + 1) * 512], start=(k == 0), stop=(k == 7))
                            av = V(acc.h[:, sub, hh * 512:(hh + 1) * 512], acc.name, (sub, hh))
                            P.stt("vector", av, pb[:], gt_[:, sub, e:e + 1], av, ALU.mult, ALU.add)
            for s in range(8):
                r0 = g0 + s * 128
                x_ = x1t[s % 2]
                P.dma("sync", x_[:], C.x1[r0:r0 + 128, :])
                P.tt("vector", acc[:, s, :], acc[:, s, :], K.gt2bc[:], ALU.mult)
                P.tt("vector", x_[:], x_[:], acc[:, s, :], ALU.add)
                P.act(actT[0][:, 0:2, :].m(lambda a: a.rearrange("p a b -> p (a b)")), x_[:], AF.Square, accum=fs["ss"][:])
                P.ts("vector", fs["ms"][:], fs["ss"][:], 1.0 / D, ALU.mult, EPS, ALU.add)
                P.act(fs["sq"][:], fs["ms"][:], AF.Sqrt)
                P.op("vector", lambda E: E.reciprocal(fs["r"].h[:], fs["sq"].h[:]), [fs["sq"][:]], [fs["r"][:]])
                P.act(x_[:], x_[:], AF.Identity, scale=fs["r"][:])
                P.tt("vector", x_[:], x_[:], gfin[:], ALU.mult)
                P.dma("sync", C.out[r0:r0 + 128, :], x_[:])
        P.barrier(); P.flush()

PHASES = 99
DBG = ()


def build_program(phases=99, dbg=(), only=None):
    nc = bass.Bass("TRN2", target_bir_lowering=False)
    with ExitStack() as outer:
        P = Prog(nc, outer)
        C = declare_io(P, dbg)
        K = phase0(P, C)
        if phases >= 1 and (only is None or 1 in only):
            phase1(P, C, K)
        if phases >= 2 and (only is None or 2 in only):
            phase2(P, C, K)
        if phases >= 3 and (only is None or 3 in only):
            phase3(P, C, K)
        if phases >= 4 and (only is None or 4 in only):
            phase4(P, C, K)
        if phases >= 5 and (only is None or 5 in only):
            phase5(P, C, K)
        if phases >= 6 and (only is None or 6 in only):
            phase6(P, C, K)
        if phases >= 7 and (only is None or 7 in only):
            phase7(P, C, K)
        P.stack = outer
        P.finish([C.out[:]])
        P.flush()
        print("instr counts", P.cnt, "waits", P.nwaits, "dma sems", {k: (len(v), max([x[1] for x in v] + [0])) for k, v in P.free_d.items()})
    return nc


_NC_CACHE = {}


def kernel(**inputs):
    inp = {k: np.asarray(v) for k, v in inputs.items()}
    key = (PHASES, DBG)
    if key not in _NC_CACHE:
        _NC_CACHE[key] = build_program(PHASES, DBG)
    nc = _NC_CACHE[key]
    in_maps = [host_prep(inp, b) for b in range(8)]
    res = run_bass_kernel_spmd(nc, in_maps, core_ids=list(range(8)))
    kernel.last = res
    out = np.stack([np.asarray(res.results[b]["dr_out"]) for b in range(8)], axis=0)
    return out.astype(np.float32)
```

```python
import numpy as np
import concourse.bass as bass
import concourse.mybir as mybir
from concourse.bass_utils import run_bass_kernel_spmd

F32 = mybir.dt.float32
BF16 = mybir.dt.bfloat16
ALU = mybir.AluOpType
AF = mybir.ActivationFunctionType
AX = mybir.AxisListType

ENGS = ["tensor", "vector", "scalar", "gpsimd", "sync"]
EPOCH = 16000


class V:
    __slots__ = ("ap", "name", "key")

    def __init__(self, ap, name, key=None):
        self.ap = ap
        self.name = name
        self.key = key

    def m(self, fn):
        return V(fn(self.ap), self.name, self.key)


class _TK:
    def __init__(self, t, key):
        self.t = t
        self.key = key

    def __getitem__(self, idx):
        return V(self.t.h[idx], self.t.name, self.key)


class T:
    def __init__(self, h, name):
        self.h = h
        self.name = name

    def __getitem__(self, idx):
        return V(self.h[idx], self.name, None)

    def k(self, key):
        return _TK(self, key)


class Prog:
    def __init__(self, nc, stack):
        self.nc = nc
        self.stack = stack
        self.semstack = stack
        self.items = {e: [] for e in ENGS}
        self.cnt = {e: 0 for e in ENGS}
        self.esems = {e: [] for e in ENGS}
        self.state = {}
        self.waited = {e: {} for e in ENGS}
        self.dsem = {}
        self.ntiles = 0
        self.nwaits = 0
        self.persist = set()

    def sb(self, name, shape, dt):
        h = self.stack.enter_context(self.nc.sbuf_tensor(name, list(shape), dt))
        return T(h, name)

    def ps(self, name, shape, dt=F32):
        h = self.stack.enter_context(self.nc.psum_tensor(name, list(shape), dt))
        return T(h, name)

    def dram(self, name, shape, dt, kind="Internal"):
        h = self.nc.dram_tensor(name, list(shape), dt, kind=kind)
        return T(h.ap() if hasattr(h, "ap") else h, name)

    def _esem(self, e, ep):
        while len(self.esems[e]) <= ep:
            s = self.semstack.enter_context(self.nc.semaphore("s_%s_%d" % (e, len(self.esems[e]))))
            self.esems[e].append(s)
        return self.esems[e][ep]

    def _dsem(self, key):
        if key not in self.dsem:
            if not hasattr(self, "free_d"):
                self.free_d = {"sw": [], "hw": []}
            fd = self.free_d[key[1]]
            if fd:
                fd.sort(key=lambda x: x[1])
                self.dsem[key] = fd.pop(0)
            else:
                self.ndsem = getattr(self, "ndsem", 0) + 1
                s = self.semstack.enter_context(self.nc.semaphore("d_%d" % self.ndsem))
                self.dsem[key] = [s, 0]
        return self.dsem[key]

    def _states(self, name, key):
        d = self.state.setdefault(name, {})
        if key is None:
            if None not in d:
                d[None] = [None, {}]
            return list(d.values())
        if key not in d:
            d[key] = [None, {}]
        out = [d[key]]
        if None in d:
            out.append(d[None])
        return out

    def _wait(self, eng, dep):
        if dep is None:
            return
        if dep[0] == "E":
            _, e2, idx = dep
            if e2 == eng and eng == "tensor":
                return
            ep = (idx - 1) // EPOCH
            val = idx - ep * EPOCH
            sem = self._esem(e2, ep)
            sk = ("E", e2, ep)
        else:
            _, dk = dep
            sem, val = self.dsem[dk]
            sk = ("D", id(sem))
        w = self.waited[eng]
        if w.get(sk, 0) >= val:
            return
        w[sk] = val
        self.nwaits += 1
        self.items[eng].append(lambda E, sem=sem, val=val: E.wait_ge(sem, val))

    def _deps(self, eng, reads, writes):
        for v in reads:
            for st in self._states(v.name, v.key):
                self._wait(eng, st[0])
        for v in writes:
            for st in self._states(v.name, v.key):
                self._wait(eng, st[0])
                for r in st[1].values():
                    self._wait(eng, r)

    def _record(self, dep, reads, writes):
        for v in reads:
            d = self.state.setdefault(v.name, {})
            if v.key not in d:
                d[v.key] = [None, {}]
            d[v.key][1][dep[:2]] = dep
        for v in writes:
            d = self.state.setdefault(v.name, {})
            if v.key is None:
                for k in list(d.keys()):
                    d[k] = [dep, {}]
                d[None] = [dep, {}]
            else:
                d[v.key] = [dep, {}]

    def op(self, eng, fn, reads, writes):
        reads = [r for r in reads if isinstance(r, V)]
        writes = [w for w in writes if isinstance(w, V)]
        self._deps(eng, reads, writes)
        self.cnt[eng] += 1
        idx = self.cnt[eng]
        ep = (idx - 1) // EPOCH
        sem = self._esem(eng, ep)
        self.items[eng].append(lambda E, fn=fn, sem=sem: fn(E).then_inc(sem, 1))
        self._record(("E", eng, idx), reads, writes)

    def dma(self, eng, out, in_, semkey=None, **kw):
        if semkey is None:
            semkey = out.name if not out.name.startswith("dr_") else in_.name
        if eng in ("scalar", "vector"):
            semkey = "%s_%s" % (semkey, eng)
        semkey = (semkey, "sw" if eng == "gpsimd" else "hw")
        self._deps(eng, [in_], [out])
        ds = self._dsem(semkey)
        ds[1] += 16
        assert ds[1] < 60000, semkey
        sem = ds[0]
        o, i = out.ap, in_.ap
        self.items[eng].append(lambda E, o=o, i=i, sem=sem, kw=kw: E.dma_start(out=o, in_=i, **kw).then_inc(sem, 16))
        self._record(("D", semkey), [in_], [out])

    def mm(self, out, lhsT, rhs, start=True, stop=True):
        self.op("tensor", lambda E: E.matmul(out.ap, lhsT.ap, rhs.ap, start=start, stop=stop),
                [lhsT, rhs] + ([] if start else [out]), [out])

    def tr(self, out, in_, ident):
        self.op("tensor", lambda E: E.transpose(out.ap, in_.ap, ident.ap), [in_, ident], [out])

    def act(self, out, in_, func, bias=0.0, scale=1.0, accum=None, eng="scalar"):
        b = bias.ap if isinstance(bias, V) else bias
        s = scale.ap if isinstance(scale, V) else scale
        kw = {}
        if accum is not None:
            kw["accum_out"] = accum.ap
        self.op("scalar", lambda E: E.activation(out.ap, in_.ap, func, bias=b, scale=s, **kw),
                [in_, bias, scale], [out] + ([accum] if accum is not None else []))

    def tt(self, eng, out, a, b, op):
        self.op(eng, lambda E: E.tensor_tensor(out.ap, a.ap, b.ap, op), [a, b], [out])

    def ts(self, eng, out, a, s1, op0, s2=None, op1=None, accum=None):
        x1 = s1.ap if isinstance(s1, V) else s1
        x2 = s2.ap if isinstance(s2, V) else s2
        kw = {}
        if op1 is not None:
            kw["op1"] = op1
        if accum is not None:
            kw["accum_out"] = accum.ap
        self.op(eng, lambda E: E.tensor_scalar(out.ap, a.ap, x1, x2, op0, **kw), [a, s1, s2],
                [out] + ([accum] if accum is not None else []))

    def stt(self, eng, out, a, s, b, op0, op1):
        x = s.ap if isinstance(s, V) else s
        self.op(eng, lambda E: E.scalar_tensor_tensor(out.ap, a.ap, x, b.ap, op0, op1), [a, s, b], [out])

    def cp(self, eng, out, in_):
        if eng == "scalar":
            self.op(eng, lambda E: E.copy(out.ap, in_.ap), [in_], [out])
        else:
            self.op(eng, lambda E: E.tensor_copy(out.ap, in_.ap), [in_], [out])

    def memset(self, eng, out, val):
        self.op(eng, lambda E: E.memset(out.ap, val), [], [out])

    def finish(self, outs):
        for v in outs:
            for st in self._states(v.name, v.key):
                self._wait("sync", st[0])
        for e in ENGS:
            if self.cnt[e] > 0:
                self._wait("sync", ("E", e, self.cnt[e]))
        for dk in list(self.dsem.keys()):
            self._wait("sync", ("D", dk))

    def barrier(self):
        for e in ENGS:
            for e2 in ENGS:
                if self.cnt[e2] > 0:
                    self._wait(e, ("E", e2, self.cnt[e2]))
            for dk in list(self.dsem.keys()):
                if dk in self.persist:
                    continue
                self._wait(e, ("D", dk))
        self.state = {}
        if not hasattr(self, "free_d"):
            self.free_d = {"sw": [], "hw": []}
        for k, v in self.dsem.items():
            if k in self.persist:
                continue
            self.free_d[k[1]].append(v)
        self.dsem = {k: v for k, v in self.dsem.items() if k in self.persist}

    def flush(self):
        self.build()
        self.items = {e: [] for e in ENGS}

    def build(self):
        nc = self.nc
        with nc.Block() as block:
            for e in ENGS:
                items = self.items[e]
                if not items:
                    continue

                def body(E, items=items):
                    for it in items:
                        it(E)
                getattr(block, e)(body)
from contextlib import ExitStack
import ml_dtypes

D = 1024
L = 4096
LC = 256
LT = L + LC
NE = 32
OFF_QK, OFF_V, OFF_IF, OFF_U, OFF_O, OFF_G, IN_COLS = 0, 1024, 2048, 2080, 2592, 3616, 5664
EPS = 1e-6


def host_prep(inp, b):
    f = np.float32
    A = lambda a: np.ascontiguousarray(a, dtype=f)
    m = {}
    m["dr_x"] = A(inp["x"][b])
    m["dr_ctx"] = A(inp["ctx"][b])
    cc = np.stack([inp["c"][b].reshape(8, 128).T, inp["c_ctx"].reshape(8, 128).T], axis=-1)
    m["dr_cc"] = A(cc)
    m["dr_w_ada"] = A(inp["w_ada"][0])
    m["dr_b_ada"] = A(inp["b_ada"][0].reshape(48, 128).T)
    m["dr_g1"] = A(inp["g_norm1"][0].reshape(8, 128).T)
    m["dr_g2"] = A(inp["g_norm2"][0].reshape(8, 128).T)
    m["dr_w_in"] = A(inp["w_in"][0])
    m["dr_wconv"] = A(inp["w_conv_qk"][0].reshape(9, 8, 128).transpose(2, 1, 0))
    m["dr_bif"] = A(np.broadcast_to(inp["b_ifgate"][0].reshape(1, 32), (128, 32)))
    m["dr_gmh"] = A(np.broadcast_to(inp["g_mh"][0].reshape(1, 1024), (128, 1024)))
    m["dr_w_a"] = A(inp["w_branch_m"][0])
    row = np.zeros((2, 3, 128, 2048), f)
    col = np.zeros((2, 3, 128, 16), f)
    for d in range(2):
        ldt = np.repeat(inp["s5_log_dt"][0, d], 64)
        for i, arr in enumerate([inp["s5_a_re"][0, d].reshape(-1), inp["s5_a_im"][0, d].reshape(-1), ldt]):
            row[d, i] = np.broadcast_to(arr.reshape(1, 2048), (128, 2048))
            col[d, i] = arr.reshape(16, 128).T
    m["dr_s5row"] = row
    m["dr_s5col"] = col
    bbd = np.zeros((2, 2, 128, 4, 512), f)
    cbd = np.zeros((2, 2, 128, 16, 128), f)
    for d in range(2):
        for ri, (bsrc, csrc) in enumerate([(inp["s5_b_re"], inp["s5_c_re"]), (inp["s5_b_im"], inp["s5_c_im"])]):
            for g in range(32):
                j, gl = divmod(g, 8)
                bbd[d, ri, gl * 16:(gl + 1) * 16, j, gl * 64:(gl + 1) * 64] = bsrc[0, d, g].T
                q, g2 = divmod(g, 2)
                cbd[d, ri, g2 * 64:(g2 + 1) * 64, q, (q % 4) * 32 + g2 * 16:(q % 4) * 32 + g2 * 16 + 16] = csrc[0, d, g].T
    m["dr_bbd"] = bbd.reshape(2, 2, 128, 2048)
    m["dr_cbd"] = cbd.reshape(2, 2, 128, 2048)
    m["dr_s5d"] = A(inp["s5_d"][0].reshape(4, 128).T)
    m["dr_w_glu"] = A(inp["w_glu"][0])
    m["dr_b_glu"] = A(inp["b_glu"][0].reshape(4, 128).T)
    m["dr_w_b"] = A(inp["w_branch_s"][0])
    m["dr_b_gate"] = A(inp["b_merge_gate"][0].reshape(16, 128).T)
    m["dr_w_o"] = A(inp["w_o"][0])
    m["dr_w_r"] = A(inp["w_router"][0])
    m["dr_b_r"] = A(np.broadcast_to(inp["b_router"][0].reshape(1, 32), (128, 32)))
    m["dr_w_ein"] = A(inp["w_e_in"][0])
    m["dr_b_ein"] = A(inp["b_e_in"][0].reshape(32, 16, 128).transpose(2, 0, 1).reshape(128, 512))
    m["dr_w_eout"] = A(inp["w_e_out"][0])
    m["dr_b_eout"] = A(inp["b_e_out"][0])
    m["dr_gfin"] = A(np.broadcast_to(inp["g_final"].reshape(1, 1024), (128, 1024)))
    cst = np.zeros((128, 7, 128), f)
    ii = np.arange(128)
    cst[:, 0] = np.eye(128)
    cst[:, 1] = (ii[:, None] <= ii[None, :])
    cst[:, 2] = (ii[:, None] >= ii[None, :])
    cst[:, 3] = 1.0
    cst[:, 4] = ii[None, :]
    cst[:, 5] = 127 - ii[None, :]
    cst[:, 6, 0] = ii
    cst[:, 6, 1] = 127 - ii
    m["dr_cst"] = cst
    return m

class Ctx:
    pass


def declare_io(P, dbg):
    C = Ctx()
    def din(name, shape, dt=F32):
        return P.dram("dr_" + name, shape, dt, kind="ExternalInput")
    C.x = din("x", [L, D]); C.ctx = din("ctx", [LC, D]); C.cc = din("cc", [128, 8, 2])
    C.w_ada = din("w_ada", [D, 6144]); C.b_ada = din("b_ada", [128, 48])
    C.g1 = din("g1", [128, 8]); C.g2 = din("g2", [128, 8]); C.w_in = din("w_in", [D, IN_COLS])
    C.wconv = din("wconv", [128, 8, 9]); C.bif = din("bif", [128, 32]); C.gmh = din("gmh", [128, 1024])
    C.w_a = din("w_a", [D, D]); C.s5row = din("s5row", [2, 3, 128, 2048]); C.s5col = din("s5col", [2, 3, 128, 16])
    C.bbd = din("bbd", [2, 2, 128, 2048]); C.cbd = din("cbd", [2, 2, 128, 2048]); C.s5d = din("s5d", [128, 4])
    C.w_glu = din("w_glu", [512, 512]); C.b_glu = din("b_glu", [128, 4]); C.w_b = din("w_b", [512, D])
    C.b_gate = din("b_gate", [128, 16]); C.w_o = din("w_o", [D, D]); C.w_r = din("w_r", [D, 32])
    C.b_r = din("b_r", [128, 32]); C.w_ein = din("w_ein", [NE, D, 2048]); C.b_ein = din("b_ein", [128, 512])
    C.w_eout = din("w_eout", [NE, D, D]); C.b_eout = din("b_eout", [NE, D]); C.gfin = din("gfin", [128, 1024])
    C.cst = din("cst", [128, 7, 128])
    C.out = P.dram("dr_out", [L, D], F32, kind="ExternalOutput")
    def scr(name, shape, dt):
        kind = "ExternalOutput" if name in dbg else "Internal"
        return P.dram("dr_s_" + name, shape, dt, kind=kind)
    C.qkpre = scr("qkpre", [1024, LT], BF16)
    C.v = scr("v", [LT, 1024], BF16)
    C.gl = scr("gl", [LT, 32], F32)
    C.uT = scr("uT", [512, LT], BF16)
    C.GT = scr("GT", [2048, L], BF16)
    C.og = scr("og", [L, 1024], BF16)
    C.qT = scr("qT", [512, LT], BF16)
    C.kT = scr("kT", [512, LT], BF16)
    C.ktok = scr("ktok", [LT, 512], BF16)
    C.hd = [scr("hf", [L, 1024], F32), scr("hb", [L, 1024], F32)]
    C.hmT = scr("hmT", [1024, L], BF16)
    C.yT = [scr("yTf", [512, L], F32), scr("yTb", [512, L], F32)]
    C.x1 = scr("x1", [L, D], F32)
    C.h2T = scr("h2T", [1024, L], BF16)
    C.gates = scr("gates", [L, 32], F32)
    C.gatesT = scr("gatesT", [32, L], F32)
    C.modT = scr("modT", [128, 96], F32)
    C.weinb = scr("weinb", [NE, D, 2048], BF16)
    C.weoutb = scr("weoutb", [NE, D, D], BF16)
    return C


def bc(v, shape, axis_pat):
    return v.m(lambda a: a.rearrange(axis_pat, o=1).to_broadcast(shape))


def phase0(P, C):
    K = Ctx()
    K.cst = P.sb("cst", [128, 7, 128], F32)
    P.dma("sync", K.cst[:], C.cst[:])
    K.ident = K.cst[:, 0, :]; K.triF = K.cst[:, 1, :]; K.triB = K.cst[:, 2, :]; K.ones = K.cst[:, 3, :]
    K.S1 = P.sb("S1", [128, 8], F32); K.SH1 = P.sb("SH1", [128, 8], F32)
    K.S1c = P.sb("S1c", [128, 8], F32); K.SH1c = P.sb("SH1c", [128, 8], F32)
    K.S2 = P.sb("S2", [128, 8], F32); K.SH2 = P.sb("SH2", [128, 8], F32)
    K.gt1bc = P.sb("gt1bc", [128, 1024], F32); K.gt2bc = P.sb("gt2bc", [128, 1024], F32)
    with ExitStack() as ph:
        P.stack = ph
        cc = P.sb("cc", [128, 8, 2], F32); sc = P.sb("sc", [128, 8, 2], F32)
        bada = P.sb("bada", [128, 48], F32); g1 = P.sb("g1", [128, 8], F32); g2 = P.sb("g2", [128, 8], F32)
        modT = P.sb("modT", [128, 48, 2], F32); tmp8 = P.sb("tmp8", [128, 8], F32)
        gt = P.sb("gt", [128, 16], F32)
        wa = [P.sb("wa%d" % i, [128, 8, 512], F32) for i in range(2)]
        diag = [P.sb("diag%d" % i, [128, 128], F32) for i in range(2)]
        pm = P.ps("pm", [128, 512]); pg = P.ps("pg", [128, 1024])
        P.dma("sync", cc[:], C.cc[:]); P.dma("sync", bada[:], C.b_ada[:])
        P.dma("sync", g1[:], C.g1[:]); P.dma("sync", g2[:], C.g2[:])
        P.act(sc[:], cc[:], AF.Silu)
        wv = C.w_ada[:].m(lambda a: a.rearrange("(k p) n -> p k n", p=128))
        for i in range(12):
            w = wa[i % 2]
            P.dma("sync" if i % 2 == 0 else "gpsimd", w[:], V(wv.ap[:, :, i * 512:(i + 1) * 512], wv.name))
            for mI in range(4):
                nn = 4 * i + mI
                for k in range(8):
                    P.mm(pm[:, nn * 2:nn * 2 + 2], w[:, k, mI * 128:(mI + 1) * 128], sc[:, k, :], start=(k == 0), stop=(k == 7))
        P.tt("vector", modT[:], pm[:, 0:96].m(lambda a: a.rearrange("p (n t) -> p n t", t=2)),
             bada[:].m(lambda a: a.rearrange("p (n o) -> p n o", o=1).to_broadcast([128, 48, 2])), ALU.add)
        P.cp("vector", K.SH1[:], modT[:, 0:8, 0]); P.cp("vector", K.SH1c[:], modT[:, 0:8, 1])
        P.cp("vector", K.SH2[:], modT[:, 24:32, 0])
        P.ts("vector", tmp8[:], modT[:, 8:16, 0], 1.0, ALU.add); P.tt("vector", K.S1[:], tmp8[:], g1[:], ALU.mult)
        P.ts("vector", tmp8[:], modT[:, 8:16, 1], 1.0, ALU.add); P.tt("vector", K.S1c[:], tmp8[:], g1[:], ALU.mult)
        P.ts("vector", tmp8[:], modT[:, 32:40, 0], 1.0, ALU.add); P.tt("vector", K.S2[:], tmp8[:], g2[:], ALU.mult)
        P.cp("vector", gt[:, 0:8], modT[:, 16:24, 0]); P.cp("vector", gt[:, 8:16], modT[:, 40:48, 0])
        for j in range(16):
            dg = diag[j % 2]
            P.ts("vector", dg[:], K.ident, gt[:, j:j + 1], ALU.mult)
            P.mm(pg[:, (j % 8) * 128:(j % 8 + 1) * 128], K.ones, dg[:])
            if j == 7:
                P.cp("vector", K.gt1bc[:], pg[:])
            if j == 15:
                P.cp("vector", K.gt2bc[:], pg[:])
        P.dma("sync", C.modT[:], modT[:].m(lambda a: a.rearrange("p n t -> p (n t)")))
        P.barrier(); P.flush()
    return K


def norm_T(P, K, xt, S, SH, hT_dst, W, need_f32=None):
    P.act(W["junk"][:], xt, AF.Square, accum=W["ss"][:])
    P.ts("vector", W["ms"][:], W["ss"][:], 1.0 / D, ALU.mult, EPS, ALU.add)
    P.act(W["sq"][:], W["ms"][:], AF.Sqrt)
    P.op("vector", lambda E: E.reciprocal(W["rstd"].h[:], W["sq"].h[:]), [W["sq"][:]], [W["rstd"][:]])
    P.act(W["xn"][:], xt, AF.Identity, scale=W["rstd"][:])
    for j in range(8):
        P.tr(W["psT"][:, j * 128:(j + 1) * 128], W["xn"][:, j * 128:(j + 1) * 128], K.ident)
    for j in range(8):
        pj = W["psT"][:, j * 128:(j + 1) * 128]
        sj = V(S.ap[:, j:j + 1], S.name, S.key); bj = V(SH.ap[:, j:j + 1], SH.name, SH.key)
        if need_f32 is not None:
            nf = V(need_f32.ap[:, j, :], need_f32.name, need_f32.key)
            P.act(nf, pj, AF.Identity, bias=bj, scale=sj)
        else:
            P.act(V(hT_dst.ap[:, j, :], hT_dst.name, hT_dst.key), pj, AF.Identity, bias=bj, scale=sj)
    if need_f32 is not None:
        P.cp("vector", hT_dst, need_f32)


def norm_work(P, sfx=""):
    W = {}
    W["junk"] = P.sb("nw_junk" + sfx, [128, 1024], BF16)
    W["xn"] = P.sb("nw_xn" + sfx, [128, 1024], F32)
    W["tm"] = P.sb("nw_tm" + sfx, [128, 8, 128], F32)
    for n in ["ss", "ms", "sq", "rstd"]:
        W[n] = P.sb("nw_" + n + sfx, [128, 1], F32)
    W["psT"] = P.ps("nw_psT" + sfx, [128, 1024])
    return W


def phase1(P, C, K):
    with ExitStack() as ph:
        P.stack = ph
        win = P.sb("win", [128, 8, IN_COLS], BF16)
        for k in range(8):
            P.dma("gpsimd", win[:, k, :], C.w_in[k * 128:(k + 1) * 128, :])
        rr = lambda a: a.rearrange("(k p) n -> p k n", p=128)
        for e in range(NE):
            for kk in range(4):
                P.dma("gpsimd", V(rr(C.weinb.h[e, kk * 256:(kk + 1) * 256, :]), C.weinb.name),
                      V(rr(C.w_ein.h[e, kk * 256:(kk + 1) * 256, :]), C.w_ein.name), semkey="cvt")
            for kk in range(2):
                P.dma("gpsimd", V(rr(C.weoutb.h[e, kk * 512:(kk + 1) * 512, :]), C.weoutb.name),
                      V(rr(C.w_eout.h[e, kk * 512:(kk + 1) * 512, :]), C.w_eout.name), semkey="cvt")
        P.persist.add(("cvt", "sw"))
        bif = P.sb("bif", [128, 32], F32); P.dma("sync", bif[:], C.bif[:])
        bgate = P.sb("bgate", [128, 16], F32); P.dma("sync", bgate[:], C.b_gate[:])
        W = norm_work(P)
        xt = [P.sb("xt%d" % i, [128, 1024], F32) for i in range(2)]
        hT = [P.sb("hT%d" % i, [128, 8, 512], BF16) for i in range(2)]
        pA = [P.ps("pA%d" % i, [128, 512]) for i in range(4)]
        st_qk = [P.sb("st_qk%d" % i, [128, 8, 512], BF16) for i in range(2)]
        st_u = [P.sb("st_u%d" % i, [128, 4, 512], BF16) for i in range(2)]
        st_G = [P.sb("st_G%d" % i, [128, 8, 512], BF16) for i in range(2)]
        st_v = [P.sb("st_v%d" % i, [128, 1024], BF16) for i in range(2)]
        st_o = [P.sb("st_o%d" % i, [128, 1024], BF16) for i in range(2)]
        st_g = [P.sb("st_g%d" % i, [128, 4, 32], F32) for i in range(2)]
        gw = {n: P.sb("gw_" + n, [128, 32], F32) for n in ["g", "e", "sp"]}
        tiles = [(True, 0, 256, 0)] + [(False, i * 512, 512, LC + i * 512) for i in range(8)]
        pai = 0
        for ti, (isc, off, nt, lto) in enumerate(tiles):
            par = ti % 2
            nsub = nt // 128
            src = C.ctx if isc else C.x
            h = hT[par]
            for s in range(nsub):
                x_ = xt[(ti * 4 + s) % 2]
                P.dma("sync", x_[:], src[off + s * 128: off + (s + 1) * 128, :])
                norm_T(P, K, x_[:], K.S1c[:] if isc else K.S1[:], K.SH1c[:] if isc else K.SH1[:],
                       h[:, :, s * 128:(s + 1) * 128], W)
            def fm(col0, ncol, stage, func, bias=None, evac="scalar"):
                nonlocal pai
                for m in range(ncol):
                    pb = pA[pai % 4]; pai += 1
                    for k in range(8):
                        P.mm(pb[:, 0:nt], win[:, k, col0 + m * 128: col0 + (m + 1) * 128], h[:, k, 0:nt], start=(k == 0), stop=(k == 7))
                    if func is None:
                        if m % 2 == 0:
                            P.cp("scalar", stage[:, m, 0:nt], pb[:, 0:nt])
                        else:
                            P.cp("vector", stage[:, m, 0:nt], pb[:, 0:nt])
                    else:
                        P.act(stage[:, m, 0:nt], pb[:, 0:nt], func, bias=V(bias.ap[:, m:m + 1], bias.name) if isinstance(bias, V) else bias[:, m:m + 1])
            fm(OFF_QK, 8, st_qk[par], None)
            P.dma("sync", V(C.qkpre.h[:, lto:lto + nt].rearrange("(j p) t -> p j t", p=128), C.qkpre.name), st_qk[par][:, :, 0:nt])
            fm(OFF_U, 4, st_u[par], None)
            P.dma("sync", V(C.uT.h[:, lto:lto + nt].rearrange("(j p) t -> p j t", p=128), C.uT.name), st_u[par][:, :, 0:nt])
            if not isc:
                for gh in range(2):
                    fm(OFF_G + gh * 1024, 8, st_G[gh], AF.Sigmoid, bias=V(bgate.h[:, gh * 8:(gh + 1) * 8], bgate.name))
                    P.dma("scalar", V(C.GT.h[gh * 1024:(gh + 1) * 1024, off:off + nt].rearrange("(j p) t -> p j t", p=128), C.GT.name), st_G[gh][:, :, 0:nt])
            for s in range(nsub):
                for hh in range(2):
                    pb = pA[pai % 4]; pai += 1
                    for k in range(8):
                        P.mm(pb[:], h[:, k, s * 128:(s + 1) * 128], win[:, k, OFF_V + hh * 512: OFF_V + (hh + 1) * 512], start=(k == 0), stop=(k == 7))
                    P.cp("vector", st_v[s % 2][:, hh * 512:(hh + 1) * 512], pb[:])
                P.dma("sync", C.v[lto + s * 128: lto + (s + 1) * 128, :], st_v[s % 2][:])
                pb = pA[pai % 4]; pai += 1
                for k in range(8):
                    P.mm(pb[:, 0:32], h[:, k, s * 128:(s + 1) * 128], win[:, k, OFF_IF:OFF_IF + 32], start=(k == 0), stop=(k == 7))
                g = gw["g"]
                P.tt("vector", g[:], pb[:, 0:32], bif[:], ALU.add)
                gv = g[:].m(lambda a: a.rearrange("p (d i h) -> p d i h", d=2, i=2))
                sg = st_g[par][:, s, :].m(lambda a: a.rearrange("p (i d h) -> p i d h", i=2, d=2))
                P.cp("vector", V(sg.ap[:, 0], sg.name), V(gv.ap[:, :, 0, :], gv.name))
                P.act(gw["e"][:], g[:], AF.Exp, scale=-1.0)
                P.act(gw["sp"][:], gw["e"][:], AF.Ln, bias=1.0)
                spv = gw["sp"][:].m(lambda a: a.rearrange("p (d i h) -> p d i h", d=2, i=2))
                P.ts("vector", V(sg.ap[:, 1], sg.name), V(spv.ap[:, :, 1, :], spv.name), -1.0, ALU.mult)
                if not isc:
                    for hh in range(2):
                        pb = pA[pai % 4]; pai += 1
                        for k in range(8):
                            P.mm(pb[:], h[:, k, s * 128:(s + 1) * 128], win[:, k, OFF_O + hh * 512: OFF_O + (hh + 1) * 512], start=(k == 0), stop=(k == 7))
                        P.act(st_o[s % 2][:, hh * 512:(hh + 1) * 512], pb[:], AF.Sigmoid)
                    P.dma("scalar", C.og[off + s * 128: off + (s + 1) * 128, :], st_o[s % 2][:])
            P.dma("sync", V(C.gl.h[lto:lto + nt, :].rearrange("(s p) c -> p s c", p=128), C.gl.name), st_g[par][:, 0:nsub, :])
        P.barrier(); P.flush()

def phase2(P, C, K):
    with ExitStack() as ph:
        P.stack = ph
        wc = P.sb("wc", [128, 8, 9], F32); P.dma("sync", wc[:], C.wconv[:])
        pre = [P.sb("pre%d" % i, [128, LT], BF16) for i in range(2)]
        acc = [P.sb("cacc%d" % i, [128, LT], F32) for i in range(2)]
        sl = [P.sb("csl%d" % i, [128, LT], F32) for i in range(2)]
        ob = [P.sb("cob%d" % i, [128, LT], BF16) for i in range(2)]
        kst = [P.sb("kst%d" % i, [128, 4, 128], BF16) for i in range(2)]
        pT = [P.ps("pT%d" % i, [128, 512]) for i in range(2)]
        for j in range(8):
            e = "vector"
            p_, a_, s_, o_ = pre[j % 2], acc[j % 2], sl[j % 2], ob[j % 2]
            P.dma("sync", p_[:], C.qkpre[j * 128:(j + 1) * 128, :])
            def w(t):
                return wc[:, j, t:t + 1]
            P.ts(e, a_[:, 0:LC], p_[:, 0:LC], w(4), ALU.mult)
            P.stt(e, a_[:, 1:LC], p_[:, 0:LC - 1], w(3), a_[:, 1:LC], ALU.mult, ALU.add)
            P.stt(e, a_[:, 0:LC - 1], p_[:, 1:LC], w(5), a_[:, 0:LC - 1], ALU.mult, ALU.add)
            pv = p_[:, LC:LT].m(lambda a: a.rearrange("p (r w) -> p r w", w=64))
            av = a_[:, LC:LT].m(lambda a: a.rearrange("p (r w) -> p r w", w=64))
            P.ts(e, a_[:, LC:LT], p_[:, LC:LT], w(4), ALU.mult)
            for dr in range(3):
                for dw in range(3):
                    if dr == 1 and dw == 1:
                        continue
                    ro = slice(max(0, 1 - dr), 64 - max(0, dr - 1)); ri = slice(max(0, dr - 1), 64 - max(0, 1 - dr))
                    co = slice(max(0, 1 - dw), 64 - max(0, dw - 1)); ci = slice(max(0, dw - 1), 64 - max(0, 1 - dw))
                    o_v = V(av.ap[:, ro, co], av.name); i_v = V(pv.ap[:, ri, ci], pv.name)
                    P.stt(e, o_v, i_v, w(dr * 3 + dw), o_v, ALU.mult, ALU.add)
            P.act(s_[:], a_[:], AF.Silu)
            if j < 4:
                if j % 2 == 0:
                    P.act(o_[:], s_[:], AF.Identity, scale=0.125)
                else:
                    P.ts("vector", o_[:], s_[:], 0.125, ALU.mult)
                P.dma("sync", C.qT[j * 128:(j + 1) * 128, :], o_[:])
            else:
                jj = j - 4
                P.cp("scalar" if j % 2 == 0 else "vector", o_[:], s_[:])
                P.dma("sync", C.kT[jj * 128:(jj + 1) * 128, :], o_[:])
                for g4 in range(9):
                    nb = min(4, 34 - g4 * 4)
                    pt = pT[g4 % 2]; ks = kst[g4 % 2]
                    for bI in range(nb):
                        blk = g4 * 4 + bI
                        P.tr(pt[:, bI * 128:(bI + 1) * 128], s_[:, blk * 128:(blk + 1) * 128], K.ident)
                    P.cp("scalar", ks[:, 0:nb, :], pt[:, 0:nb * 128].m(lambda a: a.rearrange("p (b c) -> p b c", c=128)))
                    P.dma("sync", V(C.ktok.h[g4 * 512: g4 * 512 + nb * 128, jj * 128:(jj + 1) * 128].rearrange("(b p) c -> p b c", p=128), C.ktok.name), ks[:, 0:nb, :])
        P.barrier(); P.flush()


HGRP = [(0, 3), (3, 3), (6, 2)]


def hoff(h):
    return (h // 3) * 512 + (h % 3) * 170


def chunk_orders():
    fwd = list(range(34))
    bwd = [1, 0] + list(range(33, 1, -1))
    return fwd, bwd


def phase3(P, C, K):
    with ExitStack() as ph:
        P.stack = ph
        Cn = P.sb("Cn", [128, 2, 8, 129], F32); Cnb = P.sb("Cnb", [128, 2, 8, 129], BF16)
        P.memset("vector", Cn[:], 0.0); P.memset("vector", Cnb[:], 0.0)
        NB = 2
        v1 = [[P.sb("v1_%d%d" % (d, i), [128, 8, 129], BF16) for i in range(NB)] for d in range(2)]
        glt = [[P.sb("glt_%d%d" % (d, i), [128, 32], F32) for i in range(NB)] for d in range(2)]
        kt = [[P.sb("kt_%d%d" % (d, i), [128, 9, 64], BF16) for i in range(NB)] for d in range(2)]
        qTt = [[P.sb("qTt_%d%d" % (d, i), [128, 8, 128], BF16) for i in range(NB)] for d in range(2)]
        kTt = [[P.sb("kTt_%d%d" % (d, i), [128, 8, 128], BF16) for i in range(NB)] for d in range(2)]
        for d in range(2):
            for i in range(NB):
                P.memset("vector", v1[d][i][:], 1.0)
                P.memset("vector", kt[d][i][:], 0.0)
                P.memset("vector", qTt[d][i][:], 0.0)
                P.memset("vector", kTt[d][i][:], 0.0)
        vw = [P.sb("vw%d" % d, [128, 8, 129], BF16) for d in range(2)]
        PT = [P.sb("PTs%d" % i, [128, 128], BF16) for i in range(4)]
        hd = [P.sb("hd%d" % d, [128, 8, 128], F32) for d in range(2)]
        sm = {n: [P.sb("sm_%s%d" % (n, d), [128, 8], F32) for d in range(2)] for n in ["nb", "t1", "ek", "t2", "w", "dec", "ad", "mx", "r"]}
        pG = P.ps("pG", [128, 512]); pPs = [P.ps("pP%d" % i, [128, 512]) for i in range(2)]
        pAccs = [P.ps("pAcc%d" % i, [128, 512]) for i in range(2)]; pUs = [P.ps("pU%d" % i, [128, 512]) for i in range(2)]
        gi_ctr = [0]; gu_ctr = [0]
        fwd, bwd = chunk_orders()
        pti = 0
        its3 = [(step, d) for step in range(34) for d in range(2)]

        def front(n):
            step, d = its3[n]
            g = (fwd, bwd)[d][step]
            lat = g >= 2
            bi = step % NB
            r0 = g * 128
            tri = K.triF if d == 0 else K.triB
            V1, GL, KT, QT, KTT = v1[d][bi], glt[d][bi], kt[d][bi], qTt[d][bi], kTt[d][bi]
            S = {nm_: sm[nm_][d] for nm_ in sm}
            g = (fwd, bwd)[d][step]
            lat = g >= 2
            bi = step % NB
            r0 = g * 128
            tri = K.triF if d == 0 else K.triB
            V1, GL, KT, QT, KTT = v1[d][bi], glt[d][bi], kt[d][bi], qTt[d][bi], kTt[d][bi]
            P.dma("sync", V1[:, :, 0:128], V(C.v.h[r0:r0 + 128, :].rearrange("p (h c) -> p h c", h=8), C.v.name))
            P.dma("sync", GL[:], C.gl[r0:r0 + 128, :])
            P.dma("sync", KT[:, 0:8, :], V(C.ktok.h[r0:r0 + 128, :].rearrange("p (h c) -> p h c", h=8), C.ktok.name))
            if lat:
                P.dma("sync", QT[0:64, :, :], V(C.qT.h[:, r0:r0 + 128].rearrange("(h k) t -> k h t", k=64), C.qT.name))
                P.dma("sync", KTT[0:64, :, :], V(C.kT.h[:, r0:r0 + 128].rearrange("(h k) t -> k h t", k=64), C.kT.name))
            li = GL[:, 8 * d: 8 * d + 8]; lf = GL[:, 16 + 8 * d: 16 + 8 * d + 8]
            pg = pG[:, 0:8]; pe = pG[:, 8:16]
            P.mm(pg, tri, lf); P.mm(pe, K.ones, lf)
            P.tt("vector", S["t1"][:], li, pg, ALU.subtract)
            P.tt("vector", S["t2"][:], S["t1"][:], pe, ALU.add)
            P.act(S["w"][:], S["t2"][:], AF.Exp)
            P.act(S["dec"][:], pe, AF.Exp)
            if lat:
                P.act(S["nb"][:], pg, AF.Exp, scale=-1.0)
                P.act(S["ek"][:], S["t1"][:], AF.Exp)
            P.tt("vector", vw[d][:], V1[:], S["w"][:].m(lambda a: a.rearrange("p (h o) -> p h o", o=1).to_broadcast([128, 8, 129])), ALU.mult)

        def back(n):
            nonlocal pti
            step, d = its3[n]
            g = (fwd, bwd)[d][step]
            lat = g >= 2
            bi = step % NB
            r0 = g * 128
            tri = K.triF if d == 0 else K.triB
            V1, GL, KT, QT, KTT = v1[d][bi], glt[d][bi], kt[d][bi], qTt[d][bi], kTt[d][bi]
            S = {nm_: sm[nm_][d] for nm_ in sm}
            if lat:
                for gi, (h0, nh) in enumerate(HGRP):
                    pa_bank = pAccs[gi_ctr[0] % 2]; gi_ctr[0] += 1
                    for h in range(h0, h0 + nh):
                        pp = pPs[pti % 2][:, 0:128]
                        P.mm(pp, KTT[:, h, :], QT[:, h, :])
                        pt = PT[pti % 4]; pti += 1
                        P.stt("vector", pt[:], pp, S["ek"][:, h:h + 1], tri, ALU.mult, ALU.mult)
                        pa = pa_bank[:, (h - h0) * 170:(h - h0) * 170 + 129]
                        P.mm(pa, QT[:, h, :], Cnb[:, d, h, :], start=True, stop=False)
                        P.mm(pa, pt[:], V1[:, h, :], start=False, stop=True)
                    accv = pa_bank[:, 0:nh * 170].m(lambda a: a.rearrange("p (h c) -> p h c", c=170))
                    P.act(S["ad"][:, h0:h0 + nh], V(accv.ap[:, :, 128], accv.name), AF.Abs)
                    P.tt("vector", S["mx"][:, h0:h0 + nh], S["ad"][:, h0:h0 + nh], S["nb"][:, h0:h0 + nh], ALU.max)
                    P.op("vector", lambda E, a=S["r"], b=S["mx"], h0=h0, nh=nh: E.reciprocal(a.h[:, h0:h0 + nh], b.h[:, h0:h0 + nh]), [S["mx"][:]], [S["r"][:]])
                    P.tt("vector", hd[d][:, h0:h0 + nh, :], V(accv.ap[:, :, 0:128], accv.name),
                         S["r"][:, h0:h0 + nh].m(lambda a, nh=nh: a.rearrange("p (h o) -> p h o", o=1).to_broadcast([128, nh, 128])), ALU.mult)
                P.dma("scalar", V(C.hd[d].h[r0 - LC: r0 - LC + 128, :].rearrange("p (h c) -> p h c", h=8), C.hd[d].name), hd[d][:])
            for gi, (h0, nh) in enumerate(HGRP):
                pu_bank = pUs[gu_ctr[0] % 2]; gu_ctr[0] += 1
                for h in range(h0, h0 + nh):
                    pu = pu_bank[:, (h - h0) * 170:(h - h0) * 170 + 129]
                    P.mm(pu, KT[:, h:h + 2, :].m(lambda a: a.rearrange("p a b -> p (a b)")), vw[d][:, h, :])
                cg = Cn[0:64, d, h0:h0 + nh]
                P.tt("vector", cg, cg, S["dec"][0:64, h0:h0 + nh].m(lambda a, nh=nh: a.rearrange("p (h o) -> p h o", o=1).to_broadcast([64, nh, 129])), ALU.mult)
                uv = V(pu_bank.h[0:64, 0:nh * 170].rearrange("p (h c) -> p h c", c=170)[:, :, 0:129], pu_bank.name)
                P.tt("vector", cg, cg, uv, ALU.add)
                P.cp("scalar", Cnb[0:64, d, h0:h0 + nh], cg)

        front(0)
        for n in range(68):
            if n + 1 < 68:
                front(n + 1)
            back(n)
        P.barrier(); P.flush()

TWO_PI = 6.283185307179586
I32 = mybir.dt.int32


def sincos(P, ang, s_out, c_out, tmp, tmpi, shape):
    for phase_off, dst in ((0.0, s_out), (0.25, c_out)):
        P.ts("vector", tmp, ang, 1.0 / TWO_PI, ALU.mult, phase_off, ALU.add)
        P.cp("vector", tmpi, tmp)
        P.cp("vector", tmp, tmpi)
        P.stt("vector", tmp, tmp, -TWO_PI, ang, ALU.mult, ALU.add)
        if phase_off != 0.0:
            P.ts("vector", tmp, tmp, TWO_PI * phase_off, ALU.add)
        P.ts("vector", tmp, tmp, 3.14159, ALU.min, -3.14159, ALU.max)
        P.act(dst, tmp, AF.Sin)


def phase4(P, C, K):
    with ExitStack() as ph:
        P.stack = ph
        Er = [P.sb("Er%d" % d, [128, 2048], F32) for d in range(2)]; Ei = [P.sb("Ei%d" % d, [128, 2048], F32) for d in range(2)]
        Fr = [P.sb("Fr%d" % d, [128, 16, 128], F32) for d in range(2)]; Fi = [P.sb("Fi%d" % d, [128, 16, 128], F32) for d in range(2)]
        Bbr = [P.sb("Bbr%d" % d, [128, 2048], BF16) for d in range(2)]; Bbi = [P.sb("Bbi%d" % d, [128, 2048], BF16) for d in range(2)]
        Cr = [P.sb("Cr%d" % d, [128, 2048], BF16) for d in range(2)]; Cin = [P.sb("Cin%d" % d, [128, 2048], BF16) for d in range(2)]
        Crn = [P.sb("Crn%d" % d, [128, 2048], BF16) for d in range(2)]
        ntribf = [P.sb("ntribf%d" % d, [128, 128], BF16) for d in range(2)]
        A8r = [P.sb("A8r%d" % d, [128, 16], F32) for d in range(2)]; A8i = [P.sb("A8i%d" % d, [128, 16], F32) for d in range(2)]
        tribf = [P.sb("tribf%d" % d, [128, 128], BF16) for d in range(2)]
        P.cp("vector", tribf[0][:], K.triF); P.cp("vector", tribf[1][:], K.triB)
        P.ts("vector", ntribf[0][:], K.triF, -1.0, ALU.mult); P.ts("vector", ntribf[1][:], K.triB, -1.0, ALU.mult)
        cc = [[(P.sb("cR%d%d" % (d, i), [128, 16], F32), P.sb("cI%d%d" % (d, i), [128, 16], F32)) for i in range(2)] for d in range(2)]
        for d in range(2):
            P.memset("vector", cc[d][0][0][:], 0.0); P.memset("vector", cc[d][0][1][:], 0.0)
        with ExitStack() as su:
            P.stack = su
            T = [P.sb("su%d" % i, [128, 2048], F32) for i in range(10)]
            TI = P.sb("sui", [128, 2048], I32)
            colp = P.sb("colp", [128, 3, 16], F32); lam = P.sb("lamc", [128, 2, 16], F32)
            for d in range(2):
                aRe, aIm, ldt, lRe, lIm, t5, t6, t7, t8, t9 = [t[:] for t in T]
                ti = TI[:]
                P.dma("sync", aRe, C.s5row[d, 0]); P.dma("sync", aIm, C.s5row[d, 1]); P.dma("sync", ldt, C.s5row[d, 2])
                P.act(ldt, ldt, AF.Exp)
                P.tt("vector", lRe, ldt, aRe, ALU.mult); P.tt("vector", lIm, ldt, aIm, ALU.mult)
                P.act(t5, lRe, AF.Exp)
                sincos(P, lIm, t6, t7, t8, ti, None)
                P.tt("vector", t7, t7, t5, ALU.mult)
                P.tt("vector", t6, t6, t5, ALU.mult)
                P.ts("vector", t7, t7, -1.0, ALU.add)
                P.tt("vector", t5, aRe, aRe, ALU.mult); P.tt("vector", t8, aIm, aIm, ALU.mult)
                P.tt("vector", t5, t5, t8, ALU.add)
                P.op("vector", lambda E, a=T[5]: E.reciprocal(a.h[:], a.h[:]), [t5], [t5])
                P.tt("vector", t8, t7, aRe, ALU.mult); P.tt("vector", t9, t6, aIm, ALU.mult)
                P.tt("vector", t8, t8, t9, ALU.add); P.tt("vector", t8, t8, t5, ALU.mult)
                P.tt("vector", t9, t6, aRe, ALU.mult); P.tt("vector", t6, t7, aIm, ALU.mult)
                P.tt("vector", t9, t9, t6, ALU.subtract); P.tt("vector", t9, t9, t5, ALU.mult)
                P.dma("sync", t5, C.bbd[d, 0]); P.dma("sync", t6, C.bbd[d, 1])
                P.tt("vector", t7, t8, t5, ALU.mult); P.tt("vector", aRe, t9, t6, ALU.mult)
                P.tt("vector", Bbr[d][:], t7, aRe, ALU.subtract)
                P.tt("vector", t7, t8, t6, ALU.mult); P.tt("vector", aRe, t9, t5, ALU.mult)
                P.tt("vector", Bbi[d][:], t7, aRe, ALU.add)
                P.dma("sync", t5, C.cbd[d, 0]); P.dma("sync", t6, C.cbd[d, 1])
                P.cp("vector", Cr[d][:], t5); P.ts("vector", Cin[d][:], t6, -1.0, ALU.mult)
                P.ts("vector", Crn[d][:], t5, -1.0, ALU.mult)
                idx = K.cst[:, 6, d:d + 1]
                P.ts("vector", t5, lRe, idx, ALU.mult); P.act(t5, t5, AF.Exp, scale=-1.0)
                P.ts("vector", t6, lIm, idx, ALU.mult)
                sincos(P, t6, t7, t8, t9, ti, None)
                P.tt("vector", Er[d][:], t5, t8, ALU.mult)
                P.tt("vector", t7, t5, t7, ALU.mult); P.ts("vector", Ei[d][:], t7, -1.0, ALU.mult)
                for i in range(3):
                    P.dma("sync", colp[:, i, :], C.s5col[d, i])
                P.act(colp[:, 2, :], colp[:, 2, :], AF.Exp)
                P.tt("vector", lam[:, 0, :], colp[:, 2, :], colp[:, 0, :], ALU.mult)
                P.tt("vector", lam[:, 1, :], colp[:, 2, :], colp[:, 1, :], ALU.mult)
                irow = K.cst[:, 4 + d, :]
                fR = t5.m(lambda a: a.rearrange("p (q t) -> p q t", t=128)); fI = t6.m(lambda a: a.rearrange("p (q t) -> p q t", t=128))
                for q in range(16):
                    P.ts("vector", V(fR.ap[:, q, :], fR.name), irow, lam[:, 0, q:q + 1], ALU.mult)
                    P.ts("vector", V(fI.ap[:, q, :], fI.name), irow, lam[:, 1, q:q + 1], ALU.mult)
                P.act(t5, t5, AF.Exp)
                sincos(P, t6, t7, t8, t9, ti, None)
                P.tt("vector", Fr[d][:].m(lambda a: a.rearrange("p q t -> p (q t)")), t5, t8, ALU.mult)
                P.tt("vector", Fi[d][:].m(lambda a: a.rearrange("p q t -> p (q t)")), t5, t7, ALU.mult)
                s16 = [V(t.h[:, 0:16], t.name) for t in T[5:10]]; s16i = V(TI.h[:, 0:16], TI.name)
                P.ts("vector", s16[0], lam[:, 0, :], 128.0, ALU.mult); P.act(s16[0], s16[0], AF.Exp)
                P.ts("vector", s16[1], lam[:, 1, :], 128.0, ALU.mult)
                sincos(P, s16[1], s16[2], s16[3], s16[4], s16i, None)
                P.tt("vector", A8r[d][:], s16[0], s16[3], ALU.mult); P.tt("vector", A8i[d][:], s16[0], s16[2], ALU.mult)
            P.barrier(); P.flush()
        P.stack = ph
        ut = [[P.sb("ut%d%d" % (d, i), [128, 4, 128], BF16) for i in range(2)] for d in range(2)]
        Z = [[P.sb("Z%d_%d" % (k, d), [128, 2048], BF16) for d in range(2)] for k in range(4)]
        Sp = [(P.sb("Spr%d" % i, [128, 4, 128], F32), P.sb("Spi%d" % i, [128, 4, 128], F32)) for i in range(2)]
        Hq = [[P.sb("Hq%d_%d" % (k, i), [128, 4, 128], BF16) for i in range(2)] for k in range(4)]
        sl = [P.sb("s5l%d" % i, [128, 4], F32) for i in range(4)]
        ysb = [P.sb("ysb%d" % d, [128, 4, 128], F32) for d in range(2)]
        pBr = P.ps("pBr", [128, 512]); pBi = P.ps("pBi", [128, 512])
        pSrs = [P.ps("pSr%d" % i, [128, 512]) for i in range(2)]; pSis = [P.ps("pSi%d" % i, [128, 512]) for i in range(2)]
        pYs = [P.ps("pY%d" % i, [128, 512]) for i in range(2)]
        fwd, bwd = chunk_orders()
        its = [(step, d) for step in range(34) for d in range(2)]

        def BZ(n):
            step, d = its[n]
            g = (fwd, bwd)[d][step]; r0 = g * 128
            u_ = ut[d][step % 2]
            P.dma("sync", u_[:], V(C.uT.h[:, r0:r0 + 128].rearrange("(j p) t -> p j t", p=128), C.uT.name))
            for j in range(4):
                P.mm(pBr[:], u_[:, j, :], Bbr[d][:, j * 512:(j + 1) * 512])
                P.mm(pBi[:], u_[:, j, :], Bbi[d][:, j * 512:(j + 1) * 512])
                hs = slice(j * 512, (j + 1) * 512)
                P.tt("vector", Z[0][d][:, hs], Er[d][:, hs], pBr[:], ALU.mult)
                P.tt("vector", Z[3][d][:, hs], Ei[d][:, hs], pBr[:], ALU.mult)
                P.tt("vector", Z[1][d][:, hs], Ei[d][:, hs], pBi[:], ALU.mult)
                P.tt("vector", Z[2][d][:, hs], Er[d][:, hs], pBi[:], ALU.mult)

        def cumsum(n, qg):
            step, d = its[n]
            b = (4 * n + qg) % 2
            pSr = pSrs[b]; pSi = pSis[b]
            for qq in range(4):
                q = 4 * qg + qq
                ps_r = pSr[:, qq * 128:(qq + 1) * 128]
                P.mm(ps_r, Z[0][d][:, q * 128:(q + 1) * 128], tribf[d][:], start=True, stop=False)
                P.mm(ps_r, Z[1][d][:, q * 128:(q + 1) * 128], ntribf[d][:], start=False, stop=True)
            for qq in range(4):
                q = 4 * qg + qq
                ps_i = pSi[:, qq * 128:(qq + 1) * 128]
                P.mm(ps_i, Z[2][d][:, q * 128:(q + 1) * 128], tribf[d][:], start=True, stop=False)
                P.mm(ps_i, Z[3][d][:, q * 128:(q + 1) * 128], tribf[d][:], start=False, stop=True)

        def evacH(n, qg):
            step, d = its[n]
            lat = (fwd, bwd)[d][step] >= 2
            b = (4 * n + qg) % 2
            pSr = pSrs[b]; pSi = pSis[b]
            cR, cI = cc[d][step % 2]; nR, nI = cc[d][(step + 1) % 2]
            last = 127 if d == 0 else 0
            spr, spi = Sp[b]
            qs = slice(4 * qg, 4 * qg + 4)
            for qq in range(4):
                q = 4 * qg + qq
                P.act(spr[:, qq, :], pSr[:, qq * 128:(qq + 1) * 128], AF.Identity, bias=cR[:, q:q + 1])
            for qq in range(4):
                q = 4 * qg + qq
                P.act(spi[:, qq, :], pSi[:, qq * 128:(qq + 1) * 128], AF.Identity, bias=cI[:, q:q + 1])
            P.tt("vector", sl[0][:], A8r[d][:, qs], spr[:, :, last], ALU.mult)
            P.tt("vector", sl[1][:], A8i[d][:, qs], spi[:, :, last], ALU.mult)
            P.tt("vector", nR[:, qs], sl[0][:], sl[1][:], ALU.subtract)
            P.tt("vector", sl[2][:], A8r[d][:, qs], spi[:, :, last], ALU.mult)
            P.tt("vector", sl[3][:], A8i[d][:, qs], spr[:, :, last], ALU.mult)
            P.tt("vector", nI[:, qs], sl[2][:], sl[3][:], ALU.add)
            if lat:
                H = [Hq[k][b] for k in range(4)]
                P.tt("vector", H[0][:], Fr[d][:, qs, :], spr[:], ALU.mult)
                P.tt("vector", H[1][:], Fi[d][:, qs, :], spi[:], ALU.mult)
                P.tt("vector", H[2][:], Fr[d][:, qs, :], spi[:], ALU.mult)
                P.tt("vector", H[3][:], Fi[d][:, qs, :], spr[:], ALU.mult)

        def Yq(n, qg):
            step, d = its[n]
            if (fwd, bwd)[d][step] < 2:
                return
            b = (4 * n + qg) % 2
            H = [Hq[k][b] for k in range(4)]
            py = pYs[n % 2][:, qg * 128:(qg + 1) * 128]
            for qq in range(4):
                q = 4 * qg + qq
                cs = slice(q * 128, (q + 1) * 128)
                P.mm(py, Cr[d][:, cs], H[0][:, qq, :], start=(qq == 0), stop=False)
                P.mm(py, Crn[d][:, cs], H[1][:, qq, :], start=False, stop=False)
                P.mm(py, Cin[d][:, cs], H[2][:, qq, :], start=False, stop=False)
                P.mm(py, Cin[d][:, cs], H[3][:, qq, :], start=False, stop=(qq == 3))

        BZ(0)
        for n in range(68):
            step, d = its[n]
            g = (fwd, bwd)[d][step]; lat = g >= 2; r0 = g * 128
            if n + 1 < 68:
                BZ(n + 1)
            cumsum(n, 0); evacH(n, 0)
            cumsum(n, 1); evacH(n, 1)
            Yq(n, 0)
            cumsum(n, 2); evacH(n, 2)
            Yq(n, 1)
            cumsum(n, 3); evacH(n, 3)
            Yq(n, 2); Yq(n, 3)
            if lat:
                P.cp("scalar", ysb[d][:], pYs[n % 2][:].m(lambda a: a.rearrange("p (j t) -> p j t", t=128)))
                P.dma("scalar", V(C.yT[d].h[:, r0 - LC: r0 - LC + 128].rearrange("(j p) t -> p j t", p=128), C.yT[d].name), ysb[d][:])
        P.barrier(); P.flush()

def bcl(v, n, m):
    return v.m(lambda a: a.rearrange("p (h o) -> p h o", o=1).to_broadcast([128, n, m]))


def phase5(P, C, K):
    with ExitStack() as ph:
        P.stack = ph
        gmh = P.sb("gmh", [128, 1024], F32); P.dma("sync", gmh[:], C.gmh[:])
        hf = [P.sb("p5hf%d" % i, [128, 8, 128], F32) for i in range(2)]
        hb = [P.sb("p5hb%d" % i, [128, 8, 128], F32) for i in range(2)]
        og = [P.sb("p5og%d" % i, [128, 1024], BF16) for i in range(2)]
        sqs = [P.sb("p5sq%d" % i, [128, 8, 128], F32) for i in range(2)]; hns = [P.sb("p5hn%d" % i, [128, 1024], F32) for i in range(2)]
        st = [P.sb("p5st%d" % i, [128, 8, 128], BF16) for i in range(2)]
        s8s = [{n: P.sb("p5_%s%d" % (n, i), [128, 8], F32) for n in ["ss", "ms", "sq", "r"]} for i in range(2)]
        pT = P.ps("p5pT", [128, 1024])

        def s1(t):
            a, b_, o_ = hf[t % 2], hb[t % 2], og[t % 2]
            sq = sqs[t % 2]; s8 = s8s[t % 2]
            r0 = t * 128
            P.dma("sync", a[:], V(C.hd[0].h[r0:r0 + 128, :].rearrange("p (h c) -> p h c", h=8), C.hd[0].name))
            P.dma("scalar", b_[:], V(C.hd[1].h[r0:r0 + 128, :].rearrange("p (h c) -> p h c", h=8), C.hd[1].name))
            P.dma("scalar", o_[:], C.og[r0:r0 + 128, :])
            P.tt("vector", a[:], a[:], b_[:], ALU.add)
            for h in range(8):
                P.act(sq[:, h, :], a[:, h, :], AF.Square, accum=s8["ss"][:, h:h + 1])
            P.ts("vector", s8["ms"][:], s8["ss"][:], 1.0 / 128, ALU.mult, EPS, ALU.add)
            P.act(s8["sq"][:], s8["ms"][:], AF.Sqrt)
            P.op("vector", lambda E, s8=s8: E.reciprocal(s8["r"].h[:], s8["sq"].h[:]), [s8["sq"][:]], [s8["r"][:]])

        def s2(t):
            a, o_ = hf[t % 2], og[t % 2]
            hn = hns[t % 2]; s8 = s8s[t % 2]
            r0 = t * 128
            hv = hn[:].m(lambda x: x.rearrange("p (h c) -> p h c", h=8))
            P.tt("vector", hv, a[:], bcl(s8["r"][:], 8, 128), ALU.mult)
            P.tt("vector", hn[:], hn[:], gmh[:], ALU.mult)
            P.tt("vector", hn[:], hn[:], o_[:], ALU.mult)
            for j in range(8):
                P.tr(pT[:, j * 128:(j + 1) * 128], hn[:, j * 128:(j + 1) * 128], K.ident)
            s_ = st[t % 2]
            P.cp("scalar", s_[:], pT[:].m(lambda x: x.rearrange("p (j t) -> p j t", j=8)))
            P.dma("scalar", V(C.hmT.h[:, r0:r0 + 128].rearrange("(j p) t -> p j t", p=128), C.hmT.name), s_[:])

        s1(0)
        for t in range(32):
            if t + 1 < 32:
                s1(t + 1)
            s2(t)
        P.barrier(); P.flush()


def phase6(P, C, K):
    with ExitStack() as ph:
        P.stack = ph
        wA = P.sb("wA", [128, 8, 1024], BF16); wG = P.sb("wG", [128, 4, 512], BF16)
        wB = P.sb("wB", [128, 4, 1024], BF16); wO = P.sb("wO", [128, 8, 1024], BF16)
        wR = P.sb("wR", [128, 8, 32], F32); bR = P.sb("bR", [128, 32], F32)
        bglu = P.sb("bglu", [128, 4], F32); s5d = P.sb("s5dt", [128, 4], F32)
        P.dma("gpsimd", wA[:], V(C.w_a.h.rearrange("(k p) n -> p k n", p=128), C.w_a.name))
        P.dma("gpsimd", wG[:], V(C.w_glu.h.rearrange("(k p) n -> p k n", p=128), C.w_glu.name))
        P.dma("gpsimd", wB[:], V(C.w_b.h.rearrange("(k p) n -> p k n", p=128), C.w_b.name))
        P.dma("sync", wR[:], V(C.w_r.h.rearrange("(k p) n -> p k n", p=128), C.w_r.name))
        P.dma("sync", bR[:], C.b_r[:]); P.dma("sync", bglu[:], C.b_glu[:]); P.dma("sync", s5d[:], C.s5d[:])
        with ExitStack() as su:
            P.stack = su
            wo32 = P.sb("wo32", [128, 8, 1024], F32)
            P.dma("sync", wo32[:], V(C.w_o.h.rearrange("(k p) n -> p k n", p=128), C.w_o.name))
            P.tt("vector", wO[:], wo32[:], K.gt1bc[:].m(lambda a: a.rearrange("p (o n) -> p o n", o=1).to_broadcast([128, 8, 1024])), ALU.mult)
            P.barrier(); P.flush()
        P.stack = ph
        W = norm_work(P, "6")
        hmT = [P.sb("p6hm%d" % i, [128, 8, 512], BF16) for i in range(2)]
        yf = P.sb("p6yf", [128, 4, 512], F32); yb = P.sb("p6yb", [128, 4, 512], F32)
        uT = P.sb("p6u", [128, 4, 512], BF16); GT = [P.sb("p6G%d" % i, [128, 16, 512], BF16) for i in range(2)]
        x2 = P.sb("p6x2", [128, 4, 512], F32); sg = P.sb("p6sg", [128, 4, 512], F32)
        ysg = P.sb("p6ysg", [128, 4, 512], BF16); ys2 = P.sb("p6ys2", [128, 4, 512], BF16)
        sgz = P.sb("p6sgz", [128, 512], F32); m1 = P.sb("p6m1", [128, 512], F32); m2 = P.sb("p6m2", [128, 512], F32)
        mg = P.sb("p6mg", [128, 8, 512], BF16)
        xt = [P.sb("p6xt%d" % i, [128, 1024], F32) for i in range(3)]
        h2s = P.sb("p6h2s", [128, 8, 512], BF16); h2f = P.sb("p6h2f", [128, 8, 128], F32)
        lg = P.sb("p6lg", [128, 32], F32); m8 = P.sb("p6m8", [128, 8], F32); nm = P.sb("p6nm", [128, 1], F32)
        msk = P.sb("p6msk", [128, 32], F32); ex = P.sb("p6ex", [128, 32], F32); ssum = P.sb("p6ssum", [128, 1], F32)
        gts = P.sb("p6gts", [128, 4, 32], F32); gT = P.sb("p6gT", [32, 512], F32)
        pa = [P.ps("p6pa%d" % i, [128, 512]) for i in range(4)]
        pr = P.ps("p6pr", [128, 512]); pgT = P.ps("p6pgT", [128, 512])
        pai = 0
        for t in range(8):
            o0 = t * 512
            hm = hmT[t % 2]; G = GT[t % 2]
            P.dma("sync", hm[:], V(C.hmT.h[:, o0:o0 + 512].rearrange("(j p) t -> p j t", p=128), C.hmT.name))
            P.dma("sync", yf[:], V(C.yT[0].h[:, o0:o0 + 512].rearrange("(j p) t -> p j t", p=128), C.yT[0].name))
            P.dma("sync", yb[:], V(C.yT[1].h[:, o0:o0 + 512].rearrange("(j p) t -> p j t", p=128), C.yT[1].name))
            P.dma("sync", uT[:], V(C.uT.h[:, LC + o0:LC + o0 + 512].rearrange("(j p) t -> p j t", p=128), C.uT.name))
            P.dma("sync", G[:], V(C.GT.h[:, o0:o0 + 512].rearrange("(j p) t -> p j t", p=128), C.GT.name))
            P.tt("vector", yf[:], yf[:], yb[:], ALU.add)
            for j in range(4):
                P.stt("vector", yf[:, j, :], uT[:, j, :], s5d[:, j:j + 1], yf[:, j, :], ALU.mult, ALU.add)
            P.act(x2[:], yf[:], AF.Square)
            P.ts("vector", x2[:], x2[:], 0.044715, ALU.mult, 1.0, ALU.add)
            P.tt("vector", x2[:], x2[:], yf[:], ALU.mult)
            P.act(sg[:], x2[:], AF.Sigmoid, scale=1.5957691216057308)
            P.tt("vector", ysg[:], yf[:], sg[:], ALU.mult)
            for n in range(4):
                pb = pa[pai % 4]; pai += 1
                for k in range(4):
                    P.mm(pb[:], wG[:, k, n * 128:(n + 1) * 128], ysg[:, k, :], start=(k == 0), stop=(k == 3))
                P.act(sgz[:], pb[:], AF.Sigmoid, bias=bglu[:, n:n + 1])
                P.tt("vector", ys2[:, n, :], ysg[:, n, :], sgz[:], ALU.mult)
            for n in range(8):
                pA_ = pa[pai % 4]; pai += 1
                for k in range(8):
                    P.mm(pA_[:], wA[:, k, n * 128:(n + 1) * 128], hm[:, k, :], start=(k == 0), stop=(k == 7))
                pB_ = pa[pai % 4]; pai += 1
                for k in range(4):
                    P.mm(pB_[:], wB[:, k, n * 128:(n + 1) * 128], ys2[:, k, :], start=(k == 0), stop=(k == 3))
                P.tt("vector", m1[:], pA_[:], G[:, n, :], ALU.mult)
                P.tt("vector", m2[:], pB_[:], G[:, 8 + n, :], ALU.mult)
                P.tt("vector", mg[:, n, :], m1[:], m2[:], ALU.add)
            def stA(s):
                nonlocal pai
                x_ = xt[s % 3]
                r0 = o0 + s * 128
                P.dma("sync", x_[:], C.x[r0:r0 + 128, :])
                for hh in range(2):
                    pb = pa[pai % 4]; pai += 1
                    for k in range(8):
                        P.mm(pb[:], mg[:, k, s * 128:(s + 1) * 128], wO[:, k, hh * 512:(hh + 1) * 512], start=(k == 0), stop=(k == 7))
                    P.tt("vector", x_[:, hh * 512:(hh + 1) * 512], x_[:, hh * 512:(hh + 1) * 512], pb[:], ALU.add)
                P.dma("sync", C.x1[r0:r0 + 128, :], x_[:])

            def stB(s):
                x_ = xt[s % 3]
                norm_T(P, K, x_[:], K.S2[:], K.SH2[:], h2s[:, :, s * 128:(s + 1) * 128], W, need_f32=h2f[:])
                for k in range(8):
                    P.mm(pr[:, 0:32], h2f[:, k, :], wR[:, k, :], start=(k == 0), stop=(k == 7))
                P.tt("vector", lg[:], pr[:, 0:32], bR[:], ALU.add)
                P.op("vector", lambda E: E.max(m8.h[:], lg.h[:]), [lg[:]], [m8[:]])
                P.ts("vector", msk[:], lg[:], m8[:, 3:4], ALU.is_ge)
                P.ts("vector", nm[:], m8[:, 0:1], -1.0, ALU.mult)
                P.act(ex[:], lg[:], AF.Exp, bias=nm[:])
                P.tt("vector", ex[:], ex[:], msk[:], ALU.mult)
                P.op("vector", lambda E: E.reduce_sum(ssum.h[:], ex.h[:], AX.X), [ex[:]], [ssum[:]])
                P.op("vector", lambda E: E.reciprocal(ssum.h[:], ssum.h[:]), [ssum[:]], [ssum[:]])
                P.ts("vector", gts[:, s, :], ex[:], ssum[:], ALU.mult)
                P.tr(pgT[0:32, s * 128:(s + 1) * 128], gts[:, s, :], K.ident)

            stA(0); stA(1); stB(0); stA(2); stB(1); stA(3); stB(2); stB(3)
            P.cp("vector", gT[:], pgT[0:32, :])
            P.dma("sync", V(C.h2T.h[:, o0:o0 + 512].rearrange("(j p) t -> p j t", p=128), C.h2T.name), h2s[:])
            P.dma("sync", V(C.gates.h[o0:o0 + 512, :].rearrange("(s p) e -> p s e", p=128), C.gates.name), gts[:])
            P.dma("sync", C.gatesT[:, o0:o0 + 512], gT[:])
        P.barrier(); P.flush()


def phase7(P, C, K):
    with ExitStack() as ph:
        P.stack = ph
        bein = P.sb("bein", [128, 32, 16], F32); P.dma("sync", bein[:], V(C.b_ein.h.rearrange("p (e j) -> p e j", j=16), C.b_ein.name))
        bein1 = P.sb("bein1", [128, 32, 8], F32)
        P.ts("vector", bein1[:], bein[:, :, 8:16], 1.0, ALU.add)
        beo = P.sb("beo", [32, 1024], F32); P.dma("sync", beo[:], C.b_eout[:])
        gfin = P.sb("gfin", [128, 1024], F32); P.dma("sync", gfin[:], C.gfin[:])
        Win = [P.sb("Win%d" % i, [128, 8, 2048], BF16) for i in range(2)]
        Wout = [P.sb("Wout%d" % i, [128, 8, 1024], BF16) for i in range(2)]
        h2 = P.sb("p7h2", [128, 8, 1024], BF16)
        acc = P.sb("p7acc", [128, 8, 1024], F32)
        gt_ = P.sb("p7g", [128, 8, 32], F32); gTt = P.sb("p7gT", [32, 1024], F32)
        actT = [P.sb("p7act%d" % i, [128, 8, 512], BF16) for i in range(2)]
        tg = [P.sb("p7tg%d" % i, [128, 512], F32) for i in range(2)]; ts_ = [P.sb("p7ts%d" % i, [128, 512], F32) for i in range(2)]
        tl = [P.sb("p7tl%d" % i, [128, 512], F32) for i in range(2)]
        x1t = [P.sb("p7x1", [128, 1024], F32)] * 2
        fs = {n: P.sb("p7_" + n, [128, 1], F32) for n in ["ss", "ms", "sq", "r"]}
        pz = [P.ps("p7pz%d" % i, [128, 512]) for i in range(4)]
        po = [P.ps("p7po%d" % i, [128, 512]) for i in range(4)]
        zi = 0; oi = 0; ti = 0; wi = 0
        ck = ("cvt", "sw")
        for eng_ in ("sync", "gpsimd"):
            P._wait(eng_, ("D", ck))
        P.persist.discard(ck)
        rr = lambda a: a.rearrange("(k p) n -> p k n", p=128)
        for grp in range(4):
            g0 = grp * 1024
            P.dma("sync", h2[:], V(C.h2T.h[:, g0:g0 + 1024].rearrange("(j p) t -> p j t", p=128), C.h2T.name))
            P.dma("sync", gt_[:], V(C.gates.h[g0:g0 + 1024, :].rearrange("(s p) e -> p s e", p=128), C.gates.name))
            P.dma("sync", gTt[:], C.gatesT[:, g0:g0 + 1024])
            P.ts("vector", gt_[:], gt_[:], 1.0 / 1.702, ALU.mult)
            for s in range(8):
                for hh in range(2):
                    pb = po[oi % 4]; oi += 1
                    P.mm(pb[:], gTt[:, s * 128:(s + 1) * 128], beo[:, hh * 512:(hh + 1) * 512])
                    P.cp("scalar", acc[:, s, hh * 512:(hh + 1) * 512], pb[:])
            for e in range(NE):
                wi_, wo_ = Win[wi % 2], Wout[wi % 2]; wi += 1
                for kk in range(4):
                    P.dma("sync" if kk < 2 else "gpsimd", wi_.k(("w", kk))[:, 2 * kk:2 * kk + 2, :],
                          V(rr(C.weinb.h[e, kk * 256:(kk + 1) * 256, :]), C.weinb.name))
                for kk in range(2):
                    P.dma("sync" if kk == 0 else "gpsimd", wo_.k(("w", kk))[:, 4 * kk:4 * kk + 4, :],
                          V(rr(C.weoutb.h[e, kk * 512:(kk + 1) * 512, :]), C.weoutb.name))
                for tt in range(2):
                    aT = actT[ti % 2]; ti += 1
                    for jn in range(8):
                        pg_ = pz[zi % 4]; pl_ = pz[(zi + 1) % 4]; zi += 2
                        for k in range(8):
                            P.mm(pg_[:], wi_[:, k, jn * 128:(jn + 1) * 128], h2[:, k, tt * 512:(tt + 1) * 512], start=(k == 0), stop=(k == 7))
                        for k in range(8):
                            P.mm(pl_[:], wi_[:, k, 1024 + jn * 128:1024 + (jn + 1) * 128], h2[:, k, tt * 512:(tt + 1) * 512], start=(k == 0), stop=(k == 7))
                        b = jn % 2
                        P.ts("vector", tg[b][:], pg_[:], bein[:, e, jn:jn + 1], ALU.add, 7.0, ALU.min)
                        P.act(ts_[b][:], tg[b][:], AF.Silu, scale=1.702)
                        P.ts("vector", tl[b][:], pl_[:], bein1[:, e, jn:jn + 1], ALU.add, -6.0, ALU.max)
                        P.stt("vector", aT[:, jn, :], tl[b][:], 8.0, ts_[b][:], ALU.min, ALU.mult)
                    for s in range(4):
                        sub = tt * 4 + s
                        for hh in range(2):
                            pb = po[oi % 4]; oi += 1
                            for k in range(8):
                                P.mm(pb[:], aT[:, k, s * 128:(s + 1) * 128], wo_[:, k, hh * 512:(hh + 1) * 512], start=(k == 0), stop=(k == 7))
                            av = V(acc.h[:, sub, hh * 512:(hh + 1) * 512], acc.name, (sub, hh))
                            P.stt("vector", av, pb[:], gt_[:, sub, e:e + 1], av, ALU.mult, ALU.add)
            for s in range(8):
                r0 = g0 + s * 128
                x_ = x1t[s % 2]
                P.dma("sync", x_[:], C.x1[r0:r0 + 128, :])
                P.tt("vector", acc[:, s, :], acc[:, s, :], K.gt2bc[:], ALU.mult)
                P.tt("vector", x_[:], x_[:], acc[:, s, :], ALU.add)
                P.act(actT[0][:, 0:2, :].m(lambda a: a.rearrange("p a b -> p (a b)")), x_[:], AF.Square, accum=fs["ss"][:])
                P.ts("vector", fs["ms"][:], fs["ss"][:], 1.0 / D, ALU.mult, EPS, ALU.add)
                P.act(fs["sq"][:], fs["ms"][:], AF.Sqrt)
                P.op("vector", lambda E: E.reciprocal(fs["r"].h[:], fs["sq"].h[:]), [fs["sq"][:]], [fs["r"][:]])
                P.act(x_[:], x_[:], AF.Identity, scale=fs["r"][:])
                P.tt("vector", x_[:], x_[:], gfin[:], ALU.mult)
                P.dma("sync", C.out[r0:r0 + 128, :], x_[:])
        P.barrier(); P.flush()

PHASES = 99
DBG = ()


def build_program(phases=99, dbg=(), only=None):
    nc = bass.Bass("TRN2", target_bir_lowering=False)
    with ExitStack() as outer:
        P = Prog(nc, outer)
        C = declare_io(P, dbg)
        K = phase0(P, C)
        if phases >= 1 and (only is None or 1 in only):
            phase1(P, C, K)
        if phases >= 2 and (only is None or 2 in only):
            phase2(P, C, K)
        if phases >= 3 and (only is None or 3 in only):
            phase3(P, C, K)
        if phases >= 4 and (only is None or 4 in only):
            phase4(P, C, K)
        if phases >= 5 and (only is None or 5 in only):
            phase5(P, C, K)
        if phases >= 6 and (only is None or 6 in only):
            phase6(P, C, K)
        if phases >= 7 and (only is None or 7 in only):
            phase7(P, C, K)
        P.stack = outer
        P.finish([C.out[:]])
        P.flush()
        print("instr counts", P.cnt, "waits", P.nwaits, "dma sems", {k: (len(v), max([x[1] for x in v] + [0])) for k, v in P.free_d.items()})
    return nc


_NC_CACHE = {}


def kernel(**inputs):
    inp = {k: np.asarray(v) for k, v in inputs.items()}
    key = (PHASES, DBG)
    if key not in _NC_CACHE:
        _NC_CACHE[key] = build_program(PHASES, DBG)
    nc = _NC_CACHE[key]
    in_maps = [host_prep(inp, b) for b in range(8)]
    res = run_bass_kernel_spmd(nc, in_maps, core_ids=list(range(8)))
    kernel.last = res
    out = np.stack([np.asarray(res.results[b]["dr_out"]) for b in range(8)], axis=0)
    return out.astype(np.float32)
```

```python
import numpy as np
import concourse.bass as bass
import concourse.mybir as mybir
from concourse.bass_utils import run_bass_kernel_spmd

F32 = mybir.dt.float32
BF16 = mybir.dt.bfloat16
ALU = mybir.AluOpType
AF = mybir.ActivationFunctionType
AX = mybir.AxisListType

ENGS = ["tensor", "vector", "scalar", "gpsimd", "sync"]
EPOCH = 16000


class V:
    __slots__ = ("ap", "name", "key")

    def __init__(self, ap, name, key=None):
        self.ap = ap
        self.name = name
        self.key = key

    def m(self, fn):
        return V(fn(self.ap), self.name, self.key)


class _TK:
    def __init__(self, t, key):
        self.t = t
        self.key = key

    def __getitem__(self, idx):
        return V(self.t.h[idx], self.t.name, self.key)


class T:
    def __init__(self, h, name):
        self.h = h
        self.name = name

    def __getitem__(self, idx):
        return V(self.h[idx], self.name, None)

    def k(self, key):
        return _TK(self, key)


class Prog:
    def __init__(self, nc, stack):
        self.nc = nc
        self.stack = stack
        self.semstack = stack
        self.items = {e: [] for e in ENGS}
        self.cnt = {e: 0 for e in ENGS}
        self.esems = {e: [] for e in ENGS}
        self.state = {}
        self.waited = {e: {} for e in ENGS}
        self.dsem = {}
        self.ntiles = 0
        self.nwaits = 0
        self.persist = set()

    def sb(self, name, shape, dt):
        h = self.stack.enter_context(self.nc.sbuf_tensor(name, list(shape), dt))
        return T(h, name)

    def ps(self, name, shape, dt=F32):
        h = self.stack.enter_context(self.nc.psum_tensor(name, list(shape), dt))
        return T(h, name)

    def dram(self, name, shape, dt, kind="Internal"):
        h = self.nc.dram_tensor(name, list(shape), dt, kind=kind)
        return T(h.ap() if hasattr(h, "ap") else h, name)

    def _esem(self, e, ep):
        while len(self.esems[e]) <= ep:
            s = self.semstack.enter_context(self.nc.semaphore("s_%s_%d" % (e, len(self.esems[e]))))
            self.esems[e].append(s)
        return self.esems[e][ep]

    def _dsem(self, key):
        if key not in self.dsem:
            if not hasattr(self, "free_d"):
                self.free_d = {"sw": [], "hw": []}
            fd = self.free_d[key[1]]
            if fd:
                fd.sort(key=lambda x: x[1])
                self.dsem[key] = fd.pop(0)
            else:
                self.ndsem = getattr(self, "ndsem", 0) + 1
                s = self.semstack.enter_context(self.nc.semaphore("d_%d" % self.ndsem))
                self.dsem[key] = [s, 0]
        return self.dsem[key]

    def _states(self, name, key):
        d = self.state.setdefault(name, {})
        if key is None:
            if None not in d:
                d[None] = [None, {}]
            return list(d.values())
        if key not in d:
            d[key] = [None, {}]
        out = [d[key]]
        if None in d:
            out.append(d[None])
        return out

    def _wait(self, eng, dep):
        if dep is None:
            return
        if dep[0] == "E":
            _, e2, idx = dep
            if e2 == eng and eng == "tensor":
                return
            ep = (idx - 1) // EPOCH
            val = idx - ep * EPOCH
            sem = self._esem(e2, ep)
            sk = ("E", e2, ep)
        else:
            _, dk = dep
            sem, val = self.dsem[dk]
            sk = ("D", id(sem))
        w = self.waited[eng]
        if w.get(sk, 0) >= val:
            return
        w[sk] = val
        self.nwaits += 1
        self.items[eng].append(lambda E, sem=sem, val=val: E.wait_ge(sem, val))

    def _deps(self, eng, reads, writes):
        for v in reads:
            for st in self._states(v.name, v.key):
                self._wait(eng, st[0])
        for v in writes:
            for st in self._states(v.name, v.key):
                self._wait(eng, st[0])
                for r in st[1].values():
                    self._wait(eng, r)

    def _record(self, dep, reads, writes):
        for v in reads:
            d = self.state.setdefault(v.name, {})
            if v.key not in d:
                d[v.key] = [None, {}]
            d[v.key][1][dep[:2]] = dep
        for v in writes:
            d = self.state.setdefault(v.name, {})
            if v.key is None:
                for k in list(d.keys()):
                    d[k] = [dep, {}]
                d[None] = [dep, {}]
            else:
                d[v.key] = [dep, {}]

    def op(self, eng, fn, reads, writes):
        reads = [r for r in reads if isinstance(r, V)]
        writes = [w for w in writes if isinstance(w, V)]
        self._deps(eng, reads, writes)
        self.cnt[eng] += 1
        idx = self.cnt[eng]
        ep = (idx - 1) // EPOCH
        sem = self._esem(eng, ep)
        self.items[eng].append(lambda E, fn=fn, sem=sem: fn(E).then_inc(sem, 1))
        self._record(("E", eng, idx), reads, writes)

    def dma(self, eng, out, in_, semkey=None, **kw):
        if semkey is None:
            semkey = out.name if not out.name.startswith("dr_") else in_.name
        if eng in ("scalar", "vector"):
            semkey = "%s_%s" % (semkey, eng)
        semkey = (semkey, "sw" if eng == "gpsimd" else "hw")
        self._deps(eng, [in_], [out])
        ds = self._dsem(semkey)
        ds[1] += 16
        assert ds[1] < 60000, semkey
        sem = ds[0]
        o, i = out.ap, in_.ap
        self.items[eng].append(lambda E, o=o, i=i, sem=sem, kw=kw: E.dma_start(out=o, in_=i, **kw).then_inc(sem, 16))
        self._record(("D", semkey), [in_], [out])

    def mm(self, out, lhsT, rhs, start=True, stop=True):
        self.op("tensor", lambda E: E.matmul(out.ap, lhsT.ap, rhs.ap, start=start, stop=stop),
                [lhsT, rhs] + ([] if start else [out]), [out])

    def tr(self, out, in_, ident):
        self.op("tensor", lambda E: E.transpose(out.ap, in_.ap, ident.ap), [in_, ident], [out])

    def act(self, out, in_, func, bias=0.0, scale=1.0, accum=None, eng="scalar"):
        b = bias.ap if isinstance(bias, V) else bias
        s = scale.ap if isinstance(scale, V) else scale
        kw = {}
        if accum is not None:
            kw["accum_out"] = accum.ap
        self.op("scalar", lambda E: E.activation(out.ap, in_.ap, func, bias=b, scale=s, **kw),
                [in_, bias, scale], [out] + ([accum] if accum is not None else []))

    def tt(self, eng, out, a, b, op):
        self.op(eng, lambda E: E.tensor_tensor(out.ap, a.ap, b.ap, op), [a, b], [out])

    def ts(self, eng, out, a, s1, op0, s2=None, op1=None, accum=None):
        x1 = s1.ap if isinstance(s1, V) else s1
        x2 = s2.ap if isinstance(s2, V) else s2
        kw = {}
        if op1 is not None:
            kw["op1"] = op1
        if accum is not None:
            kw["accum_out"] = accum.ap
        self.op(eng, lambda E: E.tensor_scalar(out.ap, a.ap, x1, x2, op0, **kw), [a, s1, s2],
                [out] + ([accum] if accum is not None else []))

    def stt(self, eng, out, a, s, b, op0, op1):
        x = s.ap if isinstance(s, V) else s
        self.op(eng, lambda E: E.scalar_tensor_tensor(out.ap, a.ap, x, b.ap, op0, op1), [a, s, b], [out])

    def cp(self, eng, out, in_):
        if eng == "scalar":
            self.op(eng, lambda E: E.copy(out.ap, in_.ap), [in_], [out])
        else:
            self.op(eng, lambda E: E.tensor_copy(out.ap, in_.ap), [in_], [out])

    def memset(self, eng, out, val):
        self.op(eng, lambda E: E.memset(out.ap, val), [], [out])

    def finish(self, outs):
        for v in outs:
            for st in self._states(v.name, v.key):
                self._wait("sync", st[0])
        for e in ENGS:
            if self.cnt[e] > 0:
                self._wait("sync", ("E", e, self.cnt[e]))
        for dk in list(self.dsem.keys()):
            self._wait("sync", ("D", dk))

    def barrier(self):
        for e in ENGS:
            for e2 in ENGS:
                if self.cnt[e2] > 0:
                    self._wait(e, ("E", e2, self.cnt[e2]))
            for dk in list(self.dsem.keys()):
                if dk in self.persist:
                    continue
                self._wait(e, ("D", dk))
        self.state = {}
        if not hasattr(self, "free_d"):
            self.free_d = {"sw": [], "hw": []}
        for k, v in self.dsem.items():
            if k in self.persist:
                continue
            self.free_d[k[1]].append(v)
        self.dsem = {k: v for k, v in self.dsem.items() if k in self.persist}

    def flush(self):
        self.build()
        self.items = {e: [] for e in ENGS}

    def build(self):
        nc = self.nc
        with nc.Block() as block:
            for e in ENGS:
                items = self.items[e]
                if not items:
                    continue

                def body(E, items=items):
                    for it in items:
                        it(E)
                getattr(block, e)(body)
from contextlib import ExitStack
import ml_dtypes

D = 1024
L = 4096
LC = 256
LT = L + LC
NE = 32
OFF_QK, OFF_V, OFF_IF, OFF_U, OFF_O, OFF_G, IN_COLS = 0, 1024, 2048, 2080, 2592, 3616, 5664
EPS = 1e-6


def host_prep(inp, b):
    f = np.float32
    A = lambda a: np.ascontiguousarray(a, dtype=f)
    m = {}
    m["dr_x"] = A(inp["x"][b])
    m["dr_ctx"] = A(inp["ctx"][b])
    cc = np.stack([inp["c"][b].reshape(8, 128).T, inp["c_ctx"].reshape(8, 128).T], axis=-1)
    m["dr_cc"] = A(cc)
    m["dr_w_ada"] = A(inp["w_ada"][0])
    m["dr_b_ada"] = A(inp["b_ada"][0].reshape(48, 128).T)
    m["dr_g1"] = A(inp["g_norm1"][0].reshape(8, 128).T)
    m["dr_g2"] = A(inp["g_norm2"][0].reshape(8, 128).T)
    m["dr_w_in"] = A(inp["w_in"][0])
    m["dr_wconv"] = A(inp["w_conv_qk"][0].reshape(9, 8, 128).transpose(2, 1, 0))
    m["dr_bif"] = A(np.broadcast_to(inp["b_ifgate"][0].reshape(1, 32), (128, 32)))
    m["dr_gmh"] = A(np.broadcast_to(inp["g_mh"][0].reshape(1, 1024), (128, 1024)))
    m["dr_w_a"] = A(inp["w_branch_m"][0])
    row = np.zeros((2, 3, 128, 2048), f)
    col = np.zeros((2, 3, 128, 16), f)
    for d in range(2):
        ldt = np.repeat(inp["s5_log_dt"][0, d], 64)
        for i, arr in enumerate([inp["s5_a_re"][0, d].reshape(-1), inp["s5_a_im"][0, d].reshape(-1), ldt]):
            row[d, i] = np.broadcast_to(arr.reshape(1, 2048), (128, 2048))
            col[d, i] = arr.reshape(16, 128).T
    m["dr_s5row"] = row
    m["dr_s5col"] = col
    bbd = np.zeros((2, 2, 128, 4, 512), f)
    cbd = np.zeros((2, 2, 128, 16, 128), f)
    for d in range(2):
        for ri, (bsrc, csrc) in enumerate([(inp["s5_b_re"], inp["s5_c_re"]), (inp["s5_b_im"], inp["s5_c_im"])]):
            for g in range(32):
                j, gl = divmod(g, 8)
                bbd[d, ri, gl * 16:(gl + 1) * 16, j, gl * 64:(gl + 1) * 64] = bsrc[0, d, g].T
                q, g2 = divmod(g, 2)
                cbd[d, ri, g2 * 64:(g2 + 1) * 64, q, (q % 4) * 32 + g2 * 16:(q % 4) * 32 + g2 * 16 + 16] = csrc[0, d, g].T
    m["dr_bbd"] = bbd.reshape(2, 2, 128, 2048)
    m["dr_cbd"] = cbd.reshape(2, 2, 128, 2048)
    m["dr_s5d"] = A(inp["s5_d"][0].reshape(4, 128).T)
    m["dr_w_glu"] = A(inp["w_glu"][0])
    m["dr_b_glu"] = A(inp["b_glu"][0].reshape(4, 128).T)
    m["dr_w_b"] = A(inp["w_branch_s"][0])
    m["dr_b_gate"] = A(inp["b_merge_gate"][0].reshape(16, 128).T)
    m["dr_w_o"] = A(inp["w_o"][0])
    m["dr_w_r"] = A(inp["w_router"][0])
    m["dr_b_r"] = A(np.broadcast_to(inp["b_router"][0].reshape(1, 32), (128, 32)))
    m["dr_w_ein"] = A(inp["w_e_in"][0])
    m["dr_b_ein"] = A(inp["b_e_in"][0].reshape(32, 16, 128).transpose(2, 0, 1).reshape(128, 512))
    m["dr_w_eout"] = A(inp["w_e_out"][0])
    m["dr_b_eout"] = A(inp["b_e_out"][0])
    m["dr_gfin"] = A(np.broadcast_to(inp["g_final"].reshape(1, 1024), (128, 1024)))
    cst = np.zeros((128, 7, 128), f)
    ii = np.arange(128)
    cst[:, 0] = np.eye(128)
    cst[:, 1] = (ii[:, None] <= ii[None, :])
    cst[:, 2] = (ii[:, None] >= ii[None, :])
    cst[:, 3] = 1.0
    cst[:, 4] = ii[None, :]
    cst[:, 5] = 127 - ii[None, :]
    cst[:, 6, 0] = ii
    cst[:, 6, 1] = 127 - ii
    m["dr_cst"] = cst
    return m

class Ctx:
    pass


def declare_io(P, dbg):
    C = Ctx()
    def din(name, shape, dt=F32):
        return P.dram("dr_" + name, shape, dt, kind="ExternalInput")
    C.x = din("x", [L, D]); C.ctx = din("ctx", [LC, D]); C.cc = din("cc", [128, 8, 2])
    C.w_ada = din("w_ada", [D, 6144]); C.b_ada = din("b_ada", [128, 48])
    C.g1 = din("g1", [128, 8]); C.g2 = din("g2", [128, 8]); C.w_in = din("w_in", [D, IN_COLS])
    C.wconv = din("wconv", [128, 8, 9]); C.bif = din("bif", [128, 32]); C.gmh = din("gmh", [128, 1024])
    C.w_a = din("w_a", [D, D]); C.s5row = din("s5row", [2, 3, 128, 2048]); C.s5col = din("s5col", [2, 3, 128, 16])
    C.bbd = din("bbd", [2, 2, 128, 2048]); C.cbd = din("cbd", [2, 2, 128, 2048]); C.s5d = din("s5d", [128, 4])
    C.w_glu = din("w_glu", [512, 512]); C.b_glu = din("b_glu", [128, 4]); C.w_b = din("w_b", [512, D])
    C.b_gate = din("b_gate", [128, 16]); C.w_o = din("w_o", [D, D]); C.w_r = din("w_r", [D, 32])
    C.b_r = din("b_r", [128, 32]); C.w_ein = din("w_ein", [NE, D, 2048]); C.b_ein = din("b_ein", [128, 512])
    C.w_eout = din("w_eout", [NE, D, D]); C.b_eout = din("b_eout", [NE, D]); C.gfin = din("gfin", [128, 1024])
    C.cst = din("cst", [128, 7, 128])
    C.out = P.dram("dr_out", [L, D], F32, kind="ExternalOutput")
    def scr(name, shape, dt):
        kind = "ExternalOutput" if name in dbg else "Internal"
        return P.dram("dr_s_" + name, shape, dt, kind=kind)
    C.qkpre = scr("qkpre", [1024, LT], BF16)
    C.v = scr("v", [LT, 1024], BF16)
    C.gl = scr("gl", [LT, 32], F32)
    C.uT = scr("uT", [512, LT], BF16)
    C.GT = scr("GT", [2048, L], BF16)
    C.og = scr("og", [L, 1024], BF16)
    C.qT = scr("qT", [512, LT], BF16)
    C.kT = scr("kT", [512, LT], BF16)
    C.ktok = scr("ktok", [LT, 512], BF16)
    C.hd = [scr("hf", [L, 1024], F32), scr("hb", [L, 1024], F32)]
    C.hmT = scr("hmT", [1024, L], BF16)
    C.yT = [scr("yTf", [512, L], F32), scr("yTb", [512, L], F32)]
    C.x1 = scr("x1", [L, D], F32)
    C.h2T = scr("h2T", [1024, L], BF16)
    C.gates = scr("gates", [L, 32], F32)
    C.gatesT = scr("gatesT", [32, L], F32)
    C.modT = scr("modT", [128, 96], F32)
    C.weinb = scr("weinb", [NE, D, 2048], BF16)
    C.weoutb = scr("weoutb", [NE, D, D], BF16)
    return C


def bc(v, shape, axis_pat):
    return v.m(lambda a: a.rearrange(axis_pat, o=1).to_broadcast(shape))


def phase0(P, C):
    K = Ctx()
    K.cst = P.sb("cst", [128, 7, 128], F32)
    P.dma("sync", K.cst[:], C.cst[:])
    K.ident = K.cst[:, 0, :]; K.triF = K.cst[:, 1, :]; K.triB = K.cst[:, 2, :]; K.ones = K.cst[:, 3, :]
    K.S1 = P.sb("S1", [128, 8], F32); K.SH1 = P.sb("SH1", [128, 8], F32)
    K.S1c = P.sb("S1c", [128, 8], F32); K.SH1c = P.sb("SH1c", [128, 8], F32)
    K.S2 = P.sb("S2", [128, 8], F32); K.SH2 = P.sb("SH2", [128, 8], F32)
    K.gt1bc = P.sb("gt1bc", [128, 1024], F32); K.gt2bc = P.sb("gt2bc", [128, 1024], F32)
    with ExitStack() as ph:
        P.stack = ph
        cc = P.sb("cc", [128, 8, 2], F32); sc = P.sb("sc", [128, 8, 2], F32)
        bada = P.sb("bada", [128, 48], F32); g1 = P.sb("g1", [128, 8], F32); g2 = P.sb("g2", [128, 8], F32)
        modT = P.sb("modT", [128, 48, 2], F32); tmp8 = P.sb("tmp8", [128, 8], F32)
        gt = P.sb("gt", [128, 16], F32)
        wa = [P.sb("wa%d" % i, [128, 8, 512], F32) for i in range(2)]
        diag = [P.sb("diag%d" % i, [128, 128], F32) for i in range(2)]
        pm = P.ps("pm", [128, 512]); pg = P.ps("pg", [128, 1024])
        P.dma("sync", cc[:], C.cc[:]); P.dma("sync", bada[:], C.b_ada[:])
        P.dma("sync", g1[:], C.g1[:]); P.dma("sync", g2[:], C.g2[:])
        P.act(sc[:], cc[:], AF.Silu)
        wv = C.w_ada[:].m(lambda a: a.rearrange("(k p) n -> p k n", p=128))
        for i in range(12):
            w = wa[i % 2]
            P.dma("sync" if i % 2 == 0 else "gpsimd", w[:], V(wv.ap[:, :, i * 512:(i + 1) * 512], wv.name))
            for mI in range(4):
                nn = 4 * i + mI
                for k in range(8):
                    P.mm(pm[:, nn * 2:nn * 2 + 2], w[:, k, mI * 128:(mI + 1) * 128], sc[:, k, :], start=(k == 0), stop=(k == 7))
        P.tt("vector", modT[:], pm[:, 0:96].m(lambda a: a.rearrange("p (n t) -> p n t", t=2)),
             bada[:].m(lambda a: a.rearrange("p (n o) -> p n o", o=1).to_broadcast([128, 48, 2])), ALU.add)
        P.cp("vector", K.SH1[:], modT[:, 0:8, 0]); P.cp("vector", K.SH1c[:], modT[:, 0:8, 1])
        P.cp("vector", K.SH2[:], modT[:, 24:32, 0])
        P.ts("vector", tmp8[:], modT[:, 8:16, 0], 1.0, ALU.add); P.tt("vector", K.S1[:], tmp8[:], g1[:], ALU.mult)
        P.ts("vector", tmp8[:], modT[:, 8:16, 1], 1.0, ALU.add); P.tt("vector", K.S1c[:], tmp8[:], g1[:], ALU.mult)
        P.ts("vector", tmp8[:], modT[:, 32:40, 0], 1.0, ALU.add); P.tt("vector", K.S2[:], tmp8[:], g2[:], ALU.mult)
        P.cp("vector", gt[:, 0:8], modT[:, 16:24, 0]); P.cp("vector", gt[:, 8:16], modT[:, 40:48, 0])
        for j in range(16):
            dg = diag[j % 2]
            P.ts("vector", dg[:], K.ident, gt[:, j:j + 1], ALU.mult)
            P.mm(pg[:, (j % 8) * 128:(j % 8 + 1) * 128], K.ones, dg[:])
            if j == 7:
                P.cp("vector", K.gt1bc[:], pg[:])
            if j == 15:
                P.cp("vector", K.gt2bc[:], pg[:])
        P.dma("sync", C.modT[:], modT[:].m(lambda a: a.rearrange("p n t -> p (n t)")))
        P.barrier(); P.flush()
    return K


def norm_T(P, K, xt, S, SH, hT_dst, W, need_f32=None):
    P.act(W["junk"][:], xt, AF.Square, accum=W["ss"][:])
    P.ts("vector", W["ms"][:], W["ss"][:], 1.0 / D, ALU.mult, EPS, ALU.add)
    P.act(W["sq"][:], W["ms"][:], AF.Sqrt)
    P.op("vector", lambda E: E.reciprocal(W["rstd"].h[:], W["sq"].h[:]), [W["sq"][:]], [W["rstd"][:]])
    P.act(W["xn"][:], xt, AF.Identity, scale=W["rstd"][:])
    for j in range(8):
        P.tr(W["psT"][:, j * 128:(j + 1) * 128], W["xn"][:, j * 128:(j + 1) * 128], K.ident)
    for j in range(8):
        pj = W["psT"][:, j * 128:(j + 1) * 128]
        sj = V(S.ap[:, j:j + 1], S.name, S.key); bj = V(SH.ap[:, j:j + 1], SH.name, SH.key)
        if need_f32 is not None:
            nf = V(need_f32.ap[:, j, :], need_f32.name, need_f32.key)
            P.act(nf, pj, AF.Identity, bias=bj, scale=sj)
        else:
            P.act(V(hT_dst.ap[:, j, :], hT_dst.name, hT_dst.key), pj, AF.Identity, bias=bj, scale=sj)
    if need_f32 is not None:
        P.cp("vector", hT_dst, need_f32)


def norm_work(P, sfx=""):
    W = {}
    W["junk"] = P.sb("nw_junk" + sfx, [128, 1024], BF16)
    W["xn"] = P.sb("nw_xn" + sfx, [128, 1024], F32)
    W["tm"] = P.sb("nw_tm" + sfx, [128, 8, 128], F32)
    for n in ["ss", "ms", "sq", "rstd"]:
        W[n] = P.sb("nw_" + n + sfx, [128, 1], F32)
    W["psT"] = P.ps("nw_psT" + sfx, [128, 1024])
    return W


def phase1(P, C, K):
    with ExitStack() as ph:
        P.stack = ph
        win = P.sb("win", [128, 8, IN_COLS], BF16)
        for k in range(8):
            P.dma("gpsimd", win[:, k, :], C.w_in[k * 128:(k + 1) * 128, :])
        rr = lambda a: a.rearrange("(k p) n -> p k n", p=128)
        for e in range(NE):
            for kk in range(4):
                P.dma("gpsimd", V(rr(C.weinb.h[e, kk * 256:(kk + 1) * 256, :]), C.weinb.name),
                      V(rr(C.w_ein.h[e, kk * 256:(kk + 1) * 256, :]), C.w_ein.name), semkey="cvt")
            for kk in range(2):
                P.dma("gpsimd", V(rr(C.weoutb.h[e, kk * 512:(kk + 1) * 512, :]), C.weoutb.name),
                      V(rr(C.w_eout.h[e, kk * 512:(kk + 1) * 512, :]), C.w_eout.name), semkey="cvt")
        P.persist.add(("cvt", "sw"))
        bif = P.sb("bif", [128, 32], F32); P.dma("sync", bif[:], C.bif[:])
        bgate = P.sb("bgate", [128, 16], F32); P.dma("sync", bgate[:], C.b_gate[:])
        W = norm_work(P)
        xt = [P.sb("xt%d" % i, [128, 1024], F32) for i in range(2)]
        hT = [P.sb("hT%d" % i, [128, 8, 512], BF16) for i in range(2)]
        pA = [P.ps("pA%d" % i, [128, 512]) for i in range(4)]
        st_qk = [P.sb("st_qk%d" % i, [128, 8, 512], BF16) for i in range(2)]
        st_u = [P.sb("st_u%d" % i, [128, 4, 512], BF16) for i in range(2)]
        st_G = [P.sb("st_G%d" % i, [128, 8, 512], BF16) for i in range(2)]
        st_v = [P.sb("st_v%d" % i, [128, 1024], BF16) for i in range(2)]
        st_o = [P.sb("st_o%d" % i, [128, 1024], BF16) for i in range(2)]
        st_g = [P.sb("st_g%d" % i, [128, 4, 32], F32) for i in range(2)]
        gw = {n: P.sb("gw_" + n, [128, 32], F32) for n in ["g", "e", "sp"]}
        tiles = [(True, 0, 256, 0)] + [(False, i * 512, 512, LC + i * 512) for i in range(8)]
        pai = 0
        for ti, (isc, off, nt, lto) in enumerate(tiles):
            par = ti % 2
            nsub = nt // 128
            src = C.ctx if isc else C.x
            h = hT[par]
            for s in range(nsub):
                x_ = xt[(ti * 4 + s) % 2]
                P.dma("sync", x_[:], src[off + s * 128: off + (s + 1) * 128, :])
                norm_T(P, K, x_[:], K.S1c[:] if isc else K.S1[:], K.SH1c[:] if isc else K.SH1[:],
                       h[:, :, s * 128:(s + 1) * 128], W)
            def fm(col0, ncol, stage, func, bias=None, evac="scalar"):
                nonlocal pai
                for m in range(ncol):
                    pb = pA[pai % 4]; pai += 1
                    for k in range(8):
                        P.mm(pb[:, 0:nt], win[:, k, col0 + m * 128: col0 + (m + 1) * 128], h[:, k, 0:nt], start=(k == 0), stop=(k == 7))
                    if func is None:
                        if m % 2 == 0:
                            P.cp("scalar", stage[:, m, 0:nt], pb[:, 0:nt])
                        else:
                            P.cp("vector", stage[:, m, 0:nt], pb[:, 0:nt])
                    else:
                        P.act(stage[:, m, 0:nt], pb[:, 0:nt], func, bias=V(bias.ap[:, m:m + 1], bias.name) if isinstance(bias, V) else bias[:, m:m + 1])
            fm(OFF_QK, 8, st_qk[par], None)
            P.dma("scalar", V(C.qkpre.h[:, lto:lto + nt].rearrange("(j p) t -> p j t", p=128), C.qkpre.name), st_qk[par][:, :, 0:nt])
            fm(OFF_U, 4, st_u[par], None)
            P.dma("scalar", V(C.uT.h[:, lto:lto + nt].rearrange("(j p) t -> p j t", p=128), C.uT.name), st_u[par][:, :, 0:nt])
            if not isc:
                for gh in range(2):
                    fm(OFF_G + gh * 1024, 8, st_G[gh], AF.Sigmoid, bias=V(bgate.h[:, gh * 8:(gh + 1) * 8], bgate.name))
                    P.dma("scalar", V(C.GT.h[gh * 1024:(gh + 1) * 1024, off:off + nt].rearrange("(j p) t -> p j t", p=128), C.GT.name), st_G[gh][:, :, 0:nt])
            for s in range(nsub):
                for hh in range(2):
                    pb = pA[pai % 4]; pai += 1
                    for k in range(8):
                        P.mm(pb[:], h[:, k, s * 128:(s + 1) * 128], win[:, k, OFF_V + hh * 512: OFF_V + (hh + 1) * 512], start=(k == 0), stop=(k == 7))
                    P.cp("vector", st_v[s % 2][:, hh * 512:(hh + 1) * 512], pb[:])
                P.dma("sync", C.v[lto + s * 128: lto + (s + 1) * 128, :], st_v[s % 2][:])
                pb = pA[pai % 4]; pai += 1
                for k in range(8):
                    P.mm(pb[:, 0:32], h[:, k, s * 128:(s + 1) * 128], win[:, k, OFF_IF:OFF_IF + 32], start=(k == 0), stop=(k == 7))
                g = gw["g"]
                P.tt("vector", g[:], pb[:, 0:32], bif[:], ALU.add)
                gv = g[:].m(lambda a: a.rearrange("p (d i h) -> p d i h", d=2, i=2))
                sg = st_g[par][:, s, :].m(lambda a: a.rearrange("p (i d h) -> p i d h", i=2, d=2))
                P.cp("vector", V(sg.ap[:, 0], sg.name), V(gv.ap[:, :, 0, :], gv.name))
                P.act(gw["e"][:], g[:], AF.Exp, scale=-1.0)
                P.act(gw["sp"][:], gw["e"][:], AF.Ln, bias=1.0)
                spv = gw["sp"][:].m(lambda a: a.rearrange("p (d i h) -> p d i h", d=2, i=2))
                P.ts("vector", V(sg.ap[:, 1], sg.name), V(spv.ap[:, :, 1, :], spv.name), -1.0, ALU.mult)
                if not isc:
                    for hh in range(2):
                        pb = pA[pai % 4]; pai += 1
                        for k in range(8):
                            P.mm(pb[:], h[:, k, s * 128:(s + 1) * 128], win[:, k, OFF_O + hh * 512: OFF_O + (hh + 1) * 512], start=(k == 0), stop=(k == 7))
                        P.act(st_o[s % 2][:, hh * 512:(hh + 1) * 512], pb[:], AF.Sigmoid)
                    P.dma("scalar", C.og[off + s * 128: off + (s + 1) * 128, :], st_o[s % 2][:])
            P.dma("sync", V(C.gl.h[lto:lto + nt, :].rearrange("(s p) c -> p s c", p=128), C.gl.name), st_g[par][:, 0:nsub, :])
        P.barrier(); P.flush()

def phase2(P, C, K):
    with ExitStack() as ph:
        P.stack = ph
        wc = P.sb("wc", [128, 8, 9], F32); P.dma("sync", wc[:], C.wconv[:])
        pre = [P.sb("pre%d" % i, [128, LT], BF16) for i in range(2)]
        acc = [P.sb("cacc%d" % i, [128, LT], F32) for i in range(2)]
        sl = [P.sb("csl%d" % i, [128, LT], F32) for i in range(2)]
        ob = [P.sb("cob%d" % i, [128, LT], BF16) for i in range(2)]
        kst = [P.sb("kst%d" % i, [128, 4, 128], BF16) for i in range(2)]
        pT = [P.ps("pT%d" % i, [128, 512]) for i in range(2)]
        for j in range(8):
            e = "vector"
            p_, a_, s_, o_ = pre[j % 2], acc[j % 2], sl[j % 2], ob[j % 2]
            P.dma("sync", p_[:], C.qkpre[j * 128:(j + 1) * 128, :])
            def w(t):
                return wc[:, j, t:t + 1]
            P.ts(e, a_[:, 0:LC], p_[:, 0:LC], w(4), ALU.mult)
            P.stt(e, a_[:, 1:LC], p_[:, 0:LC - 1], w(3), a_[:, 1:LC], ALU.mult, ALU.add)
            P.stt(e, a_[:, 0:LC - 1], p_[:, 1:LC], w(5), a_[:, 0:LC - 1], ALU.mult, ALU.add)
            pv = p_[:, LC:LT].m(lambda a: a.rearrange("p (r w) -> p r w", w=64))
            av = a_[:, LC:LT].m(lambda a: a.rearrange("p (r w) -> p r w", w=64))
            P.ts(e, a_[:, LC:LT], p_[:, LC:LT], w(4), ALU.mult)
            for dr in range(3):
                for dw in range(3):
                    if dr == 1 and dw == 1:
                        continue
                    ro = slice(max(0, 1 - dr), 64 - max(0, dr - 1)); ri = slice(max(0, dr - 1), 64 - max(0, 1 - dr))
                    co = slice(max(0, 1 - dw), 64 - max(0, dw - 1)); ci = slice(max(0, dw - 1), 64 - max(0, 1 - dw))
                    o_v = V(av.ap[:, ro, co], av.name); i_v = V(pv.ap[:, ri, ci], pv.name)
                    P.stt(e, o_v, i_v, w(dr * 3 + dw), o_v, ALU.mult, ALU.add)
            P.act(s_[:], a_[:], AF.Silu)
            if j < 4:
                if j % 2 == 0:
                    P.act(o_[:], s_[:], AF.Identity, scale=0.125)
                else:
                    P.ts("vector", o_[:], s_[:], 0.125, ALU.mult)
                P.dma("scalar" if j % 2 == 0 else "sync", C.qT[j * 128:(j + 1) * 128, :], o_[:])
            else:
                jj = j - 4
                P.cp("scalar" if j % 2 == 0 else "vector", o_[:], s_[:])
                P.dma("scalar" if j % 2 == 0 else "sync", C.kT[jj * 128:(jj + 1) * 128, :], o_[:])
                for g4 in range(9):
                    nb = min(4, 34 - g4 * 4)
                    pt = pT[g4 % 2]; ks = kst[g4 % 2]
                    for bI in range(nb):
                        blk = g4 * 4 + bI
                        P.tr(pt[:, bI * 128:(bI + 1) * 128], s_[:, blk * 128:(blk + 1) * 128], K.ident)
                    P.cp("scalar", ks[:, 0:nb, :], pt[:, 0:nb * 128].m(lambda a: a.rearrange("p (b c) -> p b c", c=128)))
                    P.dma("scalar", V(C.ktok.h[g4 * 512: g4 * 512 + nb * 128, jj * 128:(jj + 1) * 128].rearrange("(b p) c -> p b c", p=128), C.ktok.name), ks[:, 0:nb, :])
        P.barrier(); P.flush()


HGRP = [(0, 3), (3, 3), (6, 2)]


def hoff(h):
    return (h // 3) * 512 + (h % 3) * 170


def chunk_orders():
    fwd = list(range(34))
    bwd = [1, 0] + list(range(33, 1, -1))
    return fwd, bwd


def phase3(P, C, K):
    with ExitStack() as ph:
        P.stack = ph
        Cn = P.sb("Cn", [128, 2, 8, 129], F32); Cnb = P.sb("Cnb", [128, 2, 8, 129], BF16)
        P.memset("vector", Cn[:], 0.0); P.memset("vector", Cnb[:], 0.0)
        NB = 2
        v1 = [[P.sb("v1_%d%d" % (d, i), [128, 8, 129], BF16) for i in range(NB)] for d in range(2)]
        glt = [[P.sb("glt_%d%d" % (d, i), [128, 32], F32) for i in range(NB)] for d in range(2)]
        kt = [[P.sb("kt_%d%d" % (d, i), [128, 9, 64], BF16) for i in range(NB)] for d in range(2)]
        qTt = [[P.sb("qTt_%d%d" % (d, i), [128, 8, 128], BF16) for i in range(NB)] for d in range(2)]
        kTt = [[P.sb("kTt_%d%d" % (d, i), [128, 8, 128], BF16) for i in range(NB)] for d in range(2)]
        for d in range(2):
            for i in range(NB):
                P.memset("vector", v1[d][i][:], 1.0)
                P.memset("vector", kt[d][i][:], 0.0)
                P.memset("vector", qTt[d][i][:], 0.0)
                P.memset("vector", kTt[d][i][:], 0.0)
        vw = [P.sb("vw%d" % d, [128, 8, 129], BF16) for d in range(2)]
        PT = [P.sb("PTs%d" % i, [128, 128], BF16) for i in range(4)]
        hd = [P.sb("hd%d" % d, [128, 8, 128], F32) for d in range(2)]
        sm = {n: [P.sb("sm_%s%d" % (n, d), [128, 8], F32) for d in range(2)] for n in ["nb", "t1", "ek", "t2", "w", "dec", "ad", "mx", "r"]}
        pG = P.ps("pG", [128, 512]); pPs = [P.ps("pP%d" % i, [128, 512]) for i in range(2)]
        pAccs = [P.ps("pAcc%d" % i, [128, 512]) for i in range(2)]; pUs = [P.ps("pU%d" % i, [128, 512]) for i in range(2)]
        gi_ctr = [0]; gu_ctr = [0]
        fwd, bwd = chunk_orders()
        pti = 0
        its3 = [(step, d) for step in range(34) for d in range(2)]

        def front(n):
            step, d = its3[n]
            g = (fwd, bwd)[d][step]
            lat = g >= 2
            bi = step % NB
            r0 = g * 128
            tri = K.triF if d == 0 else K.triB
            V1, GL, KT, QT, KTT = v1[d][bi], glt[d][bi], kt[d][bi], qTt[d][bi], kTt[d][bi]
            S = {nm_: sm[nm_][d] for nm_ in sm}
            g = (fwd, bwd)[d][step]
            lat = g >= 2
            bi = step % NB
            r0 = g * 128
            tri = K.triF if d == 0 else K.triB
            V1, GL, KT, QT, KTT = v1[d][bi], glt[d][bi], kt[d][bi], qTt[d][bi], kTt[d][bi]
            P.dma("sync", V1[:, :, 0:128], V(C.v.h[r0:r0 + 128, :].rearrange("p (h c) -> p h c", h=8), C.v.name))
            P.dma("sync", GL[:], C.gl[r0:r0 + 128, :])
            P.dma("sync", KT[:, 0:8, :], V(C.ktok.h[r0:r0 + 128, :].rearrange("p (h c) -> p h c", h=8), C.ktok.name))
            if lat:
                P.dma("sync", QT[0:64, :, :], V(C.qT.h[:, r0:r0 + 128].rearrange("(h k) t -> k h t", k=64), C.qT.name))
                P.dma("sync", KTT[0:64, :, :], V(C.kT.h[:, r0:r0 + 128].rearrange("(h k) t -> k h t", k=64), C.kT.name))
            li = GL[:, 8 * d: 8 * d + 8]; lf = GL[:, 16 + 8 * d: 16 + 8 * d + 8]
            pg = pG[:, 0:8]; pe = pG[:, 8:16]
            P.mm(pg, tri, lf); P.mm(pe, K.ones, lf)
            P.tt("vector", S["t1"][:], li, pg, ALU.subtract)
            P.tt("vector", S["t2"][:], S["t1"][:], pe, ALU.add)
            P.act(S["w"][:], S["t2"][:], AF.Exp)
            P.act(S["dec"][:], pe, AF.Exp)
            if lat:
                P.act(S["nb"][:], pg, AF.Exp, scale=-1.0)
                P.act(S["ek"][:], S["t1"][:], AF.Exp)
            P.tt("vector", vw[d][:], V1[:], S["w"][:].m(lambda a: a.rearrange("p (h o) -> p h o", o=1).to_broadcast([128, 8, 129])), ALU.mult)

        def back(n):
            nonlocal pti
            step, d = its3[n]
            g = (fwd, bwd)[d][step]
            lat = g >= 2
            bi = step % NB
            r0 = g * 128
            tri = K.triF if d == 0 else K.triB
            V1, GL, KT, QT, KTT = v1[d][bi], glt[d][bi], kt[d][bi], qTt[d][bi], kTt[d][bi]
            S = {nm_: sm[nm_][d] for nm_ in sm}
            if lat:
                for gi, (h0, nh) in enumerate(HGRP):
                    pa_bank = pAccs[gi_ctr[0] % 2]; gi_ctr[0] += 1
                    for h in range(h0, h0 + nh):
                        pp = pPs[pti % 2][:, 0:128]
                        P.mm(pp, KTT[:, h, :], QT[:, h, :])
                        pt = PT[pti % 4]; pti += 1
                        P.stt("vector", pt[:], pp, S["ek"][:, h:h + 1], tri, ALU.mult, ALU.mult)
                        pa = pa_bank[:, (h - h0) * 170:(h - h0) * 170 + 129]
                        P.mm(pa, QT[:, h, :], Cnb[:, d, h, :], start=True, stop=False)
                        P.mm(pa, pt[:], V1[:, h, :], start=False, stop=True)
                    accv = pa_bank[:, 0:nh * 170].m(lambda a: a.rearrange("p (h c) -> p h c", c=170))
                    P.act(S["ad"][:, h0:h0 + nh], V(accv.ap[:, :, 128], accv.name), AF.Abs)
                    P.tt("vector", S["mx"][:, h0:h0 + nh], S["ad"][:, h0:h0 + nh], S["nb"][:, h0:h0 + nh], ALU.max)
                    P.op("vector", lambda E, a=S["r"], b=S["mx"], h0=h0, nh=nh: E.reciprocal(a.h[:, h0:h0 + nh], b.h[:, h0:h0 + nh]), [S["mx"][:]], [S["r"][:]])
                    P.tt("vector", hd[d][:, h0:h0 + nh, :], V(accv.ap[:, :, 0:128], accv.name),
                         S["r"][:, h0:h0 + nh].m(lambda a, nh=nh: a.rearrange("p (h o) -> p h o", o=1).to_broadcast([128, nh, 128])), ALU.mult)
                P.dma("sync", V(C.hd[d].h[r0 - LC: r0 - LC + 128, :].rearrange("p (h c) -> p h c", h=8), C.hd[d].name), hd[d][:])
            for gi, (h0, nh) in enumerate(HGRP):
                pu_bank = pUs[gu_ctr[0] % 2]; gu_ctr[0] += 1
                for h in range(h0, h0 + nh):
                    pu = pu_bank[:, (h - h0) * 170:(h - h0) * 170 + 129]
                    P.mm(pu, KT[:, h:h + 2, :].m(lambda a: a.rearrange("p a b -> p (a b)")), vw[d][:, h, :])
                cg = Cn[0:64, d, h0:h0 + nh]
                P.tt("vector", cg, cg, S["dec"][0:64, h0:h0 + nh].m(lambda a, nh=nh: a.rearrange("p (h o) -> p h o", o=1).to_broadcast([64, nh, 129])), ALU.mult)
                uv = V(pu_bank.h[0:64, 0:nh * 170].rearrange("p (h c) -> p h c", c=170)[:, :, 0:129], pu_bank.name)
                P.tt("vector", cg, cg, uv, ALU.add)
                P.cp("scalar", Cnb[0:64, d, h0:h0 + nh], cg)

        front(0)
        for n in range(68):
            if n + 1 < 68:
                front(n + 1)
            back(n)
        P.barrier(); P.flush()

TWO_PI = 6.283185307179586
I32 = mybir.dt.int32


def sincos(P, ang, s_out, c_out, tmp, tmpi, shape):
    for phase_off, dst in ((0.0, s_out), (0.25, c_out)):
        P.ts("vector", tmp, ang, 1.0 / TWO_PI, ALU.mult, phase_off, ALU.add)
        P.cp("vector", tmpi, tmp)
        P.cp("vector", tmp, tmpi)
        P.stt("vector", tmp, tmp, -TWO_PI, ang, ALU.mult, ALU.add)
        if phase_off != 0.0:
            P.ts("vector", tmp, tmp, TWO_PI * phase_off, ALU.add)
        P.ts("vector", tmp, tmp, 3.14159, ALU.min, -3.14159, ALU.max)
        P.act(dst, tmp, AF.Sin)


def phase4(P, C, K):
    with ExitStack() as ph:
        P.stack = ph
        Er = [P.sb("Er%d" % d, [128, 2048], F32) for d in range(2)]; Ei = [P.sb("Ei%d" % d, [128, 2048], F32) for d in range(2)]
        Fr = [P.sb("Fr%d" % d, [128, 16, 128], F32) for d in range(2)]; Fi = [P.sb("Fi%d" % d, [128, 16, 128], F32) for d in range(2)]
        Bbr = [P.sb("Bbr%d" % d, [128, 2048], BF16) for d in range(2)]; Bbi = [P.sb("Bbi%d" % d, [128, 2048], BF16) for d in range(2)]
        Cr = [P.sb("Cr%d" % d, [128, 2048], BF16) for d in range(2)]; Cin = [P.sb("Cin%d" % d, [128, 2048], BF16) for d in range(2)]
        Crn = [P.sb("Crn%d" % d, [128, 2048], BF16) for d in range(2)]
        ntribf = [P.sb("ntribf%d" % d, [128, 128], BF16) for d in range(2)]
        A8r = [P.sb("A8r%d" % d, [128, 16], F32) for d in range(2)]; A8i = [P.sb("A8i%d" % d, [128, 16], F32) for d in range(2)]
        tribf = [P.sb("tribf%d" % d, [128, 128], BF16) for d in range(2)]
        P.cp("vector", tribf[0][:], K.triF); P.cp("vector", tribf[1][:], K.triB)
        P.ts("vector", ntribf[0][:], K.triF, -1.0, ALU.mult); P.ts("vector", ntribf[1][:], K.triB, -1.0, ALU.mult)
        cc = [[(P.sb("cR%d%d" % (d, i), [128, 16], F32), P.sb("cI%d%d" % (d, i), [128, 16], F32)) for i in range(2)] for d in range(2)]
        for d in range(2):
            P.memset("vector", cc[d][0][0][:], 0.0); P.memset("vector", cc[d][0][1][:], 0.0)
        with ExitStack() as su:
            P.stack = su
            T = [P.sb("su%d" % i, [128, 2048], F32) for i in range(10)]
            TI = P.sb("sui", [128, 2048], I32)
            colp = P.sb("colp", [128, 3, 16], F32); lam = P.sb("lamc", [128, 2, 16], F32)
            for d in range(2):
                aRe, aIm, ldt, lRe, lIm, t5, t6, t7, t8, t9 = [t[:] for t in T]
                ti = TI[:]
                P.dma("sync", aRe, C.s5row[d, 0]); P.dma("sync", aIm, C.s5row[d, 1]); P.dma("sync", ldt, C.s5row[d, 2])
                P.act(ldt, ldt, AF.Exp)
                P.tt("vector", lRe, ldt, aRe, ALU.mult); P.tt("vector", lIm, ldt, aIm, ALU.mult)
                P.act(t5, lRe, AF.Exp)
                sincos(P, lIm, t6, t7, t8, ti, None)
                P.tt("vector", t7, t7, t5, ALU.mult)
                P.tt("vector", t6, t6, t5, ALU.mult)
                P.ts("vector", t7, t7, -1.0, ALU.add)
                P.tt("vector", t5, aRe, aRe, ALU.mult); P.tt("vector", t8, aIm, aIm, ALU.mult)
                P.tt("vector", t5, t5, t8, ALU.add)
                P.op("vector", lambda E, a=T[5]: E.reciprocal(a.h[:], a.h[:]), [t5], [t5])
                P.tt("vector", t8, t7, aRe, ALU.mult); P.tt("vector", t9, t6, aIm, ALU.mult)
                P.tt("vector", t8, t8, t9, ALU.add); P.tt("vector", t8, t8, t5, ALU.mult)
                P.tt("vector", t9, t6, aRe, ALU.mult); P.tt("vector", t6, t7, aIm, ALU.mult)
                P.tt("vector", t9, t9, t6, ALU.subtract); P.tt("vector", t9, t9, t5, ALU.mult)
                P.dma("sync", t5, C.bbd[d, 0]); P.dma("sync", t6, C.bbd[d, 1])
                P.tt("vector", t7, t8, t5, ALU.mult); P.tt("vector", aRe, t9, t6, ALU.mult)
                P.tt("vector", Bbr[d][:], t7, aRe, ALU.subtract)
                P.tt("vector", t7, t8, t6, ALU.mult); P.tt("vector", aRe, t9, t5, ALU.mult)
                P.tt("vector", Bbi[d][:], t7, aRe, ALU.add)
                P.dma("sync", t5, C.cbd[d, 0]); P.dma("sync", t6, C.cbd[d, 1])
                P.cp("vector", Cr[d][:], t5); P.ts("vector", Cin[d][:], t6, -1.0, ALU.mult)
                P.ts("vector", Crn[d][:], t5, -1.0, ALU.mult)
                idx = K.cst[:, 6, d:d + 1]
                P.ts("vector", t5, lRe, idx, ALU.mult); P.act(t5, t5, AF.Exp, scale=-1.0)
                P.ts("vector", t6, lIm, idx, ALU.mult)
                sincos(P, t6, t7, t8, t9, ti, None)
                P.tt("vector", Er[d][:], t5, t8, ALU.mult)
                P.tt("vector", t7, t5, t7, ALU.mult); P.ts("vector", Ei[d][:], t7, -1.0, ALU.mult)
                for i in range(3):
                    P.dma("sync", colp[:, i, :], C.s5col[d, i])
                P.act(colp[:, 2, :], colp[:, 2, :], AF.Exp)
                P.tt("vector", lam[:, 0, :], colp[:, 2, :], colp[:, 0, :], ALU.mult)
                P.tt("vector", lam[:, 1, :], colp[:, 2, :], colp[:, 1, :], ALU.mult)
                irow = K.cst[:, 4 + d, :]
                fR = t5.m(lambda a: a.rearrange("p (q t) -> p q t", t=128)); fI = t6.m(lambda a: a.rearrange("p (q t) -> p q t", t=128))
                for q in range(16):
                    P.ts("vector", V(fR.ap[:, q, :], fR.name), irow, lam[:, 0, q:q + 1], ALU.mult)
                    P.ts("vector", V(fI.ap[:, q, :], fI.name), irow, lam[:, 1, q:q + 1], ALU.mult)
                P.act(t5, t5, AF.Exp)
                sincos(P, t6, t7, t8, t9, ti, None)
                P.tt("vector", Fr[d][:].m(lambda a: a.rearrange("p q t -> p (q t)")), t5, t8, ALU.mult)
                P.tt("vector", Fi[d][:].m(lambda a: a.rearrange("p q t -> p (q t)")), t5, t7, ALU.mult)
                s16 = [V(t.h[:, 0:16], t.name) for t in T[5:10]]; s16i = V(TI.h[:, 0:16], TI.name)
                P.ts("vector", s16[0], lam[:, 0, :], 128.0, ALU.mult); P.act(s16[0], s16[0], AF.Exp)
                P.ts("vector", s16[1], lam[:, 1, :], 128.0, ALU.mult)
                sincos(P, s16[1], s16[2], s16[3], s16[4], s16i, None)
                P.tt("vector", A8r[d][:], s16[0], s16[3], ALU.mult); P.tt("vector", A8i[d][:], s16[0], s16[2], ALU.mult)
            P.barrier(); P.flush()
        P.stack = ph
        ut = [[P.sb("ut%d%d" % (d, i), [128, 4, 128], BF16) for i in range(2)] for d in range(2)]
        Z = [[P.sb("Z%d_%d" % (k, d), [128, 2048], BF16) for d in range(2)] for k in range(4)]
        Sp = [(P.sb("Spr%d" % i, [128, 4, 128], F32), P.sb("Spi%d" % i, [128, 4, 128], F32)) for i in range(2)]
        Hq = [[P.sb("Hq%d_%d" % (k, i), [128, 4, 128], BF16) for i in range(2)] for k in range(4)]
        sl = [P.sb("s5l%d" % i, [128, 4], F32) for i in range(4)]
        ysb = [P.sb("ysb%d" % d, [128, 4, 128], F32) for d in range(2)]
        pBr = P.ps("pBr", [128, 512]); pBi = P.ps("pBi", [128, 512])
        pSrs = [P.ps("pSr%d" % i, [128, 512]) for i in range(2)]; pSis = [P.ps("pSi%d" % i, [128, 512]) for i in range(2)]
        pYs = [P.ps("pY%d" % i, [128, 512]) for i in range(2)]
        fwd, bwd = chunk_orders()
        its = [(step, d) for step in range(34) for d in range(2)]

        def BZ(n):
            step, d = its[n]
            g = (fwd, bwd)[d][step]; r0 = g * 128
            u_ = ut[d][step % 2]
            P.dma("sync", u_[:], V(C.uT.h[:, r0:r0 + 128].rearrange("(j p) t -> p j t", p=128), C.uT.name))
            for j in range(4):
                P.mm(pBr[:], u_[:, j, :], Bbr[d][:, j * 512:(j + 1) * 512])
                P.mm(pBi[:], u_[:, j, :], Bbi[d][:, j * 512:(j + 1) * 512])
                hs = slice(j * 512, (j + 1) * 512)
                P.tt("vector", Z[0][d][:, hs], Er[d][:, hs], pBr[:], ALU.mult)
                P.tt("vector", Z[3][d][:, hs], Ei[d][:, hs], pBr[:], ALU.mult)
                P.tt("vector", Z[1][d][:, hs], Ei[d][:, hs], pBi[:], ALU.mult)
                P.tt("vector", Z[2][d][:, hs], Er[d][:, hs], pBi[:], ALU.mult)

        def cumsum(n, qg):
            step, d = its[n]
            b = (4 * n + qg) % 2
            pSr = pSrs[b]; pSi = pSis[b]
            for qq in range(4):
                q = 4 * qg + qq
                ps_r = pSr[:, qq * 128:(qq + 1) * 128]
                P.mm(ps_r, Z[0][d][:, q * 128:(q + 1) * 128], tribf[d][:], start=True, stop=False)
                P.mm(ps_r, Z[1][d][:, q * 128:(q + 1) * 128], ntribf[d][:], start=False, stop=True)
            for qq in range(4):
                q = 4 * qg + qq
                ps_i = pSi[:, qq * 128:(qq + 1) * 128]
                P.mm(ps_i, Z[2][d][:, q * 128:(q + 1) * 128], tribf[d][:], start=True, stop=False)
                P.mm(ps_i, Z[3][d][:, q * 128:(q + 1) * 128], tribf[d][:], start=False, stop=True)

        def evacH(n, qg):
            step, d = its[n]
            lat = (fwd, bwd)[d][step] >= 2
            b = (4 * n + qg) % 2
            pSr = pSrs[b]; pSi = pSis[b]
            cR, cI = cc[d][step % 2]; nR, nI = cc[d][(step + 1) % 2]
            last = 127 if d == 0 else 0
            spr, spi = Sp[b]
            qs = slice(4 * qg, 4 * qg + 4)
            for qq in range(4):
                q = 4 * qg + qq
                P.act(spr[:, qq, :], pSr[:, qq * 128:(qq + 1) * 128], AF.Identity, bias=cR[:, q:q + 1])
            for qq in range(4):
                q = 4 * qg + qq
                P.act(spi[:, qq, :], pSi[:, qq * 128:(qq + 1) * 128], AF.Identity, bias=cI[:, q:q + 1])
            P.tt("vector", sl[0][:], A8r[d][:, qs], spr[:, :, last], ALU.mult)
            P.tt("vector", sl[1][:], A8i[d][:, qs], spi[:, :, last], ALU.mult)
            P.tt("vector", nR[:, qs], sl[0][:], sl[1][:], ALU.subtract)
            P.tt("vector", sl[2][:], A8r[d][:, qs], spi[:, :, last], ALU.mult)
            P.tt("vector", sl[3][:], A8i[d][:, qs], spr[:, :, last], ALU.mult)
            P.tt("vector", nI[:, qs], sl[2][:], sl[3][:], ALU.add)
            if lat:
                H = [Hq[k][b] for k in range(4)]
                P.tt("vector", H[0][:], Fr[d][:, qs, :], spr[:], ALU.mult)
                P.tt("vector", H[1][:], Fi[d][:, qs, :], spi[:], ALU.mult)
                P.tt("vector", H[2][:], Fr[d][:, qs, :], spi[:], ALU.mult)
                P.tt("vector", H[3][:], Fi[d][:, qs, :], spr[:], ALU.mult)

        def Yq(n, qg):
            step, d = its[n]
            if (fwd, bwd)[d][step] < 2:
                return
            b = (4 * n + qg) % 2
            H = [Hq[k][b] for k in range(4)]
            py = pYs[n % 2][:, qg * 128:(qg + 1) * 128]
            for qq in range(4):
                q = 4 * qg + qq
                cs = slice(q * 128, (q + 1) * 128)
                P.mm(py, Cr[d][:, cs], H[0][:, qq, :], start=(qq == 0), stop=False)
                P.mm(py, Crn[d][:, cs], H[1][:, qq, :], start=False, stop=False)
                P.mm(py, Cin[d][:, cs], H[2][:, qq, :], start=False, stop=False)
                P.mm(py, Cin[d][:, cs], H[3][:, qq, :], start=False, stop=(qq == 3))

        BZ(0)
        for n in range(68):
            step, d = its[n]
            g = (fwd, bwd)[d][step]; lat = g >= 2; r0 = g * 128
            if n + 1 < 68:
                BZ(n + 1)
            cumsum(n, 0); evacH(n, 0)
            cumsum(n, 1); evacH(n, 1)
            Yq(n, 0)
            cumsum(n, 2); evacH(n, 2)
            Yq(n, 1)
            cumsum(n, 3); evacH(n, 3)
            Yq(n, 2); Yq(n, 3)
            if lat:
                P.cp("scalar", ysb[d][:], pYs[n % 2][:].m(lambda a: a.rearrange("p (j t) -> p j t", t=128)))
                P.dma("scalar", V(C.yT[d].h[:, r0 - LC: r0 - LC + 128].rearrange("(j p) t -> p j t", p=128), C.yT[d].name), ysb[d][:])
        P.barrier(); P.flush()

def bcl(v, n, m):
    return v.m(lambda a: a.rearrange("p (h o) -> p h o", o=1).to_broadcast([128, n, m]))


def phase5(P, C, K):
    with ExitStack() as ph:
        P.stack = ph
        gmh = P.sb("gmh", [128, 1024], F32); P.dma("sync", gmh[:], C.gmh[:])
        hf = [P.sb("p5hf%d" % i, [128, 8, 128], F32) for i in range(2)]
        hb = [P.sb("p5hb%d" % i, [128, 8, 128], F32) for i in range(2)]
        og = [P.sb("p5og%d" % i, [128, 1024], BF16) for i in range(2)]
        sqs = [P.sb("p5sq%d" % i, [128, 8, 128], F32) for i in range(2)]; hns = [P.sb("p5hn%d" % i, [128, 1024], F32) for i in range(2)]
        st = [P.sb("p5st%d" % i, [128, 8, 128], BF16) for i in range(2)]
        s8s = [{n: P.sb("p5_%s%d" % (n, i), [128, 8], F32) for n in ["ss", "ms", "sq", "r"]} for i in range(2)]
        pT = P.ps("p5pT", [128, 1024])

        def s1(t):
            a, b_, o_ = hf[t % 2], hb[t % 2], og[t % 2]
            sq = sqs[t % 2]; s8 = s8s[t % 2]
            r0 = t * 128
            P.dma("sync", a[:], V(C.hd[0].h[r0:r0 + 128, :].rearrange("p (h c) -> p h c", h=8), C.hd[0].name))
            P.dma("sync", b_[:], V(C.hd[1].h[r0:r0 + 128, :].rearrange("p (h c) -> p h c", h=8), C.hd[1].name))
            P.dma("sync", o_[:], C.og[r0:r0 + 128, :])
            P.tt("vector", a[:], a[:], b_[:], ALU.add)
            for h in range(8):
                P.act(sq[:, h, :], a[:, h, :], AF.Square, accum=s8["ss"][:, h:h + 1])
            P.ts("vector", s8["ms"][:], s8["ss"][:], 1.0 / 128, ALU.mult, EPS, ALU.add)
            P.act(s8["sq"][:], s8["ms"][:], AF.Sqrt)
            P.op("vector", lambda E, s8=s8: E.reciprocal(s8["r"].h[:], s8["sq"].h[:]), [s8["sq"][:]], [s8["r"][:]])

        def s2(t):
            a, o_ = hf[t % 2], og[t % 2]
            hn = hns[t % 2]; s8 = s8s[t % 2]
            r0 = t * 128
            hv = hn[:].m(lambda x: x.rearrange("p (h c) -> p h c", h=8))
            P.tt("vector", hv, a[:], bcl(s8["r"][:], 8, 128), ALU.mult)
            P.tt("vector", hn[:], hn[:], gmh[:], ALU.mult)
            P.tt("vector", hn[:], hn[:], o_[:], ALU.mult)
            for j in range(8):
                P.tr(pT[:, j * 128:(j + 1) * 128], hn[:, j * 128:(j + 1) * 128], K.ident)
            s_ = st[t % 2]
            P.cp("scalar", s_[:], pT[:].m(lambda x: x.rearrange("p (j t) -> p j t", j=8)))
            P.dma("scalar", V(C.hmT.h[:, r0:r0 + 128].rearrange("(j p) t -> p j t", p=128), C.hmT.name), s_[:])

        s1(0)
        for t in range(32):
            if t + 1 < 32:
                s1(t + 1)
            s2(t)
        P.barrier(); P.flush()


def phase6(P, C, K):
    with ExitStack() as ph:
        P.stack = ph
        wA = P.sb("wA", [128, 8, 1024], BF16); wG = P.sb("wG", [128, 4, 512], BF16)
        wB = P.sb("wB", [128, 4, 1024], BF16); wO = P.sb("wO", [128, 8, 1024], BF16)
        wR = P.sb("wR", [128, 8, 32], F32); bR = P.sb("bR", [128, 32], F32)
        bglu = P.sb("bglu", [128, 4], F32); s5d = P.sb("s5dt", [128, 4], F32)
        P.dma("gpsimd", wA[:], V(C.w_a.h.rearrange("(k p) n -> p k n", p=128), C.w_a.name))
        P.dma("gpsimd", wG[:], V(C.w_glu.h.rearrange("(k p) n -> p k n", p=128), C.w_glu.name))
        P.dma("gpsimd", wB[:], V(C.w_b.h.rearrange("(k p) n -> p k n", p=128), C.w_b.name))
        P.dma("sync", wR[:], V(C.w_r.h.rearrange("(k p) n -> p k n", p=128), C.w_r.name))
        P.dma("sync", bR[:], C.b_r[:]); P.dma("sync", bglu[:], C.b_glu[:]); P.dma("sync", s5d[:], C.s5d[:])
        with ExitStack() as su:
            P.stack = su
            wo32 = P.sb("wo32", [128, 8, 1024], F32)
            P.dma("sync", wo32[:], V(C.w_o.h.rearrange("(k p) n -> p k n", p=128), C.w_o.name))
            P.tt("vector", wO[:], wo32[:], K.gt1bc[:].m(lambda a: a.rearrange("p (o n) -> p o n", o=1).to_broadcast([128, 8, 1024])), ALU.mult)
            P.barrier(); P.flush()
        P.stack = ph
        W = norm_work(P, "6")
        hmT = [P.sb("p6hm%d" % i, [128, 8, 512], BF16) for i in range(2)]
        yf = P.sb("p6yf", [128, 4, 512], F32); yb = P.sb("p6yb", [128, 4, 512], F32)
        uT = P.sb("p6u", [128, 4, 512], BF16); GT = [P.sb("p6G%d" % i, [128, 16, 512], BF16) for i in range(2)]
        x2 = P.sb("p6x2", [128, 4, 512], F32); sg = P.sb("p6sg", [128, 4, 512], F32)
        ysg = P.sb("p6ysg", [128, 4, 512], BF16); ys2 = P.sb("p6ys2", [128, 4, 512], BF16)
        sgz = P.sb("p6sgz", [128, 512], F32); m1 = P.sb("p6m1", [128, 512], F32); m2 = P.sb("p6m2", [128, 512], F32)
        mg = P.sb("p6mg", [128, 8, 512], BF16)
        xt = [P.sb("p6xt%d" % i, [128, 1024], F32) for i in range(3)]
        h2s = P.sb("p6h2s", [128, 8, 512], BF16); h2f = P.sb("p6h2f", [128, 8, 128], F32)
        lg = P.sb("p6lg", [128, 32], F32); m8 = P.sb("p6m8", [128, 8], F32); nm = P.sb("p6nm", [128, 1], F32)
        msk = P.sb("p6msk", [128, 32], F32); ex = P.sb("p6ex", [128, 32], F32); ssum = P.sb("p6ssum", [128, 1], F32)
        gts = P.sb("p6gts", [128, 4, 32], F32); gT = P.sb("p6gT", [32, 512], F32)
        pa = [P.ps("p6pa%d" % i, [128, 512]) for i in range(4)]
        pr = P.ps("p6pr", [128, 512]); pgT = P.ps("p6pgT", [128, 512])
        pai = 0
        for t in range(8):
            o0 = t * 512
            hm = hmT[t % 2]; G = GT[t % 2]
            P.dma("sync", hm[:], V(C.hmT.h[:, o0:o0 + 512].rearrange("(j p) t -> p j t", p=128), C.hmT.name))
            P.dma("sync", yf[:], V(C.yT[0].h[:, o0:o0 + 512].rearrange("(j p) t -> p j t", p=128), C.yT[0].name))
            P.dma("sync", yb[:], V(C.yT[1].h[:, o0:o0 + 512].rearrange("(j p) t -> p j t", p=128), C.yT[1].name))
            P.dma("sync", uT[:], V(C.uT.h[:, LC + o0:LC + o0 + 512].rearrange("(j p) t -> p j t", p=128), C.uT.name))
            P.dma("sync", G[:], V(C.GT.h[:, o0:o0 + 512].rearrange("(j p) t -> p j t", p=128), C.GT.name))
            P.tt("vector", yf[:], yf[:], yb[:], ALU.add)
            for j in range(4):
                P.stt("vector", yf[:, j, :], uT[:, j, :], s5d[:, j:j + 1], yf[:, j, :], ALU.mult, ALU.add)
            P.act(x2[:], yf[:], AF.Square)
            P.ts("vector", x2[:], x2[:], 0.044715, ALU.mult, 1.0, ALU.add)
            P.tt("vector", x2[:], x2[:], yf[:], ALU.mult)
            P.act(sg[:], x2[:], AF.Sigmoid, scale=1.5957691216057308)
            P.tt("vector", ysg[:], yf[:], sg[:], ALU.mult)
            for n in range(4):
                pb = pa[pai % 4]; pai += 1
                for k in range(4):
                    P.mm(pb[:], wG[:, k, n * 128:(n + 1) * 128], ysg[:, k, :], start=(k == 0), stop=(k == 3))
                P.act(sgz[:], pb[:], AF.Sigmoid, bias=bglu[:, n:n + 1])
                P.tt("vector", ys2[:, n, :], ysg[:, n, :], sgz[:], ALU.mult)
            for n in range(8):
                pA_ = pa[pai % 4]; pai += 1
                for k in range(8):
                    P.mm(pA_[:], wA[:, k, n * 128:(n + 1) * 128], hm[:, k, :], start=(k == 0), stop=(k == 7))
                pB_ = pa[pai % 4]; pai += 1
                for k in range(4):
                    P.mm(pB_[:], wB[:, k, n * 128:(n + 1) * 128], ys2[:, k, :], start=(k == 0), stop=(k == 3))
                P.tt("vector", m1[:], pA_[:], G[:, n, :], ALU.mult)
                P.tt("vector", m2[:], pB_[:], G[:, 8 + n, :], ALU.mult)
                P.tt("vector", mg[:, n, :], m1[:], m2[:], ALU.add)
            def stA(s):
                nonlocal pai
                x_ = xt[s % 3]
                r0 = o0 + s * 128
                P.dma("sync", x_[:], C.x[r0:r0 + 128, :])
                for hh in range(2):
                    pb = pa[pai % 4]; pai += 1
                    for k in range(8):
                        P.mm(pb[:], mg[:, k, s * 128:(s + 1) * 128], wO[:, k, hh * 512:(hh + 1) * 512], start=(k == 0), stop=(k == 7))
                    P.tt("vector", x_[:, hh * 512:(hh + 1) * 512], x_[:, hh * 512:(hh + 1) * 512], pb[:], ALU.add)
                P.dma("sync", C.x1[r0:r0 + 128, :], x_[:])

            def stB(s):
                x_ = xt[s % 3]
                norm_T(P, K, x_[:], K.S2[:], K.SH2[:], h2s[:, :, s * 128:(s + 1) * 128], W, need_f32=h2f[:])
                for k in range(8):
                    P.mm(pr[:, 0:32], h2f[:, k, :], wR[:, k, :], start=(k == 0), stop=(k == 7))
                P.tt("vector", lg[:], pr[:, 0:32], bR[:], ALU.add)
                P.op("vector", lambda E: E.max(m8.h[:], lg.h[:]), [lg[:]], [m8[:]])
                P.ts("vector", msk[:], lg[:], m8[:, 3:4], ALU.is_ge)
                P.ts("vector", nm[:], m8[:, 0:1], -1.0, ALU.mult)
                P.act(ex[:], lg[:], AF.Exp, bias=nm[:])
                P.tt("vector", ex[:], ex[:], msk[:], ALU.mult)
                P.op("vector", lambda E: E.reduce_sum(ssum.h[:], ex.h[:], AX.X), [ex[:]], [ssum[:]])
                P.op("vector", lambda E: E.reciprocal(ssum.h[:], ssum.h[:]), [ssum[:]], [ssum[:]])
                P.ts("vector", gts[:, s, :], ex[:], ssum[:], ALU.mult)
                P.tr(pgT[0:32, s * 128:(s + 1) * 128], gts[:, s, :], K.ident)

            stA(0); stA(1); stB(0); stA(2); stB(1); stA(3); stB(2); stB(3)
            P.cp("vector", gT[:], pgT[0:32, :])
            P.dma("sync", V(C.h2T.h[:, o0:o0 + 512].rearrange("(j p) t -> p j t", p=128), C.h2T.name), h2s[:])
            P.dma("sync", V(C.gates.h[o0:o0 + 512, :].rearrange("(s p) e -> p s e", p=128), C.gates.name), gts[:])
            P.dma("sync", C.gatesT[:, o0:o0 + 512], gT[:])
        P.barrier(); P.flush()


def phase7(P, C, K):
    with ExitStack() as ph:
        P.stack = ph
        bein = P.sb("bein", [128, 32, 16], F32); P.dma("sync", bein[:], V(C.b_ein.h.rearrange("p (e j) -> p e j", j=16), C.b_ein.name))
        bein1 = P.sb("bein1", [128, 32, 8], F32)
        P.ts("vector", bein1[:], bein[:, :, 8:16], 1.0, ALU.add)
        beo = P.sb("beo", [32, 1024], F32); P.dma("sync", beo[:], C.b_eout[:])
        gfin = P.sb("gfin", [128, 1024], F32); P.dma("sync", gfin[:], C.gfin[:])
        Win = [P.sb("Win%d" % i, [128, 8, 2048], BF16) for i in range(2)]
        Wout = [P.sb("Wout%d" % i, [128, 8, 1024], BF16) for i in range(2)]
        h2 = P.sb("p7h2", [128, 8, 1024], BF16)
        acc = P.sb("p7acc", [128, 8, 1024], F32)
        gt_ = P.sb("p7g", [128, 8, 32], F32); gTt = P.sb("p7gT", [32, 1024], F32)
        actT = [P.sb("p7act%d" % i, [128, 8, 512], BF16) for i in range(2)]
        tg = [P.sb("p7tg%d" % i, [128, 512], F32) for i in range(2)]; ts_ = [P.sb("p7ts%d" % i, [128, 512], F32) for i in range(2)]
        tl = [P.sb("p7tl%d" % i, [128, 512], F32) for i in range(2)]
        x1t = [P.sb("p7x1", [128, 1024], F32)] * 2
        fs = {n: P.sb("p7_" + n, [128, 1], F32) for n in ["ss", "ms", "sq", "r"]}
        pz = [P.ps("p7pz%d" % i, [128, 512]) for i in range(4)]
        po = [P.ps("p7po%d" % i, [128, 512]) for i in range(4)]
        zi = 0; oi = 0; ti = 0; wi = 0
        ck = ("cvt", "sw")
        for eng_ in ("sync", "gpsimd"):
            P._wait(eng_, ("D", ck))
        P.persist.discard(ck)
        rr = lambda a: a.rearrange("(k p) n -> p k n", p=128)
        for grp in range(4):
            g0 = grp * 1024
            P.dma("sync", h2[:], V(C.h2T.h[:, g0:g0 + 1024].rearrange("(j p) t -> p j t", p=128), C.h2T.name))
            P.dma("sync", gt_[:], V(C.gates.h[g0:g0 + 1024, :].rearrange("(s p) e -> p s e", p=128), C.gates.name))
            P.dma("sync", gTt[:], C.gatesT[:, g0:g0 + 1024])
            P.ts("vector", gt_[:], gt_[:], 1.0 / 1.702, ALU.mult)
            for s in range(8):
                for hh in range(2):
                    pb = po[oi % 4]; oi += 1
                    P.mm(pb[:], gTt[:, s * 128:(s + 1) * 128], beo[:, hh * 512:(hh + 1) * 512])
                    P.cp("scalar", acc[:, s, hh * 512:(hh + 1) * 512], pb[:])
            for e in range(NE):
                wi_, wo_ = Win[wi % 2], Wout[wi % 2]; wi += 1
                for kk in range(4):
                    P.dma("sync" if kk < 2 else "gpsimd", wi_.k(("w", kk))[:, 2 * kk:2 * kk + 2, :],
                          V(rr(C.weinb.h[e, kk * 256:(kk + 1) * 256, :]), C.weinb.name))
                for kk in range(2):
                    P.dma("sync" if kk == 0 else "gpsimd", wo_.k(("w", kk))[:, 4 * kk:4 * kk + 4, :],
                          V(rr(C.weoutb.h[e, kk * 512:(kk + 1) * 512, :]), C.weoutb.name))
                for tt in range(2):
                    aT = actT[ti % 2]; ti += 1
                    for jn in range(8):
                        pg_ = pz[zi % 4]; pl_ = pz[(zi + 1) % 4]; zi += 2
                        for k in range(8):
                            P.mm(pg_[:], wi_[:, k, jn * 128:(jn + 1) * 128], h2[:, k, tt * 512:(tt + 1) * 512], start=(k == 0), stop=(k == 7))
                        for k in range(8):
                            P.mm(pl_[:], wi_[:, k, 1024 + jn * 128:1024 + (jn + 1) * 128], h2[:, k, tt * 512:(tt + 1) * 512], start=(k == 0), stop=(k == 7))
                        b = jn % 2
                        P.ts("vector", tg[b][:], pg_[:], bein[:, e, jn:jn + 1], ALU.add, 7.0, ALU.min)
                        P.act(ts_[b][:], tg[b][:], AF.Silu, scale=1.702)
                        P.ts("vector", tl[b][:], pl_[:], bein1[:, e, jn:jn + 1], ALU.add, -6.0, ALU.max)
                        P.stt("vector", aT[:, jn, :], tl[b][:], 8.0, ts_[b][:], ALU.min, ALU.mult)
                    for s in range(4):
                        sub = tt * 4 + s
                        for hh in range(2):
                            pb = po[oi % 4]; oi += 1
                            for k in range(8):
                                P.mm(pb[:], aT[:, k, s * 128:(s + 1) * 128], wo_[:, k, hh * 512:(hh + 1) * 512], start=(k == 0), stop=(k == 7))
                            av = V(acc.h[:, sub, hh * 512:(hh + 1) * 512], acc.name, (sub, hh))
                            P.stt("vector", av, pb[:], gt_[:, sub, e:e + 1], av, ALU.mult, ALU.add)
            for s in range(8):
                r0 = g0 + s * 128
                x_ = x1t[s % 2]
                P.dma("sync", x_[:], C.x1[r0:r0 + 128, :])
                P.tt("vector", acc[:, s, :], acc[:, s, :], K.gt2bc[:], ALU.mult)
                P.tt("vector", x_[:], x_[:], acc[:, s, :], ALU.add)
                P.act(actT[0][:, 0:2, :].m(lambda a: a.rearrange("p a b -> p (a b)")), x_[:], AF.Square, accum=fs["ss"][:])
                P.ts("vector", fs["ms"][:], fs["ss"][:], 1.0 / D, ALU.mult, EPS, ALU.add)
                P.act(fs["sq"][:], fs["ms"][:], AF.Sqrt)
                P.op("vector", lambda E: E.reciprocal(fs["r"].h[:], fs["sq"].h[:]), [fs["sq"][:]], [fs["r"][:]])
                P.act(x_[:], x_[:], AF.Identity, scale=fs["r"][:])
                P.tt("vector", x_[:], x_[:], gfin[:], ALU.mult)
                P.dma("sync", C.out[r0:r0 + 128, :], x_[:])
        P.barrier(); P.flush()

PHASES = 99
DBG = ()


def build_program(phases=99, dbg=(), only=None):
    nc = bass.Bass("TRN2", target_bir_lowering=False)
    with ExitStack() as outer:
        P = Prog(nc, outer)
        C = declare_io(P, dbg)
        K = phase0(P, C)
        if phases >= 1 and (only is None or 1 in only):
            phase1(P, C, K)
        if phases >= 2 and (only is None or 2 in only):
            phase2(P, C, K)
        if phases >= 3 and (only is None or 3 in only):
            phase3(P, C, K)
        if phases >= 4 and (only is None or 4 in only):
            phase4(P, C, K)
        if phases >= 5 and (only is None or 5 in only):
            phase5(P, C, K)
        if phases >= 6 and (only is None or 6 in only):
            phase6(P, C, K)
        if phases >= 7 and (only is None or 7 in only):
            phase7(P, C, K)
        P.stack = outer
        P.finish([C.out[:]])
        P.flush()
        print("instr counts", P.cnt, "waits", P.nwaits, "dma sems", {k: (len(v), max([x[1] for x in v] + [0])) for k, v in P.free_d.items()})
    return nc


_NC_CACHE = {}


def kernel(**inputs):
    inp = {k: np.asarray(v) for k, v in inputs.items()}
    key = (PHASES, DBG)
    if key not in _NC_CACHE:
        _NC_CACHE[key] = build_program(PHASES, DBG)
    nc = _NC_CACHE[key]
    in_maps = [host_prep(inp, b) for b in range(8)]
    res = run_bass_kernel_spmd(nc, in_maps, core_ids=list(range(8)))
    kernel.last = res
    out = np.stack([np.asarray(res.results[b]["dr_out"]) for b in range(8)], axis=0)
    return out.astype(np.float32)
```
